# Optimizing a Trainium2 kernel written in Bass

```python
import numpy as np
import jax, jax.numpy as jnp
from jax import lax

D_MODEL = 1024
BATCH = 2
SEQ = 8192
DEPTH = 4

N_HEADS = 4
HEAD_DIM = D_MODEL // 8
MIX_W = N_HEADS * HEAD_DIM
N_BRANCH = 3
CHUNK = 64
GLA_RANK = 16
GLA_TAU = 16
CONV_K = 4
D_FF = 4 * D_MODEL
EPS = 1e-6

kernel_name = 'hybrid_mlstm_gla_gdn_block'


def _in_sizes():
    H, W = N_HEADS, MIX_W
    return [W, W, W, W, H, H,
            W, W, W, W, GLA_RANK,
            W, W, W, W, H, H,
            N_BRANCH * D_MODEL]


def rmsnorm(x, g):
    x32 = x.astype(jnp.float32)
    y = x32 * lax.rsqrt(jnp.mean(x32 * x32, axis=-1, keepdims=True) + EPS)
    return (y * g.astype(jnp.float32)).astype(x.dtype)


def head_rmsnorm(h, g):
    B_, S_, _ = h.shape
    hh = h.reshape(B_, S_, N_HEADS, HEAD_DIM)
    hh = hh * lax.rsqrt(jnp.mean(hh * hh, axis=-1, keepdims=True) + EPS)
    return hh.reshape(B_, S_, MIX_W) * g.astype(jnp.float32)


def l2norm_heads(h):
    B_, S_, _ = h.shape
    hh = h.reshape(B_, S_, N_HEADS, HEAD_DIM)
    hh = hh * lax.rsqrt(jnp.sum(hh * hh, axis=-1, keepdims=True) + EPS)
    return hh.reshape(B_, S_, MIX_W)


def to_chunks(t):
    B_, S_, HD = t.shape
    t = t.reshape(B_, S_ // CHUNK, CHUNK, N_HEADS, HD // N_HEADS)
    return t.transpose(1, 0, 3, 2, 4)


def scalar_chunks(t):
    B_, S_, H_ = t.shape
    return t.reshape(B_, S_ // CHUNK, CHUNK, H_).transpose(1, 0, 3, 2)


def from_chunks(t):
    NC, B_, H_, L_, d = t.shape
    return t.transpose(1, 0, 3, 2, 4).reshape(B_, NC * L_, H_ * d)


def causal_conv(x, w):
    K = w.shape[0]
    xp = jnp.pad(x, ((0, 0), (K - 1, 0), (0, 0)))
    return lax.conv_general_dilated(xp, w[:, None, :], window_strides=(1,), padding='VALID',
                                    dimension_numbers=('NWC', 'WIO', 'NWC'),
                                    feature_group_count=x.shape[-1])


def mlstm_chunk(carry, xs):
    C, n, m = carry
    q, k, v, li, lf = xs
    causal = jnp.tril(jnp.ones((CHUNK, CHUNK), dtype=bool))
    b = jnp.cumsum(lf, axis=-1)
    D = jnp.where(causal, b[..., :, None] - b[..., None, :] + li[..., None, :], -jnp.inf)
    inter = b + m[..., None]
    m_t = jnp.maximum(inter, jnp.max(D, axis=-1))
    P = jnp.exp(D - m_t[..., None]) * jnp.einsum('bhtk,bhsk->bhts', q, k)
    w_inter = jnp.exp(inter - m_t)
    num = jnp.einsum('bhts,bhsv->bhtv', P, v) + w_inter[..., None] * jnp.einsum('bhtk,bhkv->bhtv', q, C)
    den = jnp.sum(P, axis=-1) + w_inter * jnp.einsum('bhtk,bhk->bht', q, n)
    h = num / jnp.maximum(jnp.abs(den), jnp.exp(-m_t))[..., None]
    bL = b[..., -1]
    g_s = bL[..., None] - b + li
    m_new = jnp.maximum(bL + m, jnp.max(g_s, axis=-1))
    w_s = jnp.exp(g_s - m_new[..., None])
    w_old = jnp.exp(bL + m - m_new)
    C = w_old[..., None, None] * C + jnp.einsum('bhsk,bhsv->bhkv', k * w_s[..., None], v)
    n = w_old[..., None] * n + jnp.einsum('bhs,bhsk->bhk', w_s, k)
    return (C, n, m_new), h


def mlstm(q, k, v, li, lf):
    B_ = q.shape[0]
    init = (jnp.zeros((B_, N_HEADS, HEAD_DIM, HEAD_DIM), jnp.float32),
            jnp.zeros((B_, N_HEADS, HEAD_DIM), jnp.float32),
            jnp.zeros((B_, N_HEADS), jnp.float32))
    xs = (to_chunks(q * HEAD_DIM ** -0.5), to_chunks(k), to_chunks(v), scalar_chunks(li), scalar_chunks(lf))
    _, hs = lax.scan(mlstm_chunk, init, xs)
    return from_chunks(hs)


def gla_chunk(S, xs):
    q, k, v, g = xs
    causal = jnp.tril(jnp.ones((CHUNK, CHUNK), dtype=bool))
    b = jnp.cumsum(g, axis=2)
    diff = b[:, :, :, None, :] - b[:, :, None, :, :]
    decay = jnp.exp(jnp.where(causal[:, :, None], diff, -jnp.inf))
    A = jnp.einsum('bhtsk,bhsk->bhts', q[:, :, :, None, :] * decay, k)
    o = jnp.einsum('bhts,bhsv->bhtv', A, v) + jnp.einsum('bhtk,bhkv->bhtv', q * jnp.exp(b), S)
    bL = b[:, :, -1:, :]
    S = jnp.exp(bL[:, :, 0, :])[..., None] * S + jnp.einsum('bhsk,bhsv->bhkv', k * jnp.exp(bL - b), v)
    return S, o


def gla(q, k, v, g):
    B_ = q.shape[0]
    init = jnp.zeros((B_, N_HEADS, HEAD_DIM, HEAD_DIM), jnp.float32)
    xs = (to_chunks(q * HEAD_DIM ** -0.5), to_chunks(k), to_chunks(v), to_chunks(g))
    _, os_ = lax.scan(gla_chunk, init, xs)
    return from_chunks(os_)


def gdn_chunk(S, xs):
    q, k, v, g, beta = xs
    causal = jnp.tril(jnp.ones((CHUNK, CHUNK), dtype=bool))
    strict = jnp.tril(jnp.ones((CHUNK, CHUNK), dtype=bool), -1)
    b = jnp.cumsum(g, axis=-1)
    decay = jnp.exp(jnp.where(causal, b[..., :, None] - b[..., None, :], -jnp.inf))
    kk = jnp.einsum('bhtk,bhsk->bhts', k, k)
    A = jnp.where(strict, beta[..., None] * kk * decay, 0.0)
    eye = jnp.eye(CHUNK, dtype=A.dtype)
    rhs = jnp.concatenate([beta[..., None] * k * jnp.exp(b)[..., None], beta[..., None] * v], axis=-1)
    sol = lax.linalg.triangular_solve(eye + A, rhs, left_side=True, lower=True, unit_diagonal=True)
    w, u = sol[..., :HEAD_DIM], sol[..., HEAD_DIM:]
    v_new = u - jnp.einsum('bhtk,bhkv->bhtv', w, S)
    qk = jnp.einsum('bhtk,bhsk->bhts', q, k) * decay
    o = jnp.einsum('bhtk,bhkv->bhtv', q * jnp.exp(b)[..., None], S) + jnp.einsum('bhts,bhsv->bhtv', qk, v_new)
    bL = b[..., -1]
    S = jnp.exp(bL)[..., None, None] * S + jnp.einsum('bhsk,bhsv->bhkv', k * jnp.exp(bL[..., None] - b)[..., None], v_new)
    return S, o


def gated_deltanet(q, k, v, g, beta):
    B_ = q.shape[0]
    init = jnp.zeros((B_, N_HEADS, HEAD_DIM, HEAD_DIM), jnp.float32)
    xs = (to_chunks(q), to_chunks(k), to_chunks(v), scalar_chunks(g), scalar_chunks(beta))
    _, os_ = lax.scan(gdn_chunk, init, xs)
    return from_chunks(os_)


def mixer_block(u, w_in, b_if, w_gla_lr, b_gla, conv_w, a_log, dt_bias, g_hm, g_hl, g_hd, w_up, w_out):
    f32 = jnp.float32
    proj = u @ w_in
    idx = [int(i) for i in np.cumsum(_in_sizes())[:-1]]
    (mq, mk, mv, mo, mi, mf, lq, lk, lv, lr, llr, dq, dk, dv, dz, dbeta, da, gates) = jnp.split(proj, idx, axis=-1)
    B_, S_, _ = u.shape
    b_if = b_if.astype(f32)
    li = mi.astype(f32) + b_if[:N_HEADS]
    lf = jax.nn.log_sigmoid(mf.astype(f32) + b_if[N_HEADS:])
    h_m = mlstm(mq.astype(f32), mk.astype(f32), mv.astype(f32), li, lf)
    h_m = head_rmsnorm(jax.nn.sigmoid(mo.astype(f32)) * h_m, g_hm)
    lg = jax.nn.log_sigmoid(llr.astype(f32) @ w_gla_lr.astype(f32) + b_gla.astype(f32)) / GLA_TAU
    h_l = gla(lq.astype(f32), lk.astype(f32), lv.astype(f32), lg)
    h_l = head_rmsnorm(h_l, g_hl) * jax.nn.silu(lr.astype(f32))
    qkv = jax.nn.silu(causal_conv(jnp.concatenate([dq, dk, dv], axis=-1).astype(f32), conv_w.astype(f32)))
    gq, gk, gv = jnp.split(qkv, 3, axis=-1)
    gq = l2norm_heads(gq) * HEAD_DIM ** -0.5
    gk = l2norm_heads(gk)
    g_dec = -jnp.exp(a_log.astype(f32)) * jax.nn.softplus(da.astype(f32) + dt_bias.astype(f32))
    beta = jax.nn.sigmoid(dbeta.astype(f32))
    h_d = gated_deltanet(gq, gk, gv, g_dec, beta)
    h_d = head_rmsnorm(h_d, g_hd) * jax.nn.silu(dz.astype(f32))
    branches = jnp.stack([h_m, h_l, h_d], axis=2).astype(u.dtype)
    up = jnp.einsum('bsnc,ncd->bsnd', branches, w_up)
    gate = jax.nn.sigmoid(gates.reshape(B_, S_, N_BRANCH, D_MODEL))
    return jnp.sum(gate * up, axis=2) @ w_out


def setup_inputs(seed: int = 0) -> dict:
    key = jax.random.key(seed)
    ks = jax.random.split(key, 20)
    n_in = sum(_in_sizes())

    def nrm(k, shape, scale):
        return jax.random.normal(k, shape, jnp.float32) * scale

    x = nrm(ks[0], (BATCH, SEQ, D_MODEL), 1.0)
    w_in = nrm(ks[1], (DEPTH, D_MODEL, n_in), D_MODEL ** -0.5)
    f_bias = jnp.linspace(3.0, 6.0, N_HEADS, dtype=jnp.float32)
    b_if = jnp.concatenate([nrm(ks[2], (DEPTH, N_HEADS), 0.1),
                            f_bias + nrm(ks[3], (DEPTH, N_HEADS), 0.1)], axis=-1)
    w_gla_lr = nrm(ks[4], (DEPTH, GLA_RANK, MIX_W), GLA_RANK ** -0.5)
    b_gla = nrm(ks[5], (DEPTH, MIX_W), 0.1)
    conv_gdn = nrm(ks[6], (DEPTH, CONV_K, 3 * MIX_W), CONV_K ** -0.5)
    a_log = jnp.log(jax.random.uniform(ks[7], (DEPTH, N_HEADS), jnp.float32, 1.0, 16.0))
    dt = jnp.exp(jax.random.uniform(ks[8], (DEPTH, N_HEADS), jnp.float32, np.log(1e-3), np.log(1e-1)))
    dt_bias = dt + jnp.log(-jnp.expm1(-dt))
    g_norm_mix = 1.0 + nrm(ks[9], (DEPTH, D_MODEL), 0.02)
    g_norm_mlp = 1.0 + nrm(ks[10], (DEPTH, D_MODEL), 0.02)
    g_head_mlstm = 1.0 + nrm(ks[11], (DEPTH, MIX_W), 0.02)
    g_head_gla = 1.0 + nrm(ks[12], (DEPTH, MIX_W), 0.02)
    g_head_gdn = 1.0 + nrm(ks[13], (DEPTH, MIX_W), 0.02)
    w_up = nrm(ks[14], (DEPTH, N_BRANCH, MIX_W, D_MODEL), MIX_W ** -0.5)
    w_out = nrm(ks[15], (DEPTH, D_MODEL, D_MODEL), D_MODEL ** -0.5)
    w_mlp_in = nrm(ks[16], (DEPTH, D_MODEL, D_FF), D_MODEL ** -0.5)
    w_mlp_out = nrm(ks[17], (DEPTH, D_FF, D_MODEL), D_FF ** -0.5)
    g_final = 1.0 + nrm(ks[18], (D_MODEL,), 0.02)
    return {'x': x, 'w_in': w_in, 'b_if': b_if, 'w_gla_lr': w_gla_lr, 'b_gla': b_gla,
            'conv_gdn': conv_gdn, 'a_log': a_log, 'dt_bias': dt_bias,
            'g_norm_mix': g_norm_mix, 'g_norm_mlp': g_norm_mlp,
            'g_head_mlstm': g_head_mlstm, 'g_head_gla': g_head_gla, 'g_head_gdn': g_head_gdn,
            'w_up': w_up, 'w_out': w_out, 'w_mlp_in': w_mlp_in, 'w_mlp_out': w_mlp_out,
            'g_final': g_final}


def reference(x, w_in, b_if, w_gla_lr, b_gla, conv_gdn, a_log, dt_bias, g_norm_mix, g_norm_mlp,
              g_head_mlstm, g_head_gla, g_head_gdn, w_up, w_out, w_mlp_in, w_mlp_out, g_final):
    for l in range(DEPTH):
        u = rmsnorm(x, g_norm_mix[l])
        x = x + mixer_block(u, w_in[l], b_if[l], w_gla_lr[l], b_gla[l], conv_gdn[l], a_log[l], dt_bias[l],
                            g_head_mlstm[l], g_head_gla[l], g_head_gdn[l], w_up[l], w_out[l]).astype(x.dtype)
        u = rmsnorm(x, g_norm_mlp[l])
        x = x + jnp.square(jax.nn.relu(u @ w_mlp_in[l])) @ w_mlp_out[l]
    return rmsnorm(x, g_final)
```

```python
import numpy as np
from contextlib import ExitStack
import concourse.bass as bass
import concourse.mybir as mybir
from concourse.bass_utils import run_bass_kernel_spmd

F32 = mybir.dt.float32
BF16 = mybir.dt.bfloat16
AF = mybir.ActivationFunctionType
ALU = mybir.AluOpType

NCORES = 8
D = 1024
SEQ = 8192
TOK = 2048
NBLK = SEQ // 512
EPS = 1e-6
HD = 128
QS = HD ** -0.5
NPRM = 36
NEGV = -30000.0
FUSED = True

O_MQ, O_MK, O_MV, O_MO, O_MI, O_MF = 0, 512, 1024, 1536, 2048, 2052
O_LQ, O_LK, O_LV, O_LR, O_LLR = 2056, 2568, 3080, 3592, 4104
O_DQ, O_DK, O_DV, O_DZ, O_DB, O_DA, O_G = 4120, 4632, 5144, 5656, 6168, 6172, 6176
WH_F, WH_T1, WH_T2, WH_L = 896, 388, 512, 16
WH = WH_F + WH_T1 + WH_T2 + WH_L

CST = {}
_off = 0
for _n, _w in [("NU", 128), ("UM", 128), ("NEGI", 128), ("NEGS", 128), ("ID", 128), ("ONE", 128),
               ("LMU", 7 * 128), ("LML", 7 * 128)]:
    CST[_n] = (_off, _w)
    _off += _w
NCST = _off


def make_consts():
    c = np.zeros((128, NCST), np.float32)
    s = np.arange(128)[:, None]
    t = np.arange(128)[None, :]
    def put(n, a):
        o, w = CST[n]
        c[:, o:o + w] = a.reshape(128, w)
    put("NU", -(s <= t).astype(np.float32))
    put("UM", (s <= t).astype(np.float32))
    put("NEGI", np.where(s <= t, 0.0, NEGV).astype(np.float32))
    put("NEGS", np.where(s < t, 0.0, NEGV).astype(np.float32))
    put("ID", np.eye(128, dtype=np.float32))
    put("ONE", np.ones((128, 128), np.float32))
    lmu = np.zeros((128, 7, 128), np.float32)
    for i in range(7):
        b = 1 << i
        m = ((s // (2 * b)) == (t // (2 * b))) & ((s % (2 * b)) < b) & ((t % (2 * b)) >= b)
        lmu[:, i, :] = m
    put("LMU", lmu)
    put("LML", np.ascontiguousarray(lmu.transpose(2, 1, 0)))
    return c


class Sched:
    CE = ("pe", "act", "dve", "pool")

    def __init__(self, nc, es, ndma=8):
        self.nc = nc
        self.prog = {e: [] for e in ("pe", "act", "dve", "pool", "sp")}
        self.esem = {e: es.enter_context(nc.semaphore("s_" + e)) for e in self.CE}
        self.ecnt = {e: 0 for e in self.CE}
        self.dsem = {q: [es.enter_context(nc.semaphore("d_%s%d" % (q, i))) for i in range(ndma)]
                     for q in ("sp", "pool")}
        self.dcnt = {q: [0] * ndma for q in ("sp", "pool")}
        self.drr = {"sp": 0, "pool": 0}
        self.ccsem = es.enter_context(nc.semaphore("s_cc"))
        self.cccnt = 0
        self.seen = {e: {} for e in self.prog}
        self.lastw = {}
        self.readers = {}
        self.nops = 0
        self.enabled = True

    @staticmethod
    def key(x):
        if isinstance(x, (str, tuple)):
            return x
        t = getattr(x, "tensor", None)
        return t.name if t is not None else x.name

    PSUM_NAMES = ("PF0", "PF1", "PT1", "PT2", "PM", "PN", "PO", "PB")

    def _deps(self, reads, writes, me=None):
        deps = []
        for r in reads:
            t = self.lastw.get(r)
            if t is not None:
                deps.append(t)
            if r in self.PSUM_NAMES:
                for k, tk in self.readers.get(r, {}).items():
                    if k != me:
                        deps.append(tk)
        for w in writes:
            t = self.lastw.get(w)
            if t is not None:
                deps.append(t)
            deps.extend(self.readers.get(w, {}).values())
        return deps

    def _commit(self, tok, reads, writes):
        for r in reads:
            d = self.readers.setdefault(r, {})
            k = tok[0]
            if k not in d or d[k][2] < tok[2]:
                d[k] = tok
        for w in writes:
            self.lastw[w] = tok
            self.readers[w] = {}

    def _add(self, eng, deps, fn, tok, inc):
        waits = {}
        for (k, sem, val, peng) in deps:
            if eng == "pe" and peng == "pe":
                continue
            if self.seen[eng].get(k, 0) >= val:
                continue
            if k not in waits or waits[k][1] < val:
                waits[k] = (sem, val)
        for k, (sem, val) in waits.items():
            self.seen[eng][k] = val
        self.prog[eng].append((list(waits.values()), fn, tok, inc))
        self.nops += 1

    def section(self, k):
        self.enabled = (k <= SEC_LIMIT)

    def op(self, eng, fn, reads=(), writes=()):
        if not self.enabled:
            return
        reads = [self.key(r) for r in reads]
        writes = [self.key(w) for w in writes]
        deps = self._deps(reads, writes, "e_" + eng)
        self.ecnt[eng] += 1
        tok = ("e_" + eng, self.esem[eng], self.ecnt[eng], eng)
        self._add(eng, deps, fn, tok, 1)
        self._commit(tok, reads, writes)

    def dma(self, q, fn, reads=(), writes=()):
        if not self.enabled:
            return
        reads = [self.key(r) for r in reads]
        writes = [self.key(w) for w in writes]
        deps = self._deps(reads, writes)
        i = self.drr[q]
        self.drr[q] = (i + 1) % len(self.dsem[q])
        k = "d_%s%d" % (q, i)
        sem = self.dsem[q][i]
        if self.dcnt[q][i] > 0:
            deps.append((k, sem, self.dcnt[q][i], "dma"))
        self.dcnt[q][i] += 16
        tok = (k, sem, self.dcnt[q][i], "dma")
        self._add(q, deps, fn, tok, 16)
        self._commit(tok, reads, writes)

    def cc(self, fn, reads=(), writes=()):
        if not self.enabled:
            return
        reads = [self.key(r) for r in reads]
        writes = [self.key(w) for w in writes]
        deps = self._deps(reads, writes)
        if self.cccnt > 0:
            deps.append(("cc", self.ccsem, self.cccnt, "cc"))
        self.cccnt += 1
        tok = ("cc", self.ccsem, self.cccnt, "cc")
        self._add("pool", deps, fn, tok, 1)
        self._commit(tok, reads, writes)

    def barrier(self):
        deps = []
        for e in self.CE:
            if self.ecnt[e] > 0:
                deps.append(("e_" + e, self.esem[e], self.ecnt[e], e + "_b"))
        for q in ("sp", "pool"):
            for i, sem in enumerate(self.dsem[q]):
                if self.dcnt[q][i] > 0:
                    deps.append(("d_%s%d" % (q, i), sem, self.dcnt[q][i], "dma"))
        if self.cccnt > 0:
            deps.append(("cc", self.ccsem, self.cccnt, "cc"))
        for e in self.prog:
            self._add(e, list(deps), None, None, 0)

    def final_wait(self, eng, keys):
        deps = []
        for k in keys:
            t = self.lastw.get(self.key(k))
            if t is not None:
                deps.append(t)
        self._add(eng, deps, None, None, 0)

    def emit(self):
        nc = self.nc
        with nc.Block() as block:
            def run(name, e):
                for waits, fn, tok, inc in self.prog[name]:
                    for sem, val in waits:
                        e.wait_ge(sem, val)
                    if fn is None:
                        continue
                    ins = fn(e)
                    ins.then_inc(tok[1], inc)

            @block.tensor
            def _(e):
                run("pe", e)

            @block.scalar
            def _(e):
                run("act", e)

            @block.vector
            def _(e):
                run("dve", e)

            @block.gpsimd
            def _(e):
                run("pool", e)

            @block.sync
            def _(e):
                run("sp", e)


def build_program(nl, do_final, dbg=False, stop=None):
    nc = bass.Bass("TRN2", target_bir_lowering=False)
    es = ExitStack()

    def din(name, shape, dt=F32):
        return nc.dram_tensor(name, list(shape), dt, kind="ExternalInput").ap()

    xT_d = din("xT", [D, TOK])
    wh_d = din("wh", [nl, 128, 8 * WH])
    wlr_d = din("wlr", [nl, 17, 128])
    prm_d = din("prm", [nl, 128, NPRM])
    gfin_d = din("gfin", [128, 8])
    msel_d = din("msel", [128, 4])
    cst_d = din("cst", [128, NCST])
    wgu_d = din("wgu", [nl, 8, 128, 36 * 128])
    wo_d = din("wo", [nl, 8, 128, 1024])
    w1_d = din("w1", [nl, 32, 128, 1024])
    w2_d = din("w2", [nl, 32, 128, 1024])
    out_d = nc.dram_tensor("outT", [D, TOK], F32, kind="ExternalOutput").ap()
    u_loc = [nc.dram_tensor("u_loc%d" % k, [D, 512], BF16, kind="Internal").ap() for k in range(4)]
    u_all = [nc.dram_tensor("u_all%d" % k, [4 * D, 512], BF16, kind="Internal").ap() for k in range(4)]
    br_loc = [nc.dram_tensor("br_loc%d" % k, [384, 1024], BF16, kind="Internal").ap() for k in range(8)]
    br_all = [nc.dram_tensor("br_all%d" % k, [4 * 384, 1024], BF16, kind="Internal").ap() for k in range(8)]
    if dbg:
        dbg_br = nc.dram_tensor("dbg_br", [384, SEQ], BF16, kind="ExternalOutput").ap()
    groups = [[0, 1, 2, 3], [4, 5, 6, 7]]

    S = Sched(nc, es)

    def sb(name, shape, dt=F32):
        return es.enter_context(nc.sbuf_tensor(name, list(shape), dt))

    def ps(name, shape, dt=F32):
        return es.enter_context(nc.psum_tensor(name, list(shape), dt))

    def rw(reads, writes):
        return [r for r in reads if r is not None and not isinstance(r, (int, float))], writes

    def mm(out, lhsT, rhs, start=True, stop=True):
        S.op("pe", lambda e: e.matmul(out, lhsT=lhsT, rhs=rhs, start=start, stop=stop),
             reads=[lhsT, rhs], writes=[out])

    def tr(out, in_, ident):
        S.op("pe", lambda e: e.transpose(out, in_, ident), reads=[in_, ident], writes=[out])

    def act(out, in_, func, bias=None, scale=None, eng="act"):
        kw = {}
        r = [in_]
        if bias is not None:
            kw["bias"] = bias
            if not isinstance(bias, (int, float)):
                r.append(bias)
        if scale is not None:
            kw["scale"] = scale
            if not isinstance(scale, (int, float)):
                r.append(scale)
        S.op("act", lambda e: e.activation(out=out, in_=in_, func=func, **kw), reads=r, writes=[out])

    def tt(eng, out, in0, in1, op):
        S.op(eng, lambda e: e.tensor_tensor(out=out, in0=in0, in1=in1, op=op), reads=[in0, in1], writes=[out])

    def tsc(eng, out, in0, s1, op0, s2=None, op1=None):
        r = [in0] + [s for s in (s1, s2) if s is not None and not isinstance(s, (int, float))]
        if op1 is None:
            S.op(eng, lambda e: e.tensor_scalar(out=out, in0=in0, scalar1=s1, scalar2=None, op0=op0),
                 reads=r, writes=[out])
        else:
            S.op(eng, lambda e: e.tensor_scalar(out=out, in0=in0, scalar1=s1, scalar2=s2, op0=op0, op1=op1),
                 reads=r, writes=[out])

    def stt(out, in0, scalar, in1, op0, op1):
        r = [in0, in1] + ([scalar] if not isinstance(scalar, (int, float)) else [])
        S.op("dve", lambda e: e.scalar_tensor_tensor(out=out, in0=in0, scalar=scalar, in1=in1, op0=op0, op1=op1),
             reads=r, writes=[out])

    def cp(eng, out, in_):
        if eng == "act":
            S.op("act", lambda e: e.activation(out=out, in_=in_, func=AF.Copy), reads=[in_], writes=[out])
        else:
            S.op(eng, lambda e: e.tensor_copy(out=out, in_=in_), reads=[in_], writes=[out])

    def recip(out, in_):
        S.op("dve", lambda e: e.reciprocal(out=out, in_=in_), reads=[in_], writes=[out])

    def memset(eng, ap, v):
        S.op(eng, lambda e: e.memset(ap, v), reads=[], writes=[ap])

    def dma(q, out, in_, **kw):
        S.dma(q, lambda e: e.dma_start(out=out, in_=in_, **kw), reads=[in_], writes=[out])

    def ldw(out, in_):
        S.dma("pool", lambda e: e.dma_start(out=out, in_=in_, max_dma_last_dim=8192), reads=[in_], writes=[out])

    xT = sb("xT_s", [128, 8, TOK])
    cst = sb("cst_s", [128, 6 * 128])
    idb = sb("idb", [128, 128], BF16)
    oneb = sb("oneb", [128, 128], BF16)
    lmu = sb("lmu", [128, 7, 128], BF16)
    lml = sb("lml", [128, 7, 128], BF16)
    prm = sb("prm_s", [128, NPRM])
    drv = sb("drv_s", [128, 4])
    gfin = sb("gfin_s", [128, 8])
    msel = sb("msel_s", [128, 4])

    def C(n):
        o, w = CST[n]
        return cst[:, o:o + w]

    NU, UM, NEGI, NEGS, ID32, ONE32 = C("NU"), C("UM"), C("NEGI"), C("NEGS"), C("ID"), C("ONE")

    PF = [ps("PF0", [128, 512]), ps("PF1", [128, 512])]
    PT1 = ps("PT1", [128, 512])
    PT2 = ps("PT2", [128, 512])
    PM = ps("PM", [128, 512])
    PN = ps("PN", [128, 512])
    PO = ps("PO", [128, 512])
    PB = ps("PB", [128, 1024], BF16)

    dma("sp", cst[:, :], cst_d[:, 0:6 * 128])
    dma("sp", gfin[:, :], gfin_d)
    dma("sp", msel[:, :], msel_d)
    dma("sp", xT[:, :, :], xT_d.rearrange("(kt p) t -> p kt t", p=128))
    cp("dve", idb[:, :], ID32)
    cp("dve", oneb[:, :], ONE32)
    o_, w_ = CST["LMU"]
    ldw(lmu[:, :, :], cst_d[:, o_:o_ + w_].rearrange("p (a b) -> p a b", a=7))
    o_, w_ = CST["LML"]
    ldw(lml[:, :, :], cst_d[:, o_:o_ + w_].rearrange("p (a b) -> p a b", a=7))

    G_MIX, G_MLP, G_HM, G_HL, G_HD, CW = 4, 12, 20, 21, 22, 23

    def pcol(i):
        return prm[:, i:i + 1]

    def rmsnorm_block(blk, gbase, gt, out_tile, sq, rs1, rs2, out_sl=slice(0, 512)):
        tsl = slice(blk * 512, (blk + 1) * 512)
        act(sq[:, :, :], xT[:, :, tsl], AF.Square)
        for kt in range(8):
            mm(PF[0][:, :], oneb[:, :], sq[:, kt, :], start=(kt == 0), stop=(kt == 7))
        act(rs1[:, :], PF[0][:, :], AF.Ln, bias=EPS, scale=1.0 / D)
        act(rs2[:, :], rs1[:, :], AF.Exp, scale=-0.5)
        for kt in range(8):
            stt(out_tile[:, kt, out_sl], xT[:, kt, tsl], gt[:, gbase + kt:gbase + kt + 1], rs2[:, :], ALU.mult, ALU.mult)

    for l in range(nl):
        if stop == "p0":
            break
        dma("sp", prm[:, :], prm_d[l])
        tsc("dve", drv[:, 0:1], prm[:, 1:2], -1.0, ALU.mult)
        act(drv[:, 1:2], prm[:, 2:3], AF.Exp)

        with ExitStack() as p1:
            def sb1(name, shape, dt=F32):
                return p1.enter_context(nc.sbuf_tensor("%s_l%d" % (name, l), list(shape), dt))
            sq = sb1("p1sq", [128, 8, 512], BF16)
            rs1 = sb1("p1rs1", [128, 512])
            rs2 = sb1("p1rs2", [128, 512])
            ub = [sb1("p1u0", [128, 8, 512], BF16), sb1("p1u1", [128, 8, 512], BF16)]
            for blk in range(4):
                rmsnorm_block(blk, G_MIX, prm, ub[blk % 2], sq, rs1, rs2)
                dma("sp", u_loc[blk].rearrange("(kt p) t -> p kt t", p=128), ub[blk % 2][:, :, :])
                S.cc(lambda e, blk=blk: e.collective_compute("AllGather", ALU.bypass, replica_groups=groups,
                                                             ins=[u_loc[blk]], outs=[u_all[blk]]),
                     reads=[u_loc[blk]], writes=[u_all[blk]])
            S.barrier()
        if stop == "p1":
            break

        with ExitStack() as p2:
            def sb2(name, shape, dt=F32):
                return p2.enter_context(nc.sbuf_tensor("%s_l%d" % (name, l), list(shape), dt))

            whs2 = sb2("whs", [128, 8 * WH], BF16)
            whs = whs2[:, :].rearrange("p (kt c) -> p kt c", kt=8)
            wlr = sb2("wlr_s", [17, 128])
            ldw(whs2[:, :], wh_d[l])
            dma("sp", wlr[:, :], wlr_d[l])
            WF = lambda kt, g: whs[:, kt, g * 128:(g + 1) * 128]
            WT1 = lambda kt: whs[:, kt, WH_F:WH_F + WH_T1]
            WT2 = lambda kt: whs[:, kt, WH_F + WH_T1:WH_F + WH_T1 + WH_T2]
            WLL = lambda kt: whs[:, kt, WH_F + WH_T1 + WH_T2:WH]

            ut = [sb2("ut0", [128, 8, 512], BF16), sb2("ut1", [128, 8, 512], BF16)]
            qTm = sb2("qTm", [128, 512], BF16)
            kTm = sb2("kTm", [128, 512], BF16)
            lqT = sb2("lqT", [128, 512])
            lkT = sb2("lkT", [128, 512])
            XC = [sb2("XC%d" % m, [128, 515]) for m in range(3)]
            cacc = sb2("cacc", [128, 512])
            csil = [sb2("csil%d" % m, [128, 512]) for m in range(2)]
            csq = sb2("csq", [128, 512], BF16)
            crn1 = sb2("crn1", [128, 512])
            crn2 = sb2("crn2", [128, 512])
            dT = [sb2("dT%d" % m, [128, 512], BF16) for m in range(3)]
            llrT = sb2("llrT", [17, 512])
            vaug = sb2("vaug", [128, 4, 192], BF16)
            sigo = sb2("sigo", [128, 4, 128], BF16)
            sm = sb2("sm", [128, 4, 4])
            km = sb2("km", [128, 4, 128])
            kl = sb2("kl", [128, 4, 128])
            vl = sb2("vl", [128, 4, 128], BF16)
            silr = sb2("silr", [128, 4, 128], BF16)
            silz = sb2("silz", [128, 4, 128], BF16)
            gt = sb2("gt", [128, 8, 4])
            LFB = sb2("LFB", [128, 128])
            LFBd = sb2("LFBd", [128, 128])
            coltm = sb2("coltm", [128, 4])
            coltd = sb2("coltd", [128, 4])
            DmT1 = sb2("DmT", [128, 128])
            EbM1 = sb2("EbM", [128, 128])
            DmT = [DmT1] * 4
            EbM = [EbM1] * 4
            ebl = sb2("ebl", [128, 12])
            PTm = [sb2("PTm%d" % j, [128, 128], BF16) for j in range(4)]
            qtm = [sb2("qtm%d" % j, [128, 128], BF16) for j in range(4)]
            kwm = [sb2("kwm%d" % j, [128, 128], BF16) for j in range(4)]
            GaT1 = sb2("GaT", [128, 128])
            GaT = [GaT1] * 4
            GaS = sb2("GaS", [128, 128])
            EbD1 = sb2("EbD", [128, 128])
            EbD = [EbD1] * 4
            Abar = sb2("Abar", [128, 4, 128], BF16)
            AbarT = sb2("AbarT", [128, 4, 128], BF16)
            QKm = [sb2("QKm%d" % j, [128, 128], BF16) for j in range(4)]
            qtd = [sb2("qtd%d" % j, [128, 128], BF16) for j in range(4)]
            khat = [sb2("khat%d" % j, [128, 128], BF16) for j in range(4)]
            kwd = [sb2("kwd%d" % j, [128, 128], BF16) for j in range(4)]
            vd = [sb2("vd%d" % j, [128, 128], BF16) for j in range(4)]
            NUl = sb2("NUl", [128, 4, 128], BF16)
            NLl = sb2("NLl", [128, 4, 128], BF16)
            Rm = sb2("Rm", [128, 4, 128], BF16)
            RTm = sb2("RTm", [128, 4, 128], BF16)
            Ysb = sb2("Ysb", [128, 4, 128], BF16)
            Ypsb = sb2("Ypsb", [128, 4, 128], BF16)
            nW0T = [sb2("nW0T%d" % j, [128, 128], BF16) for j in range(4)]
            vnew = sb2("vnew", [128, 128], BF16)
            e4 = sb2("e4", [128, 128])
            spl = sb2("spl", [128, 128])
            E1a = sb2("E1", [128, 128])
            E1 = [E1a] * 4
            E2 = sb2("E2", [128, 128])
            E3 = sb2("E3", [128, 128])
            qtl = [sb2("qtl%d" % j, [128, 128], BF16) for j in range(4)]
            ktlT = [sb2("ktlT%d" % j, [128, 128], BF16) for j in range(4)]
            ktl = [sb2("ktl%d" % j, [128, 128], BF16) for j in range(4)]
            ATl = [sb2("ATl%d" % j, [128, 128], BF16) for j in range(4)]
            NDm = sb2("NDm", [128, 4, 160])
            Ol = sb2("Ol", [128, 4, 128])
            Od = sb2("Od", [128, 4, 128])
            hg = sb2("hg", [128, 4, 128])
            junk = e4
            pre_o = sb2("pre_o", [128, 4, 128], BF16)
            brT1 = sb2("brT0", [128, 3, 512], BF16)
            brT = [brT1, brT1]
            Cn32 = sb2("Cn32", [128, 129])
            Cnb = sb2("Cnb", [128, 192], BF16)
            Sl32 = sb2("Sl32", [128, 128])
            Slb = sb2("Slb", [128, 128], BF16)
            Sd32 = sb2("Sd32", [128, 128])
            Sdb = sb2("Sdb", [128, 128], BF16)
            stmp = sb2("stmp", [128, 128])
            post = sb2("post", [128, 16])

            memset("dve", Cn32[:, :], 0.0)
            memset("dve", Cnb[:, :], 0.0)
            memset("dve", Sl32[:, :], 0.0)
            memset("dve", Slb[:, :], 0.0)
            memset("dve", Sd32[:, :], 0.0)
            memset("dve", Sdb[:, :], 0.0)
            memset("dve", vaug[:, :, :], 1.0)
            memset("dve", llrT[:, :], 1.0)
            for m in range(3):
                memset("dve", XC[m][:, 0:3], 0.0)

            def load_ut(B):
                rr, lb = B // 4, B % 4
                dma("sp", ut[B % 2][:, :, :],
                    u_all[lb].rearrange("(r kt p) t -> p r kt t", r=4, kt=8, p=128)[:, rr, :, :])

            load_ut(0)
            nb_ = min(NBLK, BLK_LIMIT)

            def block_gen(B):
                if B + 1 < nb_:
                    load_ut(B + 1)
                U = ut[B % 2]
                S.section(1)
                for g in range(7):
                    pf = PF[g % 2]
                    for kt in range(8):
                        mm(pf[:, :], WF(kt, g), U[:, kt, :], start=(kt == 0), stop=(kt == 7))
                    if g == 0:
                        act(qTm[:, :], pf[:, :], AF.Copy, scale=QS)
                    elif g == 1:
                        cp("dve", kTm[:, :], pf[:, :])
                    elif g == 2:
                        act(lqT[:, :], pf[:, :], AF.Copy, scale=QS)
                    elif g == 3:
                        cp("dve", lkT[:, :], pf[:, :])
                    else:
                        m = g - 4
                        if m % 2 == 0:
                            cp("act", XC[m][:, 3:515], pf[:, :])
                        else:
                            cp("dve", XC[m][:, 3:515], pf[:, :])
                for kt in range(8):
                    mm(PF[1][0:16, :], WLL(kt), U[:, kt, :], start=(kt == 0), stop=(kt == 7))
                cp("dve", llrT[0:16, :], PF[1][0:16, :])

                S.section(2)
                for m in range(3):
                    cw = lambda j, m=m: pcol(CW + m * 4 + j)
                    tsc("dve", cacc[:, :], XC[m][:, 3:515], cw(3), ALU.mult)
                    for j in (2, 1, 0):
                        stt(cacc[:, :], XC[m][:, j:j + 512], cw(j), cacc[:, :], ALU.mult, ALU.add)
                    cp("pool", XC[m][:, 0:3], XC[m][:, 512:515])
                    if m < 2:
                        act(csil[m][:, :], cacc[:, :], AF.Silu)
                        act(csq[:, :], csil[m][:, :], AF.Square)
                        mm(PF[m][:, :], oneb[:, :], csq[:, :])
                        act(crn1[:, :], PF[m][:, :], AF.Ln, bias=EPS)
                        act(crn2[:, :], crn1[:, :], AF.Exp, scale=-0.5)
                        if m == 0:
                            stt(dT[0][:, :], csil[0][:, :], QS, crn2[:, :], ALU.mult, ALU.mult)
                        else:
                            tt("dve", dT[1][:, :], csil[1][:, :], crn2[:, :], ALU.mult)
                    else:
                        act(dT[2][:, :], cacc[:, :], AF.Silu)

                yield
                S.section(3)
                for j in range(4):
                    tk = slice(j * 128, (j + 1) * 128)
                    for kt in range(8):
                        mm(PT1[:, 0:WH_T1], U[:, kt, tk], WT1(kt), start=(kt == 0), stop=(kt == 7))
                    for kt in range(8):
                        mm(PT2[:, :], U[:, kt, tk], WT2(kt), start=(kt == 0), stop=(kt == 7))
                    S.section(3.1)
                    cp("act", km[:, j, :], PT1[:, 0:128])
                    S.section(3.2)
                    cp("dve", vaug[:, j, 0:128], PT1[:, 128:256])
                    S.section(3.3)
                    act(sigo[:, j, :], PT1[:, 256:384], AF.Sigmoid)
                    S.section(3.4)
                    cp("dve", sm[:, j, :], PT1[:, 384:388])
                    S.section(3.5)
                    cp("act", kl[:, j, :], PT2[:, 0:128])
                    cp("dve", vl[:, j, :], PT2[:, 128:256])
                    S.section(3.6)
                    act(silr[:, j, :], PT2[:, 256:384], AF.Silu)
                    act(silz[:, j, :], PT2[:, 384:512], AF.Silu)
                    S.section(3)

                S.section(4)
                act(gt[:, 4, :], sm[:, :, 1], AF.Exp, bias=drv[:, 0:1], scale=-1.0)
                act(gt[:, 0, :], gt[:, 4, :], AF.Ln, bias=1.0)
                tsc("dve", gt[:, 1, :], sm[:, :, 0], pcol(0), ALU.add)
                act(gt[:, 5, :], sm[:, :, 3], AF.Exp, bias=pcol(3))
                act(gt[:, 6, :], gt[:, 5, :], AF.Ln, bias=1.0)
                tsc("dve", gt[:, 2, :], gt[:, 6, :], drv[:, 1:2], ALU.mult)
                act(gt[:, 7, :], sm[:, :, 2], AF.Exp, scale=-1.0)
                tsc("dve", gt[:, 7, :], gt[:, 7, :], 1.0, ALU.add)
                recip(gt[:, 3, :], gt[:, 7, :])

                def interleave(gens):
                    gens = list(gens)
                    while gens:
                        for g_ in list(gens):
                            try:
                                next(g_)
                            except StopIteration:
                                gens.remove(g_)

                def dec_mlstm():
                    for j in range(4):
                        tk = slice(j * 128, (j + 1) * 128)
                        nlc = gt[:, 0, j:j + 1]
                        mm(PM[:, 0:1], NU, nlc)
                        tsc("dve", LFB[:, :], ONE32, nlc, ALU.mult)
                        yield
                        mm(PM[:, 128:256], LFB[:, :], NU)
                        mm(PM[:, 256:384], LFB[:, :], NU, start=True, stop=False)
                        mm(PM[:, 256:384], ID32, NEGI, start=False, stop=True)
                        mm(PM[:, 384:512], kTm[:, tk], qTm[:, tk])
                        yield
                        tt("dve", coltm[:, 0:1], gt[:, 1, j:j + 1], PM[:, 0:1], ALU.subtract)
                        yield
                        act(DmT[j][:, :], PM[:, 256:384], AF.Exp, bias=coltm[:, 0:1])
                        yield
                        act(EbM[j][:, :], PM[:, 128:256], AF.Exp)
                        yield
                        tt("dve", PTm[j][:, :], PM[:, 384:512], DmT[j][:, :], ALU.mult)
                        yield
                        cp("pool", ebl[:, j:j + 1], EbM[j][:, 127:128])
                        tt("pool", qtm[j][:, :], qTm[:, tk], EbM[j][:, :], ALU.mult)
                        yield
                        tsc("pool", kwm[j][:, :], km[:, j, :], DmT[j][:, 127:128], ALU.mult, 0.0, ALU.add)
                        yield

                def dec_gdn():
                    for j in range(4):
                        tk = slice(j * 128, (j + 1) * 128)
                        gsc = gt[:, 2, j:j + 1]
                        mm(PN[:, 0:1], NU, gsc)
                        tsc("dve", LFBd[:, :], ONE32, gsc, ALU.mult)
                        yield
                        mm(PN[:, 128:256], LFBd[:, :], NU)
                        mm(PN[:, 256:384], LFBd[:, :], NU, start=True, stop=False)
                        mm(PN[:, 256:384], ID32, NEGI, start=False, stop=True)
                        mm(PN[:, 384:512], LFBd[:, :], NU, start=True, stop=False)
                        mm(PN[:, 384:512], ID32, NEGS, start=False, stop=True)
                        mm(PT1[:, 0:128], dT[1][:, tk], dT[1][:, tk])
                        mm(PT1[:, 128:256], dT[1][:, tk], dT[0][:, tk])
                        tr(PB[:, 0:128], dT[1][:, tk], idb[:, :])
                        tr(PB[:, 128:256], dT[2][:, tk], idb[:, :])
                        yield
                        tsc("dve", coltd[:, 1:2], PN[:, 0:1], -1.0, ALU.mult)
                        yield
                        act(coltd[:, 2:3], PN[:, 0:1], AF.Exp)
                        yield
                        act(GaT[j][:, :], PN[:, 256:384], AF.Exp, bias=coltd[:, 1:2])
                        yield
                        act(GaS[:, :], PN[:, 384:512], AF.Exp, bias=coltd[:, 1:2])
                        yield
                        act(EbD[j][:, :], PN[:, 128:256], AF.Exp)
                        yield
                        cp("act", vd[j][:, :], PB[:, 128:256])
                        yield
                        stt(Abar[:, j, :], PT1[:, 0:128], gt[:, 3, j:j + 1], GaS[:, :], ALU.mult, ALU.mult)
                        yield
                        tr(PB[:, 256:384], Abar[:, j, :], idb[:, :])
                        tt("dve", QKm[j][:, :], PT1[:, 128:256], GaT[j][:, :], ALU.mult)
                        yield
                        cp("pool", ebl[:, 4 + j:5 + j], EbD[j][:, 127:128])
                        tt("pool", qtd[j][:, :], dT[0][:, tk], EbD[j][:, :], ALU.mult)
                        yield
                        tsc("dve", khat[j][:, :], PB[:, 0:128], coltd[:, 2:3], ALU.mult)
                        yield
                        tsc("dve", kwd[j][:, :], PB[:, 0:128], GaT[j][:, 127:128], ALU.mult)
                        yield
                        cp("act", AbarT[:, j, :], PB[:, 256:384])
                        yield

                def dec_gla():
                    for j in range(4):
                        tk = slice(j * 128, (j + 1) * 128)
                        mm(PO[:, 0:128], llrT[0:17, tk], wlr[0:17, :])
                        yield
                        act(e4[:, :], PO[:, 0:128], AF.Exp, scale=-1.0)
                        yield
                        act(spl[:, :], e4[:, :], AF.Ln, bias=1.0)
                        yield
                        mm(PO[:, 128:256], NU, spl[:, :])
                        mm(PO[:, 256:384], spl[:, :], NU)
                        yield
                        act(E1[j][:, :], PO[:, 256:384], AF.Exp, scale=1.0 / 16)
                        yield
                        act(E2[:, :], PO[:, 256:384], AF.Exp, scale=-1.0 / 16)
                        yield
                        act(E3[:, :], PO[:, 128:256], AF.Exp, scale=-1.0 / 16)
                        yield
                        cp("pool", ebl[:, 8 + j:9 + j], E1[j][:, 127:128])
                        tt("pool", qtl[j][:, :], lqT[:, tk], E1[j][:, :], ALU.mult)
                        yield
                        tt("pool", ktlT[j][:, :], lkT[:, tk], E2[:, :], ALU.mult)
                        yield
                        tt("dve", ktl[j][:, :], kl[:, j, :], E3[:, :], ALU.mult)
                        yield
                        mm(PO[:, 384:512], ktlT[j][:, :], qtl[j][:, :])
                        yield
                        tt("dve", ATl[j][:, :], PO[:, 384:512], UM, ALU.mult)
                        yield


                def lvmask(i):
                    tt("pool", NUl[:, :, :], Abar[:, :, :], lmu[:, i:i + 1, :].to_broadcast([128, 4, 128]), ALU.mult)
                    tt("pool", NLl[:, :, :], AbarT[:, :, :], lml[:, i:i + 1, :].to_broadcast([128, 4, 128]), ALU.mult)

                def q4(bank):
                    return bank[:, :].rearrange("p (a b) -> p a b", a=4)

                def inv_gen():
                    lvmask(0)
                    yield
                    idb4 = idb[:, :].unsqueeze(1).to_broadcast([128, 4, 128])
                    tt("dve", Rm[:, :, :], idb4, NUl[:, :, :], ALU.subtract)
                    yield
                    tt("dve", RTm[:, :, :], idb4, NLl[:, :, :], ALU.subtract)
                    yield
                    for i in range(1, 7):
                        lvmask(i)
                        yield
                        last = (i == 6)
                        for j in range(4):
                            mm(PN[:, j * 128:(j + 1) * 128], NLl[:, j, :], Rm[:, j, :])
                        yield
                        cp("act", Ysb[:, :, :], q4(PN))
                        yield
                        if not last:
                            for j in range(4):
                                mm(PT1[:, j * 128:(j + 1) * 128], NUl[:, j, :], RTm[:, j, :])
                            yield
                            cp("dve", Ypsb[:, :, :], q4(PT1))
                            yield
                        for j in range(4):
                            mm(PT2[:, j * 128:(j + 1) * 128], RTm[:, j, :], Ysb[:, j, :])
                        yield
                        if not last:
                            for j in range(4):
                                mm(PF[0][:, j * 128:(j + 1) * 128], Rm[:, j, :], Ypsb[:, j, :])
                            yield
                        tt("dve", Rm[:, :, :], Rm[:, :, :], q4(PT2), ALU.subtract)
                        yield
                        if not last:
                            tt("dve", RTm[:, :, :], RTm[:, :, :], q4(PF[0]), ALU.subtract)
                            yield
                    for j in range(4):
                        mm(PF[1][:, j * 128:(j + 1) * 128], khat[j][:, :], Rm[:, j, :])
                    yield
                    for j in range(4):
                        tsc("dve", nW0T[j][:, :], PF[1][:, j * 128:(j + 1) * 128], -1.0, ALU.mult)
                    yield

                S.section(5)
                interleave([dec_gdn(), dec_mlstm()])
                S.section(8)
                interleave([inv_gen(), dec_gla()])


                S.section(9)
                def rec_mlstm():
                    for j in range(4):
                        mm(PO[:, 0:129], PTm[j][:, :], vaug[:, j, 0:129], start=True, stop=False)
                        mm(PO[:, 0:129], qtm[j][:, :], Cnb[:, 0:129], start=False, stop=True)
                        mm(PM[:, 0:129], kwm[j][:, :], vaug[:, j, 0:129])
                        yield
                        stt(Cn32[:, :], Cn32[:, :], ebl[:, j:j + 1], PM[:, 0:129], ALU.mult, ALU.add)
                        yield
                        cp("pool", Cnb[:, 0:129], Cn32[:, :])
                        yield
                        cp("act", NDm[:, j, 0:129], PO[:, 0:129])
                        yield

                def rec_gla():
                    for j in range(4):
                        mm(PF[0][:, 0:128], ATl[j][:, :], vl[:, j, :], start=True, stop=False)
                        mm(PF[0][:, 0:128], qtl[j][:, :], Slb[:, :], start=False, stop=True)
                        mm(PF[1][:, 0:128], ktl[j][:, :], vl[:, j, :])
                        yield
                        act(stmp[:, :], PF[1][:, 0:128], AF.Identity, scale=ebl[:, 8 + j:9 + j])
                        yield
                        stt(Sl32[:, :], Sl32[:, :], ebl[:, 8 + j:9 + j], stmp[:, :], ALU.mult, ALU.add)
                        yield
                        cp("pool", Slb[:, :], Sl32[:, :])
                        yield
                        cp("act", Ol[:, j, :], PF[0][:, 0:128])
                        yield

                def rec_gdn():
                    for j in range(4):
                        mm(PN[:, 0:128], Rm[:, j, :], vd[j][:, :], start=True, stop=False)
                        mm(PN[:, 0:128], nW0T[j][:, :], Sdb[:, :], start=False, stop=True)
                        yield
                        tsc("dve", vnew[:, :], PN[:, 0:128], gt[:, 3, j:j + 1], ALU.mult)
                        yield
                        mm(PT1[:, 0:128], QKm[j][:, :], vnew[:, :], start=True, stop=False)
                        mm(PT1[:, 0:128], qtd[j][:, :], Sdb[:, :], start=False, stop=True)
                        mm(PN[:, 128:256], kwd[j][:, :], vnew[:, :])
                        yield
                        stt(Sd32[:, :], Sd32[:, :], ebl[:, 4 + j:5 + j], PN[:, 128:256], ALU.mult, ALU.add)
                        yield
                        cp("pool", Sdb[:, :], Sd32[:, :])
                        yield
                        cp("act", Od[:, j, :], PT1[:, 0:128])
                        yield

                interleave([rec_gdn(), rec_mlstm(), rec_gla()])


                yield
                S.section(10)
                bt = brT[B % 2]
                act(post[:, 0:4], NDm[:, :, 128], AF.Abs)
                tsc("dve", post[:, 0:4], post[:, 0:4], 1.0, ALU.max)
                recip(post[:, 4:8], post[:, 0:4])
                for j in range(4):
                    stt(hg[:, j, :], NDm[:, j, 0:128], post[:, 4 + j:5 + j], sigo[:, j, :], ALU.mult, ALU.mult)
                srcs = [(hg, None, G_HM), (Ol, silr, G_HL), (Od, silz, G_HD)]
                for n, (src, gate, gi) in enumerate(srcs):
                    for j in range(4):
                        S.op("act", lambda e, src=src, j=j: e.activation(
                            out=junk[:, :], in_=src[:, j, :], func=AF.Square, accum_out=post[:, 8 + j:9 + j]),
                            reads=[src], writes=[junk, post])
                    act(post[:, 12:16], post[:, 8:12], AF.Ln, bias=EPS, scale=1.0 / HD)
                    act(post[:, 12:16], post[:, 12:16], AF.Exp, scale=-0.5)
                    for j in range(4):
                        if gate is None:
                            tsc("dve", pre_o[:, j, :], src[:, j, :], post[:, 12 + j:13 + j], ALU.mult)
                        else:
                            stt(pre_o[:, j, :], src[:, j, :], post[:, 12 + j:13 + j], gate[:, j, :], ALU.mult, ALU.mult)
                        tr(PB[:, 512 + j * 128:512 + (j + 1) * 128], pre_o[:, j, :], idb[:, :])
                    act(bt[:, n, :], PB[:, 512:1024], AF.Identity, scale=pcol(gi))
                kk = B // 2
                dma("sp", br_loc[kk].rearrange("(n p) t -> p n t", p=128)[:, :, (B % 2) * 512:(B % 2 + 1) * 512],
                    bt[:, :, :])
                if B % 2 == 1:
                    S.cc(lambda e, kk=kk: e.collective_compute("AllGather", ALU.bypass, replica_groups=groups,
                                                               ins=[br_loc[kk]], outs=[br_all[kk]]),
                         reads=[br_loc[kk]], writes=[br_all[kk]])
                    if dbg and l == 0:
                        dma("sp", dbg_br[:, kk * 1024:(kk + 1) * 1024], br_loc[kk])
            bgens = [block_gen(B) for B in range(nb_)]
            next(bgens[0])
            for B in range(nb_):
                next(bgens[B])
                if B + 1 < nb_:
                    next(bgens[B + 1])
                for _ in bgens[B]:
                    pass
            S.section(0)
            S.barrier()
        if stop == "p2":
            break

        with ExitStack() as p3:
            def sb3(name, shape, dt=F32):
                return p3.enter_context(nc.sbuf_tensor("%s_l%d" % (name, l), list(shape), dt))
            HT = TOK // 2
            uT = sb3("uT3", [128, 8, HT], BF16)
            brs = sb3("brs", [128, 12, HT], BF16)
            mixT = sb3("mixT", [128, 8, HT], BF16)
            brq = [sb3("brq0", [128, 12, 256], BF16), sb3("brq1", [128, 12, 256], BF16)]
            wgu = [sb3("wgu0", [128, 36 * 128], BF16), sb3("wgu1", [128, 36 * 128], BF16)]
            wo = [sb3("wo0", [128, 1024], BF16), sb3("wo1", [128, 1024], BF16)]
            sgt = [sb3("sgt0", [128, 512]), sb3("sgt1", [128, 512])]
            macc = sb3("macc", [128, 512])
            it = 0
            for hf in range(2):
                t0 = hf * HT
                for k2 in range(2):
                    dma("sp", uT[:, :, k2 * 512:(k2 + 1) * 512],
                        u_loc[hf * 2 + k2].rearrange("(kt p) t -> p kt t", p=128))
                for tb in range(HT // 256):
                    for q in range(4):
                        bq = brq[it % 2]
                        it += 1
                        dma("sp", bq[:, :, :],
                            br_all[2 * q + hf].rearrange("(hn p) t -> p hn t", p=128)[:, :, tb * 256:(tb + 1) * 256])
                        dst = brs[:, :, tb * 256:(tb + 1) * 256]
                        if q == 0:
                            tsc("dve", dst, bq[:, :, :], msel[:, 0:1], ALU.mult)
                        else:
                            stt(dst, bq[:, :, :], msel[:, q:q + 1], dst, ALU.mult, ALU.add)
                ldw(wgu[0][:, :], wgu_d[l, 0])
                for d in range(8):
                    if d + 1 < 8:
                        ldw(wgu[(d + 1) % 2][:, :], wgu_d[l, d + 1])
                    W = wgu[d % 2]
                    for tb in range(HT // 512):
                        tsl = slice(tb * 512, (tb + 1) * 512)
                        for n in range(3):
                            pg = PF[n % 2]
                            pu = PM if n % 2 == 0 else PN
                            for kt in range(8):
                                c0 = (n * 12 + kt) * 128
                                mm(pg[:, :], W[:, c0:c0 + 128], uT[:, kt, tsl], start=(kt == 0), stop=(kt == 7))
                            for h in range(4):
                                c0 = (n * 12 + 8 + h) * 128
                                mm(pu[:, :], W[:, c0:c0 + 128], brs[:, h * 3 + n, tsl], start=(h == 0), stop=(h == 3))
                            sg = sgt[n % 2]
                            act(sg[:, :], pg[:, :], AF.Sigmoid)
                            if n == 0:
                                tt("dve", macc[:, :], sg[:, :], pu[:, :], ALU.mult)
                            elif n == 1:
                                tt("dve", sg[:, :], sg[:, :], pu[:, :], ALU.mult)
                                tt("pool", macc[:, :], macc[:, :], sg[:, :], ALU.add)
                            else:
                                tt("dve", sg[:, :], sg[:, :], pu[:, :], ALU.mult)
                                tt("dve", mixT[:, d, tsl], macc[:, :], sg[:, :], ALU.add)
                ldw(wo[0][:, :], wo_d[l, 0])
                for d in range(8):
                    if d + 1 < 8:
                        ldw(wo[(d + 1) % 2][:, :], wo_d[l, d + 1])
                    W = wo[d % 2]
                    for tb in range(HT // 512):
                        tsl = slice(tb * 512, (tb + 1) * 512)
                        xsl = slice(t0 + tb * 512, t0 + (tb + 1) * 512)
                        pq = PT1 if tb % 2 == 0 else PT2
                        for kt in range(8):
                            mm(pq[:, :], W[:, kt * 128:(kt + 1) * 128], mixT[:, kt, tsl], start=(kt == 0), stop=(kt == 7))
                        tt("dve", xT[:, d, xsl], xT[:, d, xsl], pq[:, :], ALU.add)
            S.barrier()

        with ExitStack() as p4:
            def sb4(name, shape, dt=F32):
                return p4.enter_context(nc.sbuf_tensor("%s_l%d" % (name, l), list(shape), dt))
            u2 = sb4("u2T", [128, 8, TOK], BF16)
            sq = sb4("p4sq", [128, 8, 512], BF16)
            rs1 = sb4("p4rs1", [128, 512])
            rs2 = sb4("p4rs2", [128, 512])
            hT = sb4("hT", [128, 8, TOK], BF16)
            rl = [sb4("rl0", [128, 512], BF16), sb4("rl1", [128, 512], BF16)]
            w1 = [sb4("w1_%d" % i, [128, 1024], BF16) for i in range(3)]
            w2 = [sb4("w2_%d" % i, [128, 1024], BF16) for i in range(3)]
            for blk in range(4):
                rmsnorm_block(blk, G_MLP, prm, u2, sq, rs1, rs2, out_sl=slice(blk * 512, (blk + 1) * 512))
            cnt = 0
            for c in range(4):
                ldw(w1[0][:, :], w1_d[l, c * 8])
                for f in range(8):
                    if f + 1 < 8:
                        ldw(w1[(f + 1) % 3][:, :], w1_d[l, c * 8 + f + 1])
                    W = w1[f % 3]
                    for tb in range(4):
                        tsl = slice(tb * 512, (tb + 1) * 512)
                        pq = PF[cnt % 2]
                        r_ = rl[cnt % 2]
                        cnt += 1
                        for kt in range(8):
                            mm(pq[:, :], W[:, kt * 128:(kt + 1) * 128], u2[:, kt, tsl], start=(kt == 0), stop=(kt == 7))
                        act(r_[:, :], pq[:, :], AF.Relu)
                        tt("pool", hT[:, f, tsl], r_[:, :], r_[:, :], ALU.mult)
                ldw(w2[0][:, :], w2_d[l, c * 8])
                for d in range(8):
                    if d + 1 < 8:
                        ldw(w2[(d + 1) % 3][:, :], w2_d[l, c * 8 + d + 1])
                    W = w2[d % 3]
                    for tb in range(4):
                        tsl = slice(tb * 512, (tb + 1) * 512)
                        pq = PM if (tb % 2 == 0) else PN
                        for ft in range(8):
                            mm(pq[:, :], W[:, ft * 128:(ft + 1) * 128], hT[:, ft, tsl], start=(ft == 0), stop=(ft == 7))
                        tt("dve", xT[:, d, tsl], xT[:, d, tsl], pq[:, :], ALU.add)
            S.barrier()

    with ExitStack() as p5:
        def sb5(name, shape, dt=F32):
            return p5.enter_context(nc.sbuf_tensor(name, list(shape), dt))
        outv = out_d.rearrange("(kt p) t -> p kt t", p=128)
        if do_final:
            sq = sb5("p5sq", [128, 8, 512], BF16)
            rs1 = sb5("p5rs1", [128, 512])
            rs2 = sb5("p5rs2", [128, 512])
            ob = [sb5("p5o0", [128, 8, 512]), sb5("p5o1", [128, 8, 512])]
            for blk in range(4):
                rmsnorm_block(blk, 0, gfin, ob[blk % 2], sq, rs1, rs2)
                dma("sp", outv[:, :, blk * 512:(blk + 1) * 512], ob[blk % 2][:, :, :])
        else:
            dma("sp", outv, xT[:, :, :])
        fin = [out_d] + ([dbg_br] if dbg else [])
        S.final_wait("sp", fin)
        S.emit()
    es.close()
    return nc


def _tile_kxm(w, mt):
    K, M = w.shape
    a = w.reshape(K // 128, 128, M // mt, mt)
    return np.ascontiguousarray(a.transpose(2, 1, 0, 3))


def prep_shared(inp, layers):
    wgu, wo, w1, w2 = [], [], [], []
    for l in layers:
        w_in = inp["w_in"][l]
        g = _tile_kxm(np.ascontiguousarray(w_in[:, O_G:O_G + 3072]), 128)
        g = g.reshape(3, 8, 128, 8, 128)
        up = np.stack([_tile_kxm(inp["w_up"][l][n], 128) for n in range(3)])
        blk = np.concatenate([g, up], axis=3)
        blk = np.ascontiguousarray(blk.transpose(1, 2, 0, 3, 4)).reshape(8, 128, 36 * 128)
        wgu.append(blk)
        wo.append(_tile_kxm(inp["w_out"][l], 128).reshape(8, 128, 1024))
        w1.append(_tile_kxm(inp["w_mlp_in"][l], 128).reshape(32, 128, 1024))
        m2 = inp["w_mlp_out"][l].reshape(4, 8, 128, 8, 128)
        w2.append(np.ascontiguousarray(m2.transpose(0, 3, 2, 1, 4)).reshape(32, 128, 1024))
    return {"wgu": np.stack(wgu), "wo": np.stack(wo), "w1": np.stack(w1), "w2": np.stack(w2)}


def prep_core(inp, layers, c):
    h = c % 4
    hs = slice(h * 128, (h + 1) * 128)
    wh, wlr, prm = [], [], []
    for l in layers:
        w = inp["w_in"][l]
        def col(o):
            return w[:, o + h * 128:o + (h + 1) * 128]
        def one(o):
            return w[:, o + h:o + h + 1]
        cat = np.concatenate([col(O_MQ), col(O_MK), col(O_LQ), col(O_LK), col(O_DQ), col(O_DK), col(O_DV),
                              col(O_MK), col(O_MV), col(O_MO), one(O_MI), one(O_MF), one(O_DB), one(O_DA),
                              col(O_LK), col(O_LV), col(O_LR), col(O_DZ),
                              w[:, O_LLR:O_LLR + 16]], axis=1)
        a = cat.reshape(8, 128, WH).transpose(1, 0, 2)
        wh.append(np.ascontiguousarray(a).reshape(128, 8 * WH))
        wlr.append(np.concatenate([inp["w_gla_lr"][l][:, hs], inp["b_gla"][l][None, hs]], axis=0))
        p = np.zeros((128, NPRM), np.float32)
        p[:, 0] = inp["b_if"][l][h]
        p[:, 1] = inp["b_if"][l][4 + h]
        p[:, 2] = inp["a_log"][l][h]
        p[:, 3] = inp["dt_bias"][l][h]
        p[:, 4:12] = inp["g_norm_mix"][l].reshape(8, 128).T
        p[:, 12:20] = inp["g_norm_mlp"][l].reshape(8, 128).T
        p[:, 20] = inp["g_head_mlstm"][l][hs]
        p[:, 21] = inp["g_head_gla"][l][hs]
        p[:, 22] = inp["g_head_gdn"][l][hs]
        for m in range(3):
            for j in range(4):
                p[:, 23 + m * 4 + j] = inp["conv_gdn"][l][j, m * 512 + h * 128:m * 512 + (h + 1) * 128]
        prm.append(p)
    ms = np.zeros((128, 4), np.float32)
    ms[:, h] = 1.0
    return {"wh": np.stack(wh), "wlr": np.stack(wlr).astype(np.float32), "prm": np.stack(prm), "msel": ms,
            "gfin": np.ascontiguousarray(inp["g_final"].reshape(8, 128).T)}


_PROG = {}


STOP = None
SEC_LIMIT = 1000
BLK_LIMIT = 1000


def _get_prog(nl, do_final, dbg=False):
    k = (nl, do_final, dbg, STOP)
    if k not in _PROG:
        _PROG[k] = build_program(nl, do_final, dbg, STOP)
    return _PROG[k]


def _run(inp, x_cores, layers, do_final, dbg=False):
    nc = _get_prog(len(layers), do_final, dbg)
    shared = prep_shared(inp, layers)
    cst = make_consts()
    in_maps = []
    for c in range(NCORES):
        m = {"xT": x_cores[c], "cst": cst}
        m.update(shared)
        m.update(prep_core(inp, layers, c))
        in_maps.append(m)
    res = run_bass_kernel_spmd(nc, in_maps, core_ids=list(range(NCORES)))
    return res


def kernel(**inputs):
    inp = {k: np.asarray(v) for k, v in inputs.items()}
    x = inp["x"]
    x_cores = []
    for c in range(NCORES):
        b, r = c // 4, c % 4
        x_cores.append(np.ascontiguousarray(x[b, r * TOK:(r + 1) * TOK, :].T))
    if FUSED:
        res = _run(inp, x_cores, [0, 1, 2, 3], True)
        outs = [res.results[c]["outT"] for c in range(NCORES)]
    else:
        for l in range(4):
            res = _run(inp, x_cores, [l], l == 3)
            x_cores = [np.ascontiguousarray(res.results[c]["outT"]) for c in range(NCORES)]
        outs = x_cores
    out = np.empty_like(x)
    for c in range(NCORES):
        b, r = c // 4, c % 4
        out[b, r * TOK:(r + 1) * TOK, :] = outs[c].T
    return out
```

```python
import numpy as np
from contextlib import ExitStack
import concourse.bass as bass
import concourse.mybir as mybir
from concourse.bass_utils import run_bass_kernel_spmd

F32 = mybir.dt.float32
BF16 = mybir.dt.bfloat16
AF = mybir.ActivationFunctionType
ALU = mybir.AluOpType

NCORES = 8
D = 1024
SEQ = 8192
TOK = 2048
NBLK = SEQ // 512
EPS = 1e-6
HD = 128
QS = HD ** -0.5
NPRM = 36
NEGV = -30000.0
FUSED = True

O_MQ, O_MK, O_MV, O_MO, O_MI, O_MF = 0, 512, 1024, 1536, 2048, 2052
O_LQ, O_LK, O_LV, O_LR, O_LLR = 2056, 2568, 3080, 3592, 4104
O_DQ, O_DK, O_DV, O_DZ, O_DB, O_DA, O_G = 4120, 4632, 5144, 5656, 6168, 6172, 6176
WH_F, WH_T1, WH_T2, WH_L = 896, 388, 512, 16
WH = WH_F + WH_T1 + WH_T2 + WH_L

CST = {}
_off = 0
for _n, _w in [("NU", 128), ("UM", 128), ("NEGI", 128), ("NEGS", 128), ("ID", 128), ("ONE", 128),
               ("LMU", 7 * 128), ("LML", 7 * 128)]:
    CST[_n] = (_off, _w)
    _off += _w
NCST = _off


def make_consts():
    c = np.zeros((128, NCST), np.float32)
    s = np.arange(128)[:, None]
    t = np.arange(128)[None, :]
    def put(n, a):
        o, w = CST[n]
        c[:, o:o + w] = a.reshape(128, w)
    put("NU", -(s <= t).astype(np.float32))
    put("UM", (s <= t).astype(np.float32))
    put("NEGI", np.where(s <= t, 0.0, NEGV).astype(np.float32))
    put("NEGS", np.where(s < t, 0.0, NEGV).astype(np.float32))
    put("ID", np.eye(128, dtype=np.float32))
    put("ONE", np.ones((128, 128), np.float32))
    lmu = np.zeros((128, 7, 128), np.float32)
    for i in range(7):
        b = 1 << i
        m = ((s // (2 * b)) == (t // (2 * b))) & ((s % (2 * b)) < b) & ((t % (2 * b)) >= b)
        lmu[:, i, :] = m
    put("LMU", lmu)
    put("LML", np.ascontiguousarray(lmu.transpose(2, 1, 0)))
    return c


class Sched:
    CE = ("pe", "act", "dve", "pool")

    def __init__(self, nc, es, ndma=8):
        self.nc = nc
        self.prog = {e: [] for e in ("pe", "act", "dve", "pool", "sp")}
        self.esem = {e: es.enter_context(nc.semaphore("s_" + e)) for e in self.CE}
        self.ecnt = {e: 0 for e in self.CE}
        self.dsem = {q: [es.enter_context(nc.semaphore("d_%s%d" % (q, i))) for i in range(ndma)]
                     for q in ("sp", "pool")}
        self.dcnt = {q: [0] * ndma for q in ("sp", "pool")}
        self.drr = {"sp": 0, "pool": 0}
        self.ccsem = es.enter_context(nc.semaphore("s_cc"))
        self.cccnt = 0
        self.seen = {e: {} for e in self.prog}
        self.lastw = {}
        self.readers = {}
        self.nops = 0
        self.enabled = True

    @staticmethod
    def key(x):
        if isinstance(x, (str, tuple)):
            return x
        t = getattr(x, "tensor", None)
        return t.name if t is not None else x.name

    PSUM_NAMES = ("PF0", "PF1", "PT1", "PT2", "PM", "PN", "PO", "PB")

    def _deps(self, reads, writes, me=None):
        deps = []
        for r in reads:
            t = self.lastw.get(r)
            if t is not None:
                deps.append(t)
            if r in self.PSUM_NAMES:
                for k, tk in self.readers.get(r, {}).items():
                    if k != me:
                        deps.append(tk)
        for w in writes:
            t = self.lastw.get(w)
            if t is not None:
                deps.append(t)
            deps.extend(self.readers.get(w, {}).values())
        return deps

    def _commit(self, tok, reads, writes):
        for r in reads:
            d = self.readers.setdefault(r, {})
            k = tok[0]
            if k not in d or d[k][2] < tok[2]:
                d[k] = tok
        for w in writes:
            self.lastw[w] = tok
            self.readers[w] = {}

    def _add(self, eng, deps, fn, tok, inc):
        waits = {}
        for (k, sem, val, peng) in deps:
            if eng == "pe" and peng == "pe":
                continue
            if self.seen[eng].get(k, 0) >= val:
                continue
            if k not in waits or waits[k][1] < val:
                waits[k] = (sem, val)
        for k, (sem, val) in waits.items():
            self.seen[eng][k] = val
        self.prog[eng].append((list(waits.values()), fn, tok, inc))
        self.nops += 1

    def section(self, k):
        self.enabled = (k <= SEC_LIMIT)

    def op(self, eng, fn, reads=(), writes=()):
        if not self.enabled:
            return
        reads = [self.key(r) for r in reads]
        writes = [self.key(w) for w in writes]
        deps = self._deps(reads, writes, "e_" + eng)
        self.ecnt[eng] += 1
        tok = ("e_" + eng, self.esem[eng], self.ecnt[eng], eng)
        self._add(eng, deps, fn, tok, 1)
        self._commit(tok, reads, writes)

    def dma(self, q, fn, reads=(), writes=()):
        if not self.enabled:
            return
        reads = [self.key(r) for r in reads]
        writes = [self.key(w) for w in writes]
        deps = self._deps(reads, writes)
        i = self.drr[q]
        self.drr[q] = (i + 1) % len(self.dsem[q])
        k = "d_%s%d" % (q, i)
        sem = self.dsem[q][i]
        if self.dcnt[q][i] > 0:
            deps.append((k, sem, self.dcnt[q][i], "dma"))
        self.dcnt[q][i] += 16
        tok = (k, sem, self.dcnt[q][i], "dma")
        self._add(q, deps, fn, tok, 16)
        self._commit(tok, reads, writes)

    def cc(self, fn, reads=(), writes=()):
        if not self.enabled:
            return
        reads = [self.key(r) for r in reads]
        writes = [self.key(w) for w in writes]
        deps = self._deps(reads, writes)
        if self.cccnt > 0:
            deps.append(("cc", self.ccsem, self.cccnt, "cc"))
        self.cccnt += 1
        tok = ("cc", self.ccsem, self.cccnt, "cc")
        self._add("pool", deps, fn, tok, 1)
        self._commit(tok, reads, writes)

    def barrier(self):
        deps = []
        for e in self.CE:
            if self.ecnt[e] > 0:
                deps.append(("e_" + e, self.esem[e], self.ecnt[e], e + "_b"))
        for q in ("sp", "pool"):
            for i, sem in enumerate(self.dsem[q]):
                if self.dcnt[q][i] > 0:
                    deps.append(("d_%s%d" % (q, i), sem, self.dcnt[q][i], "dma"))
        if self.cccnt > 0:
            deps.append(("cc", self.ccsem, self.cccnt, "cc"))
        for e in self.prog:
            self._add(e, list(deps), None, None, 0)

    def final_wait(self, eng, keys):
        deps = []
        for k in keys:
            t = self.lastw.get(self.key(k))
            if t is not None:
                deps.append(t)
        self._add(eng, deps, None, None, 0)

    def emit(self):
        nc = self.nc
        with nc.Block() as block:
            def run(name, e):
                for waits, fn, tok, inc in self.prog[name]:
                    for sem, val in waits:
                        e.wait_ge(sem, val)
                    if fn is None:
                        continue
                    ins = fn(e)
                    ins.then_inc(tok[1], inc)

            @block.tensor
            def _(e):
                run("pe", e)

            @block.scalar
            def _(e):
                run("act", e)

            @block.vector
            def _(e):
                run("dve", e)

            @block.gpsimd
            def _(e):
                run("pool", e)

            @block.sync
            def _(e):
                run("sp", e)


def build_program(nl, do_final, dbg=False, stop=None):
    nc = bass.Bass("TRN2", target_bir_lowering=False)
    es = ExitStack()

    def din(name, shape, dt=F32):
        return nc.dram_tensor(name, list(shape), dt, kind="ExternalInput").ap()

    xT_d = din("xT", [D, TOK])
    wh_d = din("wh", [nl, 128, 8 * WH])
    wlr_d = din("wlr", [nl, 17, 128])
    prm_d = din("prm", [nl, 128, NPRM])
    gfin_d = din("gfin", [128, 8])
    msel_d = din("msel", [128, 4])
    cst_d = din("cst", [128, NCST])
    wgu_d = din("wgu", [nl, 8, 128, 36 * 128])
    wo_d = din("wo", [nl, 8, 128, 1024])
    w1_d = din("w1", [nl, 32, 128, 1024])
    w2_d = din("w2", [nl, 32, 128, 1024])
    out_d = nc.dram_tensor("outT", [D, TOK], F32, kind="ExternalOutput").ap()
    u_loc = [nc.dram_tensor("u_loc%d" % k, [D, 512], BF16, kind="Internal").ap() for k in range(4)]
    u_all = [nc.dram_tensor("u_all%d" % k, [4 * D, 512], BF16, kind="Internal").ap() for k in range(4)]
    br_loc = [nc.dram_tensor("br_loc%d" % k, [384, 1024], BF16, kind="Internal").ap() for k in range(8)]
    br_all = [nc.dram_tensor("br_all%d" % k, [4 * 384, 1024], BF16, kind="Internal").ap() for k in range(8)]
    if dbg:
        dbg_br = nc.dram_tensor("dbg_br", [384, SEQ], BF16, kind="ExternalOutput").ap()
    groups = [[0, 1, 2, 3], [4, 5, 6, 7]]

    S = Sched(nc, es)

    def sb(name, shape, dt=F32):
        return es.enter_context(nc.sbuf_tensor(name, list(shape), dt))

    def ps(name, shape, dt=F32):
        return es.enter_context(nc.psum_tensor(name, list(shape), dt))

    def rw(reads, writes):
        return [r for r in reads if r is not None and not isinstance(r, (int, float))], writes

    def mm(out, lhsT, rhs, start=True, stop=True):
        S.op("pe", lambda e: e.matmul(out, lhsT=lhsT, rhs=rhs, start=start, stop=stop),
             reads=[lhsT, rhs], writes=[out])

    def tr(out, in_, ident):
        S.op("pe", lambda e: e.transpose(out, in_, ident), reads=[in_, ident], writes=[out])

    def act(out, in_, func, bias=None, scale=None, eng="act"):
        kw = {}
        r = [in_]
        if bias is not None:
            kw["bias"] = bias
            if not isinstance(bias, (int, float)):
                r.append(bias)
        if scale is not None:
            kw["scale"] = scale
            if not isinstance(scale, (int, float)):
                r.append(scale)
        S.op("act", lambda e: e.activation(out=out, in_=in_, func=func, **kw), reads=r, writes=[out])

    def tt(eng, out, in0, in1, op):
        S.op(eng, lambda e: e.tensor_tensor(out=out, in0=in0, in1=in1, op=op), reads=[in0, in1], writes=[out])

    def tsc(eng, out, in0, s1, op0, s2=None, op1=None):
        r = [in0] + [s for s in (s1, s2) if s is not None and not isinstance(s, (int, float))]
        if op1 is None:
            S.op(eng, lambda e: e.tensor_scalar(out=out, in0=in0, scalar1=s1, scalar2=None, op0=op0),
                 reads=r, writes=[out])
        else:
            S.op(eng, lambda e: e.tensor_scalar(out=out, in0=in0, scalar1=s1, scalar2=s2, op0=op0, op1=op1),
                 reads=r, writes=[out])

    def stt(out, in0, scalar, in1, op0, op1):
        r = [in0, in1] + ([scalar] if not isinstance(scalar, (int, float)) else [])
        S.op("dve", lambda e: e.scalar_tensor_tensor(out=out, in0=in0, scalar=scalar, in1=in1, op0=op0, op1=op1),
             reads=r, writes=[out])

    def cp(eng, out, in_):
        if eng == "act":
            S.op("act", lambda e: e.activation(out=out, in_=in_, func=AF.Copy), reads=[in_], writes=[out])
        else:
            S.op(eng, lambda e: e.tensor_copy(out=out, in_=in_), reads=[in_], writes=[out])

    def recip(out, in_):
        S.op("dve", lambda e: e.reciprocal(out=out, in_=in_), reads=[in_], writes=[out])

    def memset(eng, ap, v):
        S.op(eng, lambda e: e.memset(ap, v), reads=[], writes=[ap])

    def dma(q, out, in_, **kw):
        S.dma(q, lambda e: e.dma_start(out=out, in_=in_, **kw), reads=[in_], writes=[out])

    def ldw(out, in_):
        S.dma("pool", lambda e: e.dma_start(out=out, in_=in_, max_dma_last_dim=8192), reads=[in_], writes=[out])

    xT = sb("xT_s", [128, 8, TOK])
    cst = sb("cst_s", [128, 6 * 128])
    idb = sb("idb", [128, 128], BF16)
    oneb = sb("oneb", [128, 128], BF16)
    lmu = sb("lmu", [128, 7, 128], BF16)
    lml = sb("lml", [128, 7, 128], BF16)
    prm = sb("prm_s", [128, NPRM])
    drv = sb("drv_s", [128, 4])
    gfin = sb("gfin_s", [128, 8])
    msel = sb("msel_s", [128, 4])

    def C(n):
        o, w = CST[n]
        return cst[:, o:o + w]

    NU, UM, NEGI, NEGS, ID32, ONE32 = C("NU"), C("UM"), C("NEGI"), C("NEGS"), C("ID"), C("ONE")

    PF = [ps("PF0", [128, 512]), ps("PF1", [128, 512])]
    PT1 = ps("PT1", [128, 512])
    PT2 = ps("PT2", [128, 512])
    PM = ps("PM", [128, 512])
    PN = ps("PN", [128, 512])
    PO = ps("PO", [128, 512])
    PB = ps("PB", [128, 1024], BF16)

    dma("sp", cst[:, :], cst_d[:, 0:6 * 128])
    dma("sp", gfin[:, :], gfin_d)
    dma("sp", msel[:, :], msel_d)
    dma("sp", xT[:, :, :], xT_d.rearrange("(kt p) t -> p kt t", p=128))
    cp("dve", idb[:, :], ID32)
    cp("dve", oneb[:, :], ONE32)
    o_, w_ = CST["LMU"]
    ldw(lmu[:, :, :], cst_d[:, o_:o_ + w_].rearrange("p (a b) -> p a b", a=7))
    o_, w_ = CST["LML"]
    ldw(lml[:, :, :], cst_d[:, o_:o_ + w_].rearrange("p (a b) -> p a b", a=7))

    G_MIX, G_MLP, G_HM, G_HL, G_HD, CW = 4, 12, 20, 21, 22, 23

    def pcol(i):
        return prm[:, i:i + 1]

    def rmsnorm_block(blk, gbase, gt, out_tile, sq, rs1, rs2, out_sl=slice(0, 512)):
        tsl = slice(blk * 512, (blk + 1) * 512)
        act(sq[:, :, :], xT[:, :, tsl], AF.Square)
        for kt in range(8):
            mm(PF[0][:, :], oneb[:, :], sq[:, kt, :], start=(kt == 0), stop=(kt == 7))
        act(rs1[:, :], PF[0][:, :], AF.Ln, bias=EPS, scale=1.0 / D)
        act(rs2[:, :], rs1[:, :], AF.Exp, scale=-0.5)
        for kt in range(8):
            stt(out_tile[:, kt, out_sl], xT[:, kt, tsl], gt[:, gbase + kt:gbase + kt + 1], rs2[:, :], ALU.mult, ALU.mult)

    for l in range(nl):
        if stop == "p0":
            break
        dma("sp", prm[:, :], prm_d[l])
        tsc("dve", drv[:, 0:1], prm[:, 1:2], -1.0, ALU.mult)
        act(drv[:, 1:2], prm[:, 2:3], AF.Exp)

        with ExitStack() as p1:
            def sb1(name, shape, dt=F32):
                return p1.enter_context(nc.sbuf_tensor("%s_l%d" % (name, l), list(shape), dt))
            sq = sb1("p1sq", [128, 8, 512], BF16)
            rs1 = sb1("p1rs1", [128, 512])
            rs2 = sb1("p1rs2", [128, 512])
            ub = [sb1("p1u0", [128, 8, 512], BF16), sb1("p1u1", [128, 8, 512], BF16)]
            for blk in range(4):
                rmsnorm_block(blk, G_MIX, prm, ub[blk % 2], sq, rs1, rs2)
                dma("sp", u_loc[blk].rearrange("(kt p) t -> p kt t", p=128), ub[blk % 2][:, :, :])
                S.cc(lambda e, blk=blk: e.collective_compute("AllGather", ALU.bypass, replica_groups=groups,
                                                             ins=[u_loc[blk]], outs=[u_all[blk]]),
                     reads=[u_loc[blk]], writes=[u_all[blk]])
            S.barrier()
        if stop == "p1":
            break

        with ExitStack() as p2:
            def sb2(name, shape, dt=F32):
                return p2.enter_context(nc.sbuf_tensor("%s_l%d" % (name, l), list(shape), dt))

            whs2 = sb2("whs", [128, 8 * WH], BF16)
            whs = whs2[:, :].rearrange("p (kt c) -> p kt c", kt=8)
            wlr = sb2("wlr_s", [17, 128])
            ldw(whs2[:, :], wh_d[l])
            dma("sp", wlr[:, :], wlr_d[l])
            WF = lambda kt, g: whs[:, kt, g * 128:(g + 1) * 128]
            WT1 = lambda kt: whs[:, kt, WH_F:WH_F + WH_T1]
            WT2 = lambda kt: whs[:, kt, WH_F + WH_T1:WH_F + WH_T1 + WH_T2]
            WLL = lambda kt: whs[:, kt, WH_F + WH_T1 + WH_T2:WH]

            ut = [sb2("ut0", [128, 8, 512], BF16), sb2("ut1", [128, 8, 512], BF16)]
            qTm = sb2("qTm", [128, 512], BF16)
            kTm = sb2("kTm", [128, 512], BF16)
            lqT = sb2("lqT", [128, 512])
            lkT = sb2("lkT", [128, 512])
            XC = [sb2("XC%d" % m, [128, 515]) for m in range(3)]
            cacc = sb2("cacc", [128, 512])
            csil = [sb2("csil%d" % m, [128, 512]) for m in range(2)]
            csq = sb2("csq", [128, 512], BF16)
            crn1 = sb2("crn1", [128, 512])
            crn2 = sb2("crn2", [128, 512])
            dT = [sb2("dT%d" % m, [128, 512], BF16) for m in range(3)]
            llrT = sb2("llrT", [17, 512])
            vaug = sb2("vaug", [128, 4, 192], BF16)
            sigo = sb2("sigo", [128, 4, 128], BF16)
            sm = sb2("sm", [128, 4, 4])
            km = sb2("km", [128, 4, 128])
            kl = sb2("kl", [128, 4, 128])
            vl = sb2("vl", [128, 4, 128], BF16)
            silr = sb2("silr", [128, 4, 128], BF16)
            silz = sb2("silz", [128, 4, 128], BF16)
            gt = sb2("gt", [128, 8, 4])
            LFB = sb2("LFB", [128, 128])
            LFBd = sb2("LFBd", [128, 128])
            coltm = sb2("coltm", [128, 4])
            coltd = sb2("coltd", [128, 4])
            DmT1 = sb2("DmT", [128, 128])
            EbM1 = sb2("EbM", [128, 128])
            DmT = [DmT1] * 4
            EbM = [EbM1] * 4
            ebl = sb2("ebl", [128, 12])
            PTm = [sb2("PTm%d" % j, [128, 128], BF16) for j in range(4)]
            qtm = [sb2("qtm%d" % j, [128, 128], BF16) for j in range(4)]
            kwm = [sb2("kwm%d" % j, [128, 128], BF16) for j in range(4)]
            GaT1 = sb2("GaT", [128, 128])
            GaT = [GaT1] * 4
            GaS = sb2("GaS", [128, 128])
            EbD1 = sb2("EbD", [128, 128])
            EbD = [EbD1] * 4
            Abar = sb2("Abar", [128, 4, 128], BF16)
            AbarT = sb2("AbarT", [128, 4, 128], BF16)
            QKm = [sb2("QKm%d" % j, [128, 128], BF16) for j in range(4)]
            qtd = [sb2("qtd%d" % j, [128, 128], BF16) for j in range(4)]
            khat = [sb2("khat%d" % j, [128, 128], BF16) for j in range(4)]
            kwd = [sb2("kwd%d" % j, [128, 128], BF16) for j in range(4)]
            vd = [sb2("vd%d" % j, [128, 128], BF16) for j in range(4)]
            NUl = sb2("NUl", [128, 4, 128], BF16)
            NLl = sb2("NLl", [128, 4, 128], BF16)
            Rm = sb2("Rm", [128, 4, 128], BF16)
            RTm = sb2("RTm", [128, 4, 128], BF16)
            Ysb = sb2("Ysb", [128, 4, 128], BF16)
            Ypsb = sb2("Ypsb", [128, 4, 128], BF16)
            nW0T = [sb2("nW0T%d" % j, [128, 128], BF16) for j in range(4)]
            vnew = sb2("vnew", [128, 128], BF16)
            e4 = sb2("e4", [128, 128])
            spl = sb2("spl", [128, 128])
            E1a = sb2("E1", [128, 128])
            E1 = [E1a] * 4
            E2 = sb2("E2", [128, 128])
            E3 = sb2("E3", [128, 128])
            qtl = [sb2("qtl%d" % j, [128, 128], BF16) for j in range(4)]
            ktlT = [sb2("ktlT%d" % j, [128, 128], BF16) for j in range(4)]
            ktl = [sb2("ktl%d" % j, [128, 128], BF16) for j in range(4)]
            ATl = [sb2("ATl%d" % j, [128, 128], BF16) for j in range(4)]
            NDm = sb2("NDm", [128, 4, 160])
            Ol = sb2("Ol", [128, 4, 128])
            Od = sb2("Od", [128, 4, 128])
            hg = sb2("hg", [128, 4, 128])
            junk = e4
            pre_o = sb2("pre_o", [128, 4, 128], BF16)
            brT1 = sb2("brT0", [128, 3, 512], BF16)
            brT = [brT1, brT1]
            Cn32 = sb2("Cn32", [128, 129])
            Cnb = sb2("Cnb", [128, 192], BF16)
            Sl32 = sb2("Sl32", [128, 128])
            Slb = sb2("Slb", [128, 128], BF16)
            Sd32 = sb2("Sd32", [128, 128])
            Sdb = sb2("Sdb", [128, 128], BF16)
            stmp = sb2("stmp", [128, 128])
            post = sb2("post", [128, 16])

            memset("dve", Cn32[:, :], 0.0)
            memset("dve", Cnb[:, :], 0.0)
            memset("dve", Sl32[:, :], 0.0)
            memset("dve", Slb[:, :], 0.0)
            memset("dve", Sd32[:, :], 0.0)
            memset("dve", Sdb[:, :], 0.0)
            memset("dve", vaug[:, :, :], 1.0)
            memset("dve", llrT[:, :], 1.0)
            for m in range(3):
                memset("dve", XC[m][:, 0:3], 0.0)

            def load_ut(B):
                rr, lb = B // 4, B % 4
                dma("sp", ut[B % 2][:, :, :],
                    u_all[lb].rearrange("(r kt p) t -> p r kt t", r=4, kt=8, p=128)[:, rr, :, :])

            load_ut(0)
            nb_ = min(NBLK, BLK_LIMIT)

            def block_gen(B):
                if B + 1 < nb_:
                    load_ut(B + 1)
                U = ut[B % 2]
                S.section(1)
                for g in range(7):
                    pf = PF[g % 2]
                    for kt in range(8):
                        mm(pf[:, :], WF(kt, g), U[:, kt, :], start=(kt == 0), stop=(kt == 7))
                    if g == 0:
                        act(qTm[:, :], pf[:, :], AF.Copy, scale=QS)
                    elif g == 1:
                        cp("dve", kTm[:, :], pf[:, :])
                    elif g == 2:
                        act(lqT[:, :], pf[:, :], AF.Copy, scale=QS)
                    elif g == 3:
                        cp("dve", lkT[:, :], pf[:, :])
                    else:
                        m = g - 4
                        if m % 2 == 0:
                            cp("act", XC[m][:, 3:515], pf[:, :])
                        else:
                            cp("dve", XC[m][:, 3:515], pf[:, :])
                for kt in range(8):
                    mm(PF[1][0:16, :], WLL(kt), U[:, kt, :], start=(kt == 0), stop=(kt == 7))
                cp("dve", llrT[0:16, :], PF[1][0:16, :])

                S.section(2)
                for m in range(3):
                    cw = lambda j, m=m: pcol(CW + m * 4 + j)
                    tsc("dve", cacc[:, :], XC[m][:, 3:515], cw(3), ALU.mult)
                    for j in (2, 1, 0):
                        stt(cacc[:, :], XC[m][:, j:j + 512], cw(j), cacc[:, :], ALU.mult, ALU.add)
                    cp("pool", XC[m][:, 0:3], XC[m][:, 512:515])
                    if m < 2:
                        act(csil[m][:, :], cacc[:, :], AF.Silu)
                        act(csq[:, :], csil[m][:, :], AF.Square)
                        mm(PF[m][:, :], oneb[:, :], csq[:, :])
                        act(crn1[:, :], PF[m][:, :], AF.Ln, bias=EPS)
                        act(crn2[:, :], crn1[:, :], AF.Exp, scale=-0.5)
                        if m == 0:
                            stt(dT[0][:, :], csil[0][:, :], QS, crn2[:, :], ALU.mult, ALU.mult)
                        else:
                            tt("dve", dT[1][:, :], csil[1][:, :], crn2[:, :], ALU.mult)
                    else:
                        act(dT[2][:, :], cacc[:, :], AF.Silu)

                yield
                S.section(3)
                for j in range(4):
                    tk = slice(j * 128, (j + 1) * 128)
                    for kt in range(8):
                        mm(PT1[:, 0:WH_T1], U[:, kt, tk], WT1(kt), start=(kt == 0), stop=(kt == 7))
                    for kt in range(8):
                        mm(PT2[:, :], U[:, kt, tk], WT2(kt), start=(kt == 0), stop=(kt == 7))
                    S.section(3.1)
                    cp("act", km[:, j, :], PT1[:, 0:128])
                    S.section(3.2)
                    cp("dve", vaug[:, j, 0:128], PT1[:, 128:256])
                    S.section(3.3)
                    act(sigo[:, j, :], PT1[:, 256:384], AF.Sigmoid)
                    S.section(3.4)
                    cp("dve", sm[:, j, :], PT1[:, 384:388])
                    S.section(3.5)
                    cp("act", kl[:, j, :], PT2[:, 0:128])
                    cp("dve", vl[:, j, :], PT2[:, 128:256])
                    S.section(3.6)
                    act(silr[:, j, :], PT2[:, 256:384], AF.Silu)
                    act(silz[:, j, :], PT2[:, 384:512], AF.Silu)
                    S.section(3)

                S.section(4)
                act(gt[:, 4, :], sm[:, :, 1], AF.Exp, bias=drv[:, 0:1], scale=-1.0)
                act(gt[:, 0, :], gt[:, 4, :], AF.Ln, bias=1.0)
                tsc("dve", gt[:, 1, :], sm[:, :, 0], pcol(0), ALU.add)
                act(gt[:, 5, :], sm[:, :, 3], AF.Exp, bias=pcol(3))
                act(gt[:, 6, :], gt[:, 5, :], AF.Ln, bias=1.0)
                tsc("dve", gt[:, 2, :], gt[:, 6, :], drv[:, 1:2], ALU.mult)
                act(gt[:, 7, :], sm[:, :, 2], AF.Exp, scale=-1.0)
                tsc("dve", gt[:, 7, :], gt[:, 7, :], 1.0, ALU.add)
                recip(gt[:, 3, :], gt[:, 7, :])

                def interleave(gens):
                    gens = list(gens)
                    while gens:
                        for g_ in list(gens):
                            try:
                                next(g_)
                            except StopIteration:
                                gens.remove(g_)

                def dec_mlstm():
                    for j in range(4):
                        tk = slice(j * 128, (j + 1) * 128)
                        nlc = gt[:, 0, j:j + 1]
                        mm(PM[:, 0:1], NU, nlc)
                        tsc("dve", LFB[:, :], ONE32, nlc, ALU.mult)
                        yield
                        mm(PM[:, 128:256], LFB[:, :], NU)
                        mm(PM[:, 256:384], LFB[:, :], NU, start=True, stop=False)
                        mm(PM[:, 256:384], ID32, NEGI, start=False, stop=True)
                        mm(PM[:, 384:512], kTm[:, tk], qTm[:, tk])
                        yield
                        tt("dve", coltm[:, 0:1], gt[:, 1, j:j + 1], PM[:, 0:1], ALU.subtract)
                        yield
                        act(DmT[j][:, :], PM[:, 256:384], AF.Exp, bias=coltm[:, 0:1])
                        yield
                        act(EbM[j][:, :], PM[:, 128:256], AF.Exp)
                        yield
                        tt("dve", PTm[j][:, :], PM[:, 384:512], DmT[j][:, :], ALU.mult)
                        yield
                        cp("pool", ebl[:, j:j + 1], EbM[j][:, 127:128])
                        tt("pool", qtm[j][:, :], qTm[:, tk], EbM[j][:, :], ALU.mult)
                        yield
                        tsc("pool", kwm[j][:, :], km[:, j, :], DmT[j][:, 127:128], ALU.mult, 0.0, ALU.add)
                        yield

                def dec_gdn():
                    for j in range(4):
                        tk = slice(j * 128, (j + 1) * 128)
                        gsc = gt[:, 2, j:j + 1]
                        mm(PN[:, 0:1], NU, gsc)
                        tsc("dve", LFBd[:, :], ONE32, gsc, ALU.mult)
                        yield
                        mm(PN[:, 128:256], LFBd[:, :], NU)
                        mm(PN[:, 256:384], LFBd[:, :], NU, start=True, stop=False)
                        mm(PN[:, 256:384], ID32, NEGI, start=False, stop=True)
                        mm(PN[:, 384:512], LFBd[:, :], NU, start=True, stop=False)
                        mm(PN[:, 384:512], ID32, NEGS, start=False, stop=True)
                        mm(PT1[:, 0:128], dT[1][:, tk], dT[1][:, tk])
                        mm(PT1[:, 128:256], dT[1][:, tk], dT[0][:, tk])
                        tr(PB[:, 0:128], dT[1][:, tk], idb[:, :])
                        tr(PB[:, 128:256], dT[2][:, tk], idb[:, :])
                        yield
                        tsc("dve", coltd[:, 1:2], PN[:, 0:1], -1.0, ALU.mult)
                        yield
                        act(coltd[:, 2:3], PN[:, 0:1], AF.Exp)
                        yield
                        act(GaT[j][:, :], PN[:, 256:384], AF.Exp, bias=coltd[:, 1:2])
                        yield
                        act(GaS[:, :], PN[:, 384:512], AF.Exp, bias=coltd[:, 1:2])
                        yield
                        act(EbD[j][:, :], PN[:, 128:256], AF.Exp)
                        yield
                        cp("act", vd[j][:, :], PB[:, 128:256])
                        yield
                        stt(Abar[:, j, :], PT1[:, 0:128], gt[:, 3, j:j + 1], GaS[:, :], ALU.mult, ALU.mult)
                        yield
                        tr(PB[:, 256:384], Abar[:, j, :], idb[:, :])
                        tt("dve", QKm[j][:, :], PT1[:, 128:256], GaT[j][:, :], ALU.mult)
                        yield
                        cp("pool", ebl[:, 4 + j:5 + j], EbD[j][:, 127:128])
                        tt("pool", qtd[j][:, :], dT[0][:, tk], EbD[j][:, :], ALU.mult)
                        yield
                        tsc("dve", khat[j][:, :], PB[:, 0:128], coltd[:, 2:3], ALU.mult)
                        yield
                        tsc("dve", kwd[j][:, :], PB[:, 0:128], GaT[j][:, 127:128], ALU.mult)
                        yield
                        cp("act", AbarT[:, j, :], PB[:, 256:384])
                        yield

                def dec_gla():
                    for j in range(4):
                        tk = slice(j * 128, (j + 1) * 128)
                        mm(PO[:, 0:128], llrT[0:17, tk], wlr[0:17, :])
                        yield
                        act(e4[:, :], PO[:, 0:128], AF.Exp, scale=-1.0)
                        yield
                        act(spl[:, :], e4[:, :], AF.Ln, bias=1.0)
                        yield
                        mm(PO[:, 128:256], NU, spl[:, :])
                        mm(PO[:, 256:384], spl[:, :], NU)
                        yield
                        act(E1[j][:, :], PO[:, 256:384], AF.Exp, scale=1.0 / 16)
                        yield
                        act(E2[:, :], PO[:, 256:384], AF.Exp, scale=-1.0 / 16)
                        yield
                        act(E3[:, :], PO[:, 128:256], AF.Exp, scale=-1.0 / 16)
                        yield
                        cp("pool", ebl[:, 8 + j:9 + j], E1[j][:, 127:128])
                        tt("pool", qtl[j][:, :], lqT[:, tk], E1[j][:, :], ALU.mult)
                        yield
                        tt("pool", ktlT[j][:, :], lkT[:, tk], E2[:, :], ALU.mult)
                        yield
                        tt("dve", ktl[j][:, :], kl[:, j, :], E3[:, :], ALU.mult)
                        yield
                        mm(PO[:, 384:512], ktlT[j][:, :], qtl[j][:, :])
                        yield
                        tt("dve", ATl[j][:, :], PO[:, 384:512], UM, ALU.mult)
                        yield

                S.section(5)
                interleave([dec_mlstm(), dec_gdn(), dec_gla()])

                S.section(8)
                def lvmask(i):
                    tt("pool", NUl[:, :, :], Abar[:, :, :], lmu[:, i:i + 1, :].to_broadcast([128, 4, 128]), ALU.mult)
                    tt("pool", NLl[:, :, :], AbarT[:, :, :], lml[:, i:i + 1, :].to_broadcast([128, 4, 128]), ALU.mult)
                lvmask(0)
                idb4 = idb[:, :].unsqueeze(1).to_broadcast([128, 4, 128])
                tt("dve", Rm[:, :, :], idb4, NUl[:, :, :], ALU.subtract)
                tt("dve", RTm[:, :, :], idb4, NLl[:, :, :], ALU.subtract)
                for i in range(1, 7):
                    lvmask(i)
                    last = (i == 6)
                    for j in range(4):
                        mm(PM[:, j * 128:(j + 1) * 128], NLl[:, j, :], Rm[:, j, :])
                    cp("act", Ysb[:, :, :], PM[:, :].rearrange("p (a b) -> p a b", a=4))
                    if not last:
                        for j in range(4):
                            mm(PN[:, j * 128:(j + 1) * 128], NUl[:, j, :], RTm[:, j, :])
                        cp("dve", Ypsb[:, :, :], PN[:, :].rearrange("p (a b) -> p a b", a=4))
                    for j in range(4):
                        mm(PO[:, j * 128:(j + 1) * 128], RTm[:, j, :], Ysb[:, j, :])
                    if not last:
                        for j in range(4):
                            mm(PT1[:, j * 128:(j + 1) * 128], Rm[:, j, :], Ypsb[:, j, :])
                    tt("dve", Rm[:, :, :], Rm[:, :, :], PO[:, :].rearrange("p (a b) -> p a b", a=4), ALU.subtract)
                    if not last:
                        tt("dve", RTm[:, :, :], RTm[:, :, :], PT1[:, :].rearrange("p (a b) -> p a b", a=4), ALU.subtract)
                for j in range(4):
                    mm(PN[:, j * 128:(j + 1) * 128], khat[j][:, :], Rm[:, j, :])
                    tsc("dve", nW0T[j][:, :], PN[:, j * 128:(j + 1) * 128], -1.0, ALU.mult)

                S.section(9)
                def rec_mlstm():
                    for j in range(4):
                        mm(PO[:, 0:129], PTm[j][:, :], vaug[:, j, 0:129], start=True, stop=False)
                        mm(PO[:, 0:129], qtm[j][:, :], Cnb[:, 0:129], start=False, stop=True)
                        mm(PM[:, 0:129], kwm[j][:, :], vaug[:, j, 0:129])
                        yield
                        stt(Cn32[:, :], Cn32[:, :], ebl[:, j:j + 1], PM[:, 0:129], ALU.mult, ALU.add)
                        yield
                        cp("pool", Cnb[:, 0:129], Cn32[:, :])
                        yield
                        cp("act", NDm[:, j, 0:129], PO[:, 0:129])
                        yield

                def rec_gla():
                    for j in range(4):
                        mm(PF[0][:, 0:128], ATl[j][:, :], vl[:, j, :], start=True, stop=False)
                        mm(PF[0][:, 0:128], qtl[j][:, :], Slb[:, :], start=False, stop=True)
                        mm(PF[1][:, 0:128], ktl[j][:, :], vl[:, j, :])
                        yield
                        act(stmp[:, :], PF[1][:, 0:128], AF.Identity, scale=ebl[:, 8 + j:9 + j])
                        yield
                        stt(Sl32[:, :], Sl32[:, :], ebl[:, 8 + j:9 + j], stmp[:, :], ALU.mult, ALU.add)
                        yield
                        cp("pool", Slb[:, :], Sl32[:, :])
                        yield
                        cp("act", Ol[:, j, :], PF[0][:, 0:128])
                        yield

                def rec_gdn():
                    for j in range(4):
                        mm(PN[:, 0:128], Rm[:, j, :], vd[j][:, :], start=True, stop=False)
                        mm(PN[:, 0:128], nW0T[j][:, :], Sdb[:, :], start=False, stop=True)
                        yield
                        tsc("dve", vnew[:, :], PN[:, 0:128], gt[:, 3, j:j + 1], ALU.mult)
                        yield
                        mm(PT1[:, 0:128], QKm[j][:, :], vnew[:, :], start=True, stop=False)
                        mm(PT1[:, 0:128], qtd[j][:, :], Sdb[:, :], start=False, stop=True)
                        mm(PN[:, 128:256], kwd[j][:, :], vnew[:, :])
                        yield
                        stt(Sd32[:, :], Sd32[:, :], ebl[:, 4 + j:5 + j], PN[:, 128:256], ALU.mult, ALU.add)
                        yield
                        cp("pool", Sdb[:, :], Sd32[:, :])
                        yield
                        cp("act", Od[:, j, :], PT1[:, 0:128])
                        yield

                interleave([rec_gdn(), rec_mlstm(), rec_gla()])


                yield
                S.section(10)
                bt = brT[B % 2]
                act(post[:, 0:4], NDm[:, :, 128], AF.Abs)
                tsc("dve", post[:, 0:4], post[:, 0:4], 1.0, ALU.max)
                recip(post[:, 4:8], post[:, 0:4])
                for j in range(4):
                    stt(hg[:, j, :], NDm[:, j, 0:128], post[:, 4 + j:5 + j], sigo[:, j, :], ALU.mult, ALU.mult)
                srcs = [(hg, None, G_HM), (Ol, silr, G_HL), (Od, silz, G_HD)]
                for n, (src, gate, gi) in enumerate(srcs):
                    for j in range(4):
                        S.op("act", lambda e, src=src, j=j: e.activation(
                            out=junk[:, :], in_=src[:, j, :], func=AF.Square, accum_out=post[:, 8 + j:9 + j]),
                            reads=[src], writes=[junk, post])
                    act(post[:, 12:16], post[:, 8:12], AF.Ln, bias=EPS, scale=1.0 / HD)
                    act(post[:, 12:16], post[:, 12:16], AF.Exp, scale=-0.5)
                    for j in range(4):
                        if gate is None:
                            tsc("dve", pre_o[:, j, :], src[:, j, :], post[:, 12 + j:13 + j], ALU.mult)
                        else:
                            stt(pre_o[:, j, :], src[:, j, :], post[:, 12 + j:13 + j], gate[:, j, :], ALU.mult, ALU.mult)
                        tr(PB[:, 512 + j * 128:512 + (j + 1) * 128], pre_o[:, j, :], idb[:, :])
                    act(bt[:, n, :], PB[:, 512:1024], AF.Identity, scale=pcol(gi))
                kk = B // 2
                dma("sp", br_loc[kk].rearrange("(n p) t -> p n t", p=128)[:, :, (B % 2) * 512:(B % 2 + 1) * 512],
                    bt[:, :, :])
                if B % 2 == 1:
                    S.cc(lambda e, kk=kk: e.collective_compute("AllGather", ALU.bypass, replica_groups=groups,
                                                               ins=[br_loc[kk]], outs=[br_all[kk]]),
                         reads=[br_loc[kk]], writes=[br_all[kk]])
                    if dbg and l == 0:
                        dma("sp", dbg_br[:, kk * 1024:(kk + 1) * 1024], br_loc[kk])
            bgens = [block_gen(B) for B in range(nb_)]
            next(bgens[0])
            for B in range(nb_):
                next(bgens[B])
                if B + 1 < nb_:
                    next(bgens[B + 1])
                for _ in bgens[B]:
                    pass
            S.section(0)
            S.barrier()
        if stop == "p2":
            break

        with ExitStack() as p3:
            def sb3(name, shape, dt=F32):
                return p3.enter_context(nc.sbuf_tensor("%s_l%d" % (name, l), list(shape), dt))
            HT = TOK // 2
            uT = sb3("uT3", [128, 8, HT], BF16)
            brs = sb3("brs", [128, 12, HT], BF16)
            mixT = sb3("mixT", [128, 8, HT], BF16)
            brq = [sb3("brq0", [128, 12, 256], BF16), sb3("brq1", [128, 12, 256], BF16)]
            wgu = [sb3("wgu0", [128, 36 * 128], BF16), sb3("wgu1", [128, 36 * 128], BF16)]
            wo = [sb3("wo0", [128, 1024], BF16), sb3("wo1", [128, 1024], BF16)]
            sgt = [sb3("sgt0", [128, 512]), sb3("sgt1", [128, 512])]
            macc = sb3("macc", [128, 512])
            it = 0
            for hf in range(2):
                t0 = hf * HT
                for k2 in range(2):
                    dma("sp", uT[:, :, k2 * 512:(k2 + 1) * 512],
                        u_loc[hf * 2 + k2].rearrange("(kt p) t -> p kt t", p=128))
                for tb in range(HT // 256):
                    for q in range(4):
                        bq = brq[it % 2]
                        it += 1
                        dma("sp", bq[:, :, :],
                            br_all[2 * q + hf].rearrange("(hn p) t -> p hn t", p=128)[:, :, tb * 256:(tb + 1) * 256])
                        dst = brs[:, :, tb * 256:(tb + 1) * 256]
                        if q == 0:
                            tsc("dve", dst, bq[:, :, :], msel[:, 0:1], ALU.mult)
                        else:
                            stt(dst, bq[:, :, :], msel[:, q:q + 1], dst, ALU.mult, ALU.add)
                ldw(wgu[0][:, :], wgu_d[l, 0])
                for d in range(8):
                    if d + 1 < 8:
                        ldw(wgu[(d + 1) % 2][:, :], wgu_d[l, d + 1])
                    W = wgu[d % 2]
                    for tb in range(HT // 512):
                        tsl = slice(tb * 512, (tb + 1) * 512)
                        for n in range(3):
                            pg = PF[n % 2]
                            pu = PM if n % 2 == 0 else PN
                            for kt in range(8):
                                c0 = (n * 12 + kt) * 128
                                mm(pg[:, :], W[:, c0:c0 + 128], uT[:, kt, tsl], start=(kt == 0), stop=(kt == 7))
                            for h in range(4):
                                c0 = (n * 12 + 8 + h) * 128
                                mm(pu[:, :], W[:, c0:c0 + 128], brs[:, h * 3 + n, tsl], start=(h == 0), stop=(h == 3))
                            sg = sgt[n % 2]
                            act(sg[:, :], pg[:, :], AF.Sigmoid)
                            if n == 0:
                                tt("dve", macc[:, :], sg[:, :], pu[:, :], ALU.mult)
                            elif n == 1:
                                tt("dve", sg[:, :], sg[:, :], pu[:, :], ALU.mult)
                                tt("pool", macc[:, :], macc[:, :], sg[:, :], ALU.add)
                            else:
                                tt("dve", sg[:, :], sg[:, :], pu[:, :], ALU.mult)
                                tt("dve", mixT[:, d, tsl], macc[:, :], sg[:, :], ALU.add)
                ldw(wo[0][:, :], wo_d[l, 0])
                for d in range(8):
                    if d + 1 < 8:
                        ldw(wo[(d + 1) % 2][:, :], wo_d[l, d + 1])
                    W = wo[d % 2]
                    for tb in range(HT // 512):
                        tsl = slice(tb * 512, (tb + 1) * 512)
                        xsl = slice(t0 + tb * 512, t0 + (tb + 1) * 512)
                        pq = PT1 if tb % 2 == 0 else PT2
                        for kt in range(8):
                            mm(pq[:, :], W[:, kt * 128:(kt + 1) * 128], mixT[:, kt, tsl], start=(kt == 0), stop=(kt == 7))
                        tt("dve", xT[:, d, xsl], xT[:, d, xsl], pq[:, :], ALU.add)
            S.barrier()

        with ExitStack() as p4:
            def sb4(name, shape, dt=F32):
                return p4.enter_context(nc.sbuf_tensor("%s_l%d" % (name, l), list(shape), dt))
            u2 = sb4("u2T", [128, 8, TOK], BF16)
            sq = sb4("p4sq", [128, 8, 512], BF16)
            rs1 = sb4("p4rs1", [128, 512])
            rs2 = sb4("p4rs2", [128, 512])
            hT = sb4("hT", [128, 8, TOK], BF16)
            rl = [sb4("rl0", [128, 512], BF16), sb4("rl1", [128, 512], BF16)]
            w1 = [sb4("w1_%d" % i, [128, 1024], BF16) for i in range(3)]
            w2 = [sb4("w2_%d" % i, [128, 1024], BF16) for i in range(3)]
            for blk in range(4):
                rmsnorm_block(blk, G_MLP, prm, u2, sq, rs1, rs2, out_sl=slice(blk * 512, (blk + 1) * 512))
            cnt = 0
            for c in range(4):
                ldw(w1[0][:, :], w1_d[l, c * 8])
                for f in range(8):
                    if f + 1 < 8:
                        ldw(w1[(f + 1) % 3][:, :], w1_d[l, c * 8 + f + 1])
                    W = w1[f % 3]
                    for tb in range(4):
                        tsl = slice(tb * 512, (tb + 1) * 512)
                        pq = PF[cnt % 2]
                        r_ = rl[cnt % 2]
                        cnt += 1
                        for kt in range(8):
                            mm(pq[:, :], W[:, kt * 128:(kt + 1) * 128], u2[:, kt, tsl], start=(kt == 0), stop=(kt == 7))
                        act(r_[:, :], pq[:, :], AF.Relu)
                        tt("pool", hT[:, f, tsl], r_[:, :], r_[:, :], ALU.mult)
                ldw(w2[0][:, :], w2_d[l, c * 8])
                for d in range(8):
                    if d + 1 < 8:
                        ldw(w2[(d + 1) % 3][:, :], w2_d[l, c * 8 + d + 1])
                    W = w2[d % 3]
                    for tb in range(4):
                        tsl = slice(tb * 512, (tb + 1) * 512)
                        pq = PM if (tb % 2 == 0) else PN
                        for ft in range(8):
                            mm(pq[:, :], W[:, ft * 128:(ft + 1) * 128], hT[:, ft, tsl], start=(ft == 0), stop=(ft == 7))
                        tt("dve", xT[:, d, tsl], xT[:, d, tsl], pq[:, :], ALU.add)
            S.barrier()

    with ExitStack() as p5:
        def sb5(name, shape, dt=F32):
            return p5.enter_context(nc.sbuf_tensor(name, list(shape), dt))
        outv = out_d.rearrange("(kt p) t -> p kt t", p=128)
        if do_final:
            sq = sb5("p5sq", [128, 8, 512], BF16)
            rs1 = sb5("p5rs1", [128, 512])
            rs2 = sb5("p5rs2", [128, 512])
            ob = [sb5("p5o0", [128, 8, 512]), sb5("p5o1", [128, 8, 512])]
            for blk in range(4):
                rmsnorm_block(blk, 0, gfin, ob[blk % 2], sq, rs1, rs2)
                dma("sp", outv[:, :, blk * 512:(blk + 1) * 512], ob[blk % 2][:, :, :])
        else:
            dma("sp", outv, xT[:, :, :])
        fin = [out_d] + ([dbg_br] if dbg else [])
        S.final_wait("sp", fin)
        S.emit()
    es.close()
    return nc


def _tile_kxm(w, mt):
    K, M = w.shape
    a = w.reshape(K // 128, 128, M // mt, mt)
    return np.ascontiguousarray(a.transpose(2, 1, 0, 3))


def prep_shared(inp, layers):
    wgu, wo, w1, w2 = [], [], [], []
    for l in layers:
        w_in = inp["w_in"][l]
        g = _tile_kxm(np.ascontiguousarray(w_in[:, O_G:O_G + 3072]), 128)
        g = g.reshape(3, 8, 128, 8, 128)
        up = np.stack([_tile_kxm(inp["w_up"][l][n], 128) for n in range(3)])
        blk = np.concatenate([g, up], axis=3)
        blk = np.ascontiguousarray(blk.transpose(1, 2, 0, 3, 4)).reshape(8, 128, 36 * 128)
        wgu.append(blk)
        wo.append(_tile_kxm(inp["w_out"][l], 128).reshape(8, 128, 1024))
        w1.append(_tile_kxm(inp["w_mlp_in"][l], 128).reshape(32, 128, 1024))
        m2 = inp["w_mlp_out"][l].reshape(4, 8, 128, 8, 128)
        w2.append(np.ascontiguousarray(m2.transpose(0, 3, 2, 1, 4)).reshape(32, 128, 1024))
    return {"wgu": np.stack(wgu), "wo": np.stack(wo), "w1": np.stack(w1), "w2": np.stack(w2)}


def prep_core(inp, layers, c):
    h = c % 4
    hs = slice(h * 128, (h + 1) * 128)
    wh, wlr, prm = [], [], []
    for l in layers:
        w = inp["w_in"][l]
        def col(o):
            return w[:, o + h * 128:o + (h + 1) * 128]
        def one(o):
            return w[:, o + h:o + h + 1]
        cat = np.concatenate([col(O_MQ), col(O_MK), col(O_LQ), col(O_LK), col(O_DQ), col(O_DK), col(O_DV),
                              col(O_MK), col(O_MV), col(O_MO), one(O_MI), one(O_MF), one(O_DB), one(O_DA),
                              col(O_LK), col(O_LV), col(O_LR), col(O_DZ),
                              w[:, O_LLR:O_LLR + 16]], axis=1)
        a = cat.reshape(8, 128, WH).transpose(1, 0, 2)
        wh.append(np.ascontiguousarray(a).reshape(128, 8 * WH))
        wlr.append(np.concatenate([inp["w_gla_lr"][l][:, hs], inp["b_gla"][l][None, hs]], axis=0))
        p = np.zeros((128, NPRM), np.float32)
        p[:, 0] = inp["b_if"][l][h]
        p[:, 1] = inp["b_if"][l][4 + h]
        p[:, 2] = inp["a_log"][l][h]
        p[:, 3] = inp["dt_bias"][l][h]
        p[:, 4:12] = inp["g_norm_mix"][l].reshape(8, 128).T
        p[:, 12:20] = inp["g_norm_mlp"][l].reshape(8, 128).T
        p[:, 20] = inp["g_head_mlstm"][l][hs]
        p[:, 21] = inp["g_head_gla"][l][hs]
        p[:, 22] = inp["g_head_gdn"][l][hs]
        for m in range(3):
            for j in range(4):
                p[:, 23 + m * 4 + j] = inp["conv_gdn"][l][j, m * 512 + h * 128:m * 512 + (h + 1) * 128]
        prm.append(p)
    ms = np.zeros((128, 4), np.float32)
    ms[:, h] = 1.0
    return {"wh": np.stack(wh), "wlr": np.stack(wlr).astype(np.float32), "prm": np.stack(prm), "msel": ms,
            "gfin": np.ascontiguousarray(inp["g_final"].reshape(8, 128).T)}


_PROG = {}


STOP = None
SEC_LIMIT = 1000
BLK_LIMIT = 1000


def _get_prog(nl, do_final, dbg=False):
    k = (nl, do_final, dbg, STOP)
    if k not in _PROG:
        _PROG[k] = build_program(nl, do_final, dbg, STOP)
    return _PROG[k]


def _run(inp, x_cores, layers, do_final, dbg=False):
    nc = _get_prog(len(layers), do_final, dbg)
    shared = prep_shared(inp, layers)
    cst = make_consts()
    in_maps = []
    for c in range(NCORES):
        m = {"xT": x_cores[c], "cst": cst}
        m.update(shared)
        m.update(prep_core(inp, layers, c))
        in_maps.append(m)
    res = run_bass_kernel_spmd(nc, in_maps, core_ids=list(range(NCORES)))
    return res


def kernel(**inputs):
    inp = {k: np.asarray(v) for k, v in inputs.items()}
    x = inp["x"]
    x_cores = []
    for c in range(NCORES):
        b, r = c // 4, c % 4
        x_cores.append(np.ascontiguousarray(x[b, r * TOK:(r + 1) * TOK, :].T))
    if FUSED:
        res = _run(inp, x_cores, [0, 1, 2, 3], True)
        outs = [res.results[c]["outT"] for c in range(NCORES)]
    else:
        for l in range(4):
            res = _run(inp, x_cores, [l], l == 3)
            x_cores = [np.ascontiguousarray(res.results[c]["outT"]) for c in range(NCORES)]
        outs = x_cores
    out = np.empty_like(x)
    for c in range(NCORES):
        b, r = c // 4, c % 4
        out[b, r * TOK:(r + 1) * TOK, :] = outs[c].T
    return out
```

```python
import numpy as np
from contextlib import ExitStack
import concourse.bass as bass
import concourse.mybir as mybir
from concourse.bass_utils import run_bass_kernel_spmd

F32 = mybir.dt.float32
BF16 = mybir.dt.bfloat16
AF = mybir.ActivationFunctionType
ALU = mybir.AluOpType

NCORES = 8
D = 1024
SEQ = 8192
TOK = 2048
NBLK = SEQ // 512
EPS = 1e-6
HD = 128
QS = HD ** -0.5
NPRM = 36
NEGV = -30000.0
FUSED = True

O_MQ, O_MK, O_MV, O_MO, O_MI, O_MF = 0, 512, 1024, 1536, 2048, 2052
O_LQ, O_LK, O_LV, O_LR, O_LLR = 2056, 2568, 3080, 3592, 4104
O_DQ, O_DK, O_DV, O_DZ, O_DB, O_DA, O_G = 4120, 4632, 5144, 5656, 6168, 6172, 6176
WH_F, WH_T1, WH_T2, WH_L = 896, 388, 512, 16
WH = WH_F + WH_T1 + WH_T2 + WH_L

CST = {}
_off = 0
for _n, _w in [("NU", 128), ("UM", 128), ("NEGI", 128), ("NEGS", 128), ("ID", 128), ("ONE", 128),
               ("LMU", 7 * 128), ("LML", 7 * 128)]:
    CST[_n] = (_off, _w)
    _off += _w
NCST = _off


def make_consts():
    c = np.zeros((128, NCST), np.float32)
    s = np.arange(128)[:, None]
    t = np.arange(128)[None, :]
    def put(n, a):
        o, w = CST[n]
        c[:, o:o + w] = a.reshape(128, w)
    put("NU", -(s <= t).astype(np.float32))
    put("UM", (s <= t).astype(np.float32))
    put("NEGI", np.where(s <= t, 0.0, NEGV).astype(np.float32))
    put("NEGS", np.where(s < t, 0.0, NEGV).astype(np.float32))
    put("ID", np.eye(128, dtype=np.float32))
    put("ONE", np.ones((128, 128), np.float32))
    lmu = np.zeros((128, 7, 128), np.float32)
    for i in range(7):
        b = 1 << i
        m = ((s // (2 * b)) == (t // (2 * b))) & ((s % (2 * b)) < b) & ((t % (2 * b)) >= b)
        lmu[:, i, :] = m
    put("LMU", lmu)
    put("LML", np.ascontiguousarray(lmu.transpose(2, 1, 0)))
    return c


class Sched:
    CE = ("pe", "act", "dve", "pool")

    def __init__(self, nc, es, ndma=8):
        self.nc = nc
        self.prog = {e: [] for e in ("pe", "act", "dve", "pool", "sp")}
        self.esem = {e: es.enter_context(nc.semaphore("s_" + e)) for e in self.CE}
        self.ecnt = {e: 0 for e in self.CE}
        self.dsem = {q: [es.enter_context(nc.semaphore("d_%s%d" % (q, i))) for i in range(ndma)]
                     for q in ("sp", "pool")}
        self.dcnt = {q: [0] * ndma for q in ("sp", "pool")}
        self.drr = {"sp": 0, "pool": 0}
        self.ccsem = es.enter_context(nc.semaphore("s_cc"))
        self.cccnt = 0
        self.seen = {e: {} for e in self.prog}
        self.lastw = {}
        self.readers = {}
        self.nops = 0
        self.enabled = True

    @staticmethod
    def key(x):
        if isinstance(x, (str, tuple)):
            return x
        t = getattr(x, "tensor", None)
        return t.name if t is not None else x.name

    PSUM_NAMES = ("PF0", "PF1", "PT1", "PT2", "PM", "PN", "PO", "PB")

    def _deps(self, reads, writes, me=None):
        deps = []
        for r in reads:
            t = self.lastw.get(r)
            if t is not None:
                deps.append(t)
            if r in self.PSUM_NAMES:
                for k, tk in self.readers.get(r, {}).items():
                    if k != me:
                        deps.append(tk)
        for w in writes:
            t = self.lastw.get(w)
            if t is not None:
                deps.append(t)
            deps.extend(self.readers.get(w, {}).values())
        return deps

    def _commit(self, tok, reads, writes):
        for r in reads:
            d = self.readers.setdefault(r, {})
            k = tok[0]
            if k not in d or d[k][2] < tok[2]:
                d[k] = tok
        for w in writes:
            self.lastw[w] = tok
            self.readers[w] = {}

    def _add(self, eng, deps, fn, tok, inc):
        waits = {}
        for (k, sem, val, peng) in deps:
            if eng == "pe" and peng == "pe":
                continue
            if self.seen[eng].get(k, 0) >= val:
                continue
            if k not in waits or waits[k][1] < val:
                waits[k] = (sem, val)
        for k, (sem, val) in waits.items():
            self.seen[eng][k] = val
        self.prog[eng].append((list(waits.values()), fn, tok, inc))
        self.nops += 1

    def section(self, k):
        self.enabled = (k <= SEC_LIMIT)

    def op(self, eng, fn, reads=(), writes=()):
        if not self.enabled:
            return
        reads = [self.key(r) for r in reads]
        writes = [self.key(w) for w in writes]
        deps = self._deps(reads, writes, "e_" + eng)
        self.ecnt[eng] += 1
        tok = ("e_" + eng, self.esem[eng], self.ecnt[eng], eng)
        self._add(eng, deps, fn, tok, 1)
        self._commit(tok, reads, writes)

    def dma(self, q, fn, reads=(), writes=()):
        if not self.enabled:
            return
        reads = [self.key(r) for r in reads]
        writes = [self.key(w) for w in writes]
        deps = self._deps(reads, writes)
        i = self.drr[q]
        self.drr[q] = (i + 1) % len(self.dsem[q])
        k = "d_%s%d" % (q, i)
        sem = self.dsem[q][i]
        if self.dcnt[q][i] > 0:
            deps.append((k, sem, self.dcnt[q][i], "dma"))
        self.dcnt[q][i] += 16
        tok = (k, sem, self.dcnt[q][i], "dma")
        self._add(q, deps, fn, tok, 16)
        self._commit(tok, reads, writes)

    def cc(self, fn, reads=(), writes=()):
        if not self.enabled:
            return
        reads = [self.key(r) for r in reads]
        writes = [self.key(w) for w in writes]
        deps = self._deps(reads, writes)
        if self.cccnt > 0:
            deps.append(("cc", self.ccsem, self.cccnt, "cc"))
        self.cccnt += 1
        tok = ("cc", self.ccsem, self.cccnt, "cc")
        self._add("pool", deps, fn, tok, 1)
        self._commit(tok, reads, writes)

    def barrier(self):
        deps = []
        for e in self.CE:
            if self.ecnt[e] > 0:
                deps.append(("e_" + e, self.esem[e], self.ecnt[e], e + "_b"))
        for q in ("sp", "pool"):
            for i, sem in enumerate(self.dsem[q]):
                if self.dcnt[q][i] > 0:
                    deps.append(("d_%s%d" % (q, i), sem, self.dcnt[q][i], "dma"))
        if self.cccnt > 0:
            deps.append(("cc", self.ccsem, self.cccnt, "cc"))
        for e in self.prog:
            self._add(e, list(deps), None, None, 0)

    def final_wait(self, eng, keys):
        deps = []
        for k in keys:
            t = self.lastw.get(self.key(k))
            if t is not None:
                deps.append(t)
        self._add(eng, deps, None, None, 0)

    def emit(self):
        nc = self.nc
        with nc.Block() as block:
            def run(name, e):
                for waits, fn, tok, inc in self.prog[name]:
                    for sem, val in waits:
                        e.wait_ge(sem, val)
                    if fn is None:
                        continue
                    ins = fn(e)
                    ins.then_inc(tok[1], inc)

            @block.tensor
            def _(e):
                run("pe", e)

            @block.scalar
            def _(e):
                run("act", e)

            @block.vector
            def _(e):
                run("dve", e)

            @block.gpsimd
            def _(e):
                run("pool", e)

            @block.sync
            def _(e):
                run("sp", e)


def build_program(nl, do_final, dbg=False, stop=None):
    nc = bass.Bass("TRN2", target_bir_lowering=False)
    es = ExitStack()

    def din(name, shape, dt=F32):
        return nc.dram_tensor(name, list(shape), dt, kind="ExternalInput").ap()

    xT_d = din("xT", [D, TOK])
    wh_d = din("wh", [nl, 128, 8 * WH])
    wlr_d = din("wlr", [nl, 17, 128])
    prm_d = din("prm", [nl, 128, NPRM])
    gfin_d = din("gfin", [128, 8])
    msel_d = din("msel", [128, 4])
    cst_d = din("cst", [128, NCST])
    wgu_d = din("wgu", [nl, 8, 128, 36 * 128])
    wo_d = din("wo", [nl, 8, 128, 1024])
    w1_d = din("w1", [nl, 32, 128, 1024])
    w2_d = din("w2", [nl, 32, 128, 1024])
    out_d = nc.dram_tensor("outT", [D, TOK], F32, kind="ExternalOutput").ap()
    u_loc = [nc.dram_tensor("u_loc%d" % k, [D, 512], BF16, kind="Internal").ap() for k in range(4)]
    u_all = [nc.dram_tensor("u_all%d" % k, [4 * D, 512], BF16, kind="Internal").ap() for k in range(4)]
    br_loc = [nc.dram_tensor("br_loc%d" % k, [384, 1024], BF16, kind="Internal").ap() for k in range(8)]
    br_all = [nc.dram_tensor("br_all%d" % k, [4 * 384, 1024], BF16, kind="Internal").ap() for k in range(8)]
    if dbg:
        dbg_br = nc.dram_tensor("dbg_br", [384, SEQ], BF16, kind="ExternalOutput").ap()
    groups = [[0, 1, 2, 3], [4, 5, 6, 7]]

    S = Sched(nc, es)

    def sb(name, shape, dt=F32):
        return es.enter_context(nc.sbuf_tensor(name, list(shape), dt))

    def ps(name, shape, dt=F32):
        return es.enter_context(nc.psum_tensor(name, list(shape), dt))

    def rw(reads, writes):
        return [r for r in reads if r is not None and not isinstance(r, (int, float))], writes

    def mm(out, lhsT, rhs, start=True, stop=True):
        S.op("pe", lambda e: e.matmul(out, lhsT=lhsT, rhs=rhs, start=start, stop=stop),
             reads=[lhsT, rhs], writes=[out])

    def tr(out, in_, ident):
        S.op("pe", lambda e: e.transpose(out, in_, ident), reads=[in_, ident], writes=[out])

    def act(out, in_, func, bias=None, scale=None, eng="act"):
        kw = {}
        r = [in_]
        if bias is not None:
            kw["bias"] = bias
            if not isinstance(bias, (int, float)):
                r.append(bias)
        if scale is not None:
            kw["scale"] = scale
            if not isinstance(scale, (int, float)):
                r.append(scale)
        S.op("act", lambda e: e.activation(out=out, in_=in_, func=func, **kw), reads=r, writes=[out])

    def tt(eng, out, in0, in1, op):
        S.op(eng, lambda e: e.tensor_tensor(out=out, in0=in0, in1=in1, op=op), reads=[in0, in1], writes=[out])

    def tsc(eng, out, in0, s1, op0, s2=None, op1=None):
        r = [in0] + [s for s in (s1, s2) if s is not None and not isinstance(s, (int, float))]
        if op1 is None:
            S.op(eng, lambda e: e.tensor_scalar(out=out, in0=in0, scalar1=s1, scalar2=None, op0=op0),
                 reads=r, writes=[out])
        else:
            S.op(eng, lambda e: e.tensor_scalar(out=out, in0=in0, scalar1=s1, scalar2=s2, op0=op0, op1=op1),
                 reads=r, writes=[out])

    def stt(out, in0, scalar, in1, op0, op1):
        r = [in0, in1] + ([scalar] if not isinstance(scalar, (int, float)) else [])
        S.op("dve", lambda e: e.scalar_tensor_tensor(out=out, in0=in0, scalar=scalar, in1=in1, op0=op0, op1=op1),
             reads=r, writes=[out])

    def cp(eng, out, in_):
        if eng == "act":
            S.op("act", lambda e: e.activation(out=out, in_=in_, func=AF.Copy), reads=[in_], writes=[out])
        else:
            S.op(eng, lambda e: e.tensor_copy(out=out, in_=in_), reads=[in_], writes=[out])

    def recip(out, in_):
        S.op("dve", lambda e: e.reciprocal(out=out, in_=in_), reads=[in_], writes=[out])

    def memset(eng, ap, v):
        S.op(eng, lambda e: e.memset(ap, v), reads=[], writes=[ap])

    def dma(q, out, in_, **kw):
        S.dma(q, lambda e: e.dma_start(out=out, in_=in_, **kw), reads=[in_], writes=[out])

    def ldw(out, in_):
        S.dma("pool", lambda e: e.dma_start(out=out, in_=in_, max_dma_last_dim=8192), reads=[in_], writes=[out])

    xT = sb("xT_s", [128, 8, TOK])
    cst = sb("cst_s", [128, 6 * 128])
    idb = sb("idb", [128, 128], BF16)
    oneb = sb("oneb", [128, 128], BF16)
    lmu = sb("lmu", [128, 7, 128], BF16)
    lml = sb("lml", [128, 7, 128], BF16)
    prm = sb("prm_s", [128, NPRM])
    drv = sb("drv_s", [128, 4])
    gfin = sb("gfin_s", [128, 8])
    msel = sb("msel_s", [128, 4])

    def C(n):
        o, w = CST[n]
        return cst[:, o:o + w]

    NU, UM, NEGI, NEGS, ID32, ONE32 = C("NU"), C("UM"), C("NEGI"), C("NEGS"), C("ID"), C("ONE")

    PF = [ps("PF0", [128, 512]), ps("PF1", [128, 512])]
    PT1 = ps("PT1", [128, 512])
    PT2 = ps("PT2", [128, 512])
    PM = ps("PM", [128, 512])
    PN = ps("PN", [128, 512])
    PO = ps("PO", [128, 512])
    PB = ps("PB", [128, 1024], BF16)

    dma("sp", cst[:, :], cst_d[:, 0:6 * 128])
    dma("sp", gfin[:, :], gfin_d)
    dma("sp", msel[:, :], msel_d)
    dma("sp", xT[:, :, :], xT_d.rearrange("(kt p) t -> p kt t", p=128))
    cp("dve", idb[:, :], ID32)
    cp("dve", oneb[:, :], ONE32)
    o_, w_ = CST["LMU"]
    ldw(lmu[:, :, :], cst_d[:, o_:o_ + w_].rearrange("p (a b) -> p a b", a=7))
    o_, w_ = CST["LML"]
    ldw(lml[:, :, :], cst_d[:, o_:o_ + w_].rearrange("p (a b) -> p a b", a=7))

    G_MIX, G_MLP, G_HM, G_HL, G_HD, CW = 4, 12, 20, 21, 22, 23

    def pcol(i):
        return prm[:, i:i + 1]

    def rmsnorm_block(blk, gbase, gt, out_tile, sq, rs1, rs2, out_sl=slice(0, 512)):
        tsl = slice(blk * 512, (blk + 1) * 512)
        act(sq[:, :, :], xT[:, :, tsl], AF.Square)
        for kt in range(8):
            mm(PF[0][:, :], oneb[:, :], sq[:, kt, :], start=(kt == 0), stop=(kt == 7))
        act(rs1[:, :], PF[0][:, :], AF.Ln, bias=EPS, scale=1.0 / D)
        act(rs2[:, :], rs1[:, :], AF.Exp, scale=-0.5)
        for kt in range(8):
            stt(out_tile[:, kt, out_sl], xT[:, kt, tsl], gt[:, gbase + kt:gbase + kt + 1], rs2[:, :], ALU.mult, ALU.mult)

    for l in range(nl):
        if stop == "p0":
            break
        dma("sp", prm[:, :], prm_d[l])
        tsc("dve", drv[:, 0:1], prm[:, 1:2], -1.0, ALU.mult)
        act(drv[:, 1:2], prm[:, 2:3], AF.Exp)

        with ExitStack() as p1:
            def sb1(name, shape, dt=F32):
                return p1.enter_context(nc.sbuf_tensor("%s_l%d" % (name, l), list(shape), dt))
            sq = sb1("p1sq", [128, 8, 512], BF16)
            rs1 = sb1("p1rs1", [128, 512])
            rs2 = sb1("p1rs2", [128, 512])
            ub = [sb1("p1u0", [128, 8, 512], BF16), sb1("p1u1", [128, 8, 512], BF16)]
            for blk in range(4):
                rmsnorm_block(blk, G_MIX, prm, ub[blk % 2], sq, rs1, rs2)
                dma("sp", u_loc[blk].rearrange("(kt p) t -> p kt t", p=128), ub[blk % 2][:, :, :])
                S.cc(lambda e, blk=blk: e.collective_compute("AllGather", ALU.bypass, replica_groups=groups,
                                                             ins=[u_loc[blk]], outs=[u_all[blk]]),
                     reads=[u_loc[blk]], writes=[u_all[blk]])
            S.barrier()
        if stop == "p1":
            break

        with ExitStack() as p2:
            def sb2(name, shape, dt=F32):
                return p2.enter_context(nc.sbuf_tensor("%s_l%d" % (name, l), list(shape), dt))

            whs2 = sb2("whs", [128, 8 * WH], BF16)
            whs = whs2[:, :].rearrange("p (kt c) -> p kt c", kt=8)
            wlr = sb2("wlr_s", [17, 128])
            ldw(whs2[:, :], wh_d[l])
            dma("sp", wlr[:, :], wlr_d[l])
            WF = lambda kt, g: whs[:, kt, g * 128:(g + 1) * 128]
            WT1 = lambda kt: whs[:, kt, WH_F:WH_F + WH_T1]
            WT2 = lambda kt: whs[:, kt, WH_F + WH_T1:WH_F + WH_T1 + WH_T2]
            WLL = lambda kt: whs[:, kt, WH_F + WH_T1 + WH_T2:WH]

            ut = [sb2("ut0", [128, 8, 512], BF16), sb2("ut1", [128, 8, 512], BF16)]
            qTm = sb2("qTm", [128, 512], BF16)
            kTm = sb2("kTm", [128, 512], BF16)
            lqT = sb2("lqT", [128, 512])
            lkT = sb2("lkT", [128, 512])
            XC = [sb2("XC%d" % m, [128, 515]) for m in range(3)]
            cacc3 = [sb2("cacc%d" % m, [128, 512]) for m in range(3)]
            csil = [sb2("csil%d" % m, [128, 512]) for m in range(2)]
            csq = sb2("csq", [128, 512], BF16)
            crn1 = sb2("crn1", [128, 512])
            crn2 = sb2("crn2", [128, 512])
            dT = [sb2("dT%d" % m, [128, 512], BF16) for m in range(3)]
            llrT = sb2("llrT", [17, 512])
            vaug = sb2("vaug", [128, 4, 192], BF16)
            sigo = sb2("sigo", [128, 4, 128], BF16)
            sm = sb2("sm", [128, 4, 4])
            km = sb2("km", [128, 4, 128])
            kl = sb2("kl", [128, 4, 128])
            vl = sb2("vl", [128, 4, 128], BF16)
            silr = sb2("silr", [128, 4, 128], BF16)
            silz = sb2("silz", [128, 4, 128], BF16)
            gt = sb2("gt", [128, 8, 4])
            LFB = sb2("LFB", [128, 128])
            LFBd = sb2("LFBd", [128, 128])
            coltm = sb2("coltm", [128, 4])
            coltd = sb2("coltd", [128, 4])
            DmT1 = sb2("DmT", [128, 128])
            EbM1 = sb2("EbM", [128, 128])
            DmT = [DmT1] * 4
            EbM = [EbM1] * 4
            ebl = sb2("ebl", [128, 12])
            PTm = [sb2("PTm%d" % j, [128, 128], BF16) for j in range(4)]
            qtm = [sb2("qtm%d" % j, [128, 128], BF16) for j in range(4)]
            kwm = [sb2("kwm%d" % j, [128, 128], BF16) for j in range(4)]
            GaT1 = sb2("GaT", [128, 128])
            GaT = [GaT1] * 4
            GaS = sb2("GaS", [128, 128])
            EbD1 = sb2("EbD", [128, 128])
            EbD = [EbD1] * 4
            Abar = sb2("Abar", [128, 4, 128], BF16)
            AbarT = sb2("AbarT", [128, 4, 128], BF16)
            QKm = [sb2("QKm%d" % j, [128, 128], BF16) for j in range(4)]
            qtd = [sb2("qtd%d" % j, [128, 128], BF16) for j in range(4)]
            khat = [sb2("khat%d" % j, [128, 128], BF16) for j in range(4)]
            kwd = [sb2("kwd%d" % j, [128, 128], BF16) for j in range(4)]
            vd = [sb2("vd%d" % j, [128, 128], BF16) for j in range(4)]
            NUl = sb2("NUl", [128, 4, 128], BF16)
            NLl = sb2("NLl", [128, 4, 128], BF16)
            Rm = sb2("Rm", [128, 4, 128], BF16)
            RTm = sb2("RTm", [128, 4, 128], BF16)
            Ysb = sb2("Ysb", [128, 4, 128], BF16)
            Ypsb = sb2("Ypsb", [128, 4, 128], BF16)
            nW0T = [sb2("nW0T%d" % j, [128, 128], BF16) for j in range(4)]
            vnew = sb2("vnew", [128, 128], BF16)
            e4 = sb2("e4", [128, 128])
            spl = sb2("spl", [128, 128])
            E1a = sb2("E1", [128, 128])
            E1 = [E1a] * 4
            E2 = sb2("E2", [128, 128])
            E3 = sb2("E3", [128, 128])
            qtl = [sb2("qtl%d" % j, [128, 128], BF16) for j in range(4)]
            ktlT = [sb2("ktlT%d" % j, [128, 128], BF16) for j in range(4)]
            ktl = [sb2("ktl%d" % j, [128, 128], BF16) for j in range(4)]
            ATl = [sb2("ATl%d" % j, [128, 128], BF16) for j in range(4)]
            NDm = sb2("NDm", [128, 4, 160])
            Ol = sb2("Ol", [128, 4, 128])
            Od = sb2("Od", [128, 4, 128])
            hg = sb2("hg", [128, 4, 128])
            junk = e4
            pre_o = sb2("pre_o", [128, 4, 128], BF16)
            brT1 = sb2("brT0", [128, 3, 512], BF16)
            brT = [brT1, brT1]
            Cn32 = sb2("Cn32", [128, 129])
            Cnb = sb2("Cnb", [128, 192], BF16)
            Sl32 = sb2("Sl32", [128, 128])
            Slb = sb2("Slb", [128, 128], BF16)
            Sd32 = sb2("Sd32", [128, 128])
            Sdb = sb2("Sdb", [128, 128], BF16)
            stmp = sb2("stmp", [128, 128])
            post = sb2("post", [128, 16])

            memset("dve", Cn32[:, :], 0.0)
            memset("dve", Cnb[:, :], 0.0)
            memset("dve", Sl32[:, :], 0.0)
            memset("dve", Slb[:, :], 0.0)
            memset("dve", Sd32[:, :], 0.0)
            memset("dve", Sdb[:, :], 0.0)
            memset("dve", vaug[:, :, :], 1.0)
            memset("dve", llrT[:, :], 1.0)
            for m in range(3):
                memset("dve", XC[m][:, 0:3], 0.0)

            def load_ut(B):
                rr, lb = B // 4, B % 4
                dma("sp", ut[B % 2][:, :, :],
                    u_all[lb].rearrange("(r kt p) t -> p r kt t", r=4, kt=8, p=128)[:, rr, :, :])

            load_ut(0)
            for B in range(min(NBLK, BLK_LIMIT)):
                if B + 1 < min(NBLK, BLK_LIMIT):
                    load_ut(B + 1)
                U = ut[B % 2]
                S.section(1)
                for g in range(7):
                    pf = PF[g % 2]
                    for kt in range(8):
                        mm(pf[:, :], WF(kt, g), U[:, kt, :], start=(kt == 0), stop=(kt == 7))
                    if g == 0:
                        act(qTm[:, :], pf[:, :], AF.Copy, scale=QS)
                    elif g == 1:
                        cp("dve", kTm[:, :], pf[:, :])
                    elif g == 2:
                        act(lqT[:, :], pf[:, :], AF.Copy, scale=QS)
                    elif g == 3:
                        cp("dve", lkT[:, :], pf[:, :])
                    else:
                        m = g - 4
                        if m % 2 == 0:
                            cp("act", XC[m][:, 3:515], pf[:, :])
                        else:
                            cp("dve", XC[m][:, 3:515], pf[:, :])
                for kt in range(8):
                    mm(PF[1][0:16, :], WLL(kt), U[:, kt, :], start=(kt == 0), stop=(kt == 7))
                cp("dve", llrT[0:16, :], PF[1][0:16, :])

                S.section(2)
                for m in range(3):
                    cw = lambda j, m=m: pcol(CW + m * 4 + j)
                    ca = cacc3[m]
                    tsc("dve", ca[:, :], XC[m][:, 3:515], cw(3), ALU.mult)
                    for j in (2, 1, 0):
                        stt(ca[:, :], XC[m][:, j:j + 512], cw(j), ca[:, :], ALU.mult, ALU.add)
                    cp("pool", XC[m][:, 0:3], XC[m][:, 512:515])
                act(csil[0][:, :], cacc3[0][:, :], AF.Silu)
                act(csil[1][:, :], cacc3[1][:, :], AF.Silu)
                act(dT[2][:, :], cacc3[2][:, :], AF.Silu)
                for m in range(2):
                    act(csq[:, :], csil[m][:, :], AF.Square)
                    mm(PF[m][:, :], oneb[:, :], csq[:, :])
                    act(crn1[:, :], PF[m][:, :], AF.Ln, bias=EPS)
                    act(crn2[:, :], crn1[:, :], AF.Exp, scale=-0.5)
                    if m == 0:
                        stt(dT[0][:, :], csil[0][:, :], QS, crn2[:, :], ALU.mult, ALU.mult)
                    else:
                        tt("dve", dT[1][:, :], csil[1][:, :], crn2[:, :], ALU.mult)

                S.section(3)
                for j in range(4):
                    tk = slice(j * 128, (j + 1) * 128)
                    for kt in range(8):
                        mm(PT1[:, 0:WH_T1], U[:, kt, tk], WT1(kt), start=(kt == 0), stop=(kt == 7))
                    for kt in range(8):
                        mm(PT2[:, :], U[:, kt, tk], WT2(kt), start=(kt == 0), stop=(kt == 7))
                    S.section(3.1)
                    cp("act", km[:, j, :], PT1[:, 0:128])
                    S.section(3.2)
                    cp("dve", vaug[:, j, 0:128], PT1[:, 128:256])
                    S.section(3.3)
                    cp("dve", sigo[:, j, :], PT1[:, 256:384])
                    S.section(3.4)
                    cp("dve", sm[:, j, :], PT1[:, 384:388])
                    S.section(3.5)
                    cp("act", kl[:, j, :], PT2[:, 0:128])
                    cp("dve", vl[:, j, :], PT2[:, 128:256])
                    S.section(3.6)
                    cp("act", silr[:, j, :], PT2[:, 256:384])
                    cp("dve", silz[:, j, :], PT2[:, 384:512])
                    S.section(3)
                act(silr[:, :, :], silr[:, :, :], AF.Silu)
                act(silz[:, :, :], silz[:, :, :], AF.Silu)
                act(sigo[:, :, :], sigo[:, :, :], AF.Sigmoid)

                S.section(4)
                act(gt[:, 4, :], sm[:, :, 1], AF.Exp, bias=drv[:, 0:1], scale=-1.0)
                act(gt[:, 0, :], gt[:, 4, :], AF.Ln, bias=1.0)
                tsc("dve", gt[:, 1, :], sm[:, :, 0], pcol(0), ALU.add)
                act(gt[:, 5, :], sm[:, :, 3], AF.Exp, bias=pcol(3))
                act(gt[:, 6, :], gt[:, 5, :], AF.Ln, bias=1.0)
                tsc("dve", gt[:, 2, :], gt[:, 6, :], drv[:, 1:2], ALU.mult)
                act(gt[:, 7, :], sm[:, :, 2], AF.Exp, scale=-1.0)
                tsc("dve", gt[:, 7, :], gt[:, 7, :], 1.0, ALU.add)
                recip(gt[:, 3, :], gt[:, 7, :])

                def interleave(gens):
                    gens = list(gens)
                    while gens:
                        for g_ in list(gens):
                            try:
                                next(g_)
                            except StopIteration:
                                gens.remove(g_)

                def dec_mlstm():
                    for j in range(4):
                        tk = slice(j * 128, (j + 1) * 128)
                        nlc = gt[:, 0, j:j + 1]
                        mm(PM[:, 0:1], NU, nlc)
                        tsc("dve", LFB[:, :], ONE32, nlc, ALU.mult)
                        yield
                        mm(PM[:, 128:256], LFB[:, :], NU)
                        mm(PM[:, 256:384], LFB[:, :], NU, start=True, stop=False)
                        mm(PM[:, 256:384], ID32, NEGI, start=False, stop=True)
                        mm(PM[:, 384:512], kTm[:, tk], qTm[:, tk])
                        yield
                        tt("dve", coltm[:, 0:1], gt[:, 1, j:j + 1], PM[:, 0:1], ALU.subtract)
                        yield
                        act(DmT[j][:, :], PM[:, 256:384], AF.Exp, bias=coltm[:, 0:1])
                        yield
                        act(EbM[j][:, :], PM[:, 128:256], AF.Exp)
                        yield
                        tt("dve", PTm[j][:, :], PM[:, 384:512], DmT[j][:, :], ALU.mult)
                        yield
                        cp("pool", ebl[:, j:j + 1], EbM[j][:, 127:128])
                        tt("pool", qtm[j][:, :], qTm[:, tk], EbM[j][:, :], ALU.mult)
                        yield
                        tsc("pool", kwm[j][:, :], km[:, j, :], DmT[j][:, 127:128], ALU.mult, 0.0, ALU.add)
                        yield

                def dec_gdn():
                    for j in range(4):
                        tk = slice(j * 128, (j + 1) * 128)
                        gsc = gt[:, 2, j:j + 1]
                        mm(PN[:, 0:1], NU, gsc)
                        tsc("dve", LFBd[:, :], ONE32, gsc, ALU.mult)
                        yield
                        mm(PN[:, 128:256], LFBd[:, :], NU)
                        mm(PN[:, 256:384], LFBd[:, :], NU, start=True, stop=False)
                        mm(PN[:, 256:384], ID32, NEGI, start=False, stop=True)
                        mm(PN[:, 384:512], LFBd[:, :], NU, start=True, stop=False)
                        mm(PN[:, 384:512], ID32, NEGS, start=False, stop=True)
                        mm(PT1[:, 0:128], dT[1][:, tk], dT[1][:, tk])
                        mm(PT1[:, 128:256], dT[1][:, tk], dT[0][:, tk])
                        tr(PB[:, 0:128], dT[1][:, tk], idb[:, :])
                        tr(PB[:, 128:256], dT[2][:, tk], idb[:, :])
                        yield
                        tsc("dve", coltd[:, 1:2], PN[:, 0:1], -1.0, ALU.mult)
                        yield
                        act(coltd[:, 2:3], PN[:, 0:1], AF.Exp)
                        yield
                        act(GaT[j][:, :], PN[:, 256:384], AF.Exp, bias=coltd[:, 1:2])
                        yield
                        act(GaS[:, :], PN[:, 384:512], AF.Exp, bias=coltd[:, 1:2])
                        yield
                        act(EbD[j][:, :], PN[:, 128:256], AF.Exp)
                        yield
                        cp("act", vd[j][:, :], PB[:, 128:256])
                        yield
                        stt(Abar[:, j, :], PT1[:, 0:128], gt[:, 3, j:j + 1], GaS[:, :], ALU.mult, ALU.mult)
                        yield
                        tr(PB[:, 256:384], Abar[:, j, :], idb[:, :])
                        tt("dve", QKm[j][:, :], PT1[:, 128:256], GaT[j][:, :], ALU.mult)
                        yield
                        cp("pool", ebl[:, 4 + j:5 + j], EbD[j][:, 127:128])
                        tt("pool", qtd[j][:, :], dT[0][:, tk], EbD[j][:, :], ALU.mult)
                        yield
                        tsc("dve", khat[j][:, :], PB[:, 0:128], coltd[:, 2:3], ALU.mult)
                        yield
                        tsc("dve", kwd[j][:, :], PB[:, 0:128], GaT[j][:, 127:128], ALU.mult)
                        yield
                        cp("act", AbarT[:, j, :], PB[:, 256:384])
                        yield

                def dec_gla():
                    for j in range(4):
                        tk = slice(j * 128, (j + 1) * 128)
                        mm(PO[:, 0:128], llrT[0:17, tk], wlr[0:17, :])
                        yield
                        act(e4[:, :], PO[:, 0:128], AF.Exp, scale=-1.0)
                        yield
                        act(spl[:, :], e4[:, :], AF.Ln, bias=1.0)
                        yield
                        mm(PO[:, 128:256], NU, spl[:, :])
                        mm(PO[:, 256:384], spl[:, :], NU)
                        yield
                        act(E1[j][:, :], PO[:, 256:384], AF.Exp, scale=1.0 / 16)
                        yield
                        act(E2[:, :], PO[:, 256:384], AF.Exp, scale=-1.0 / 16)
                        yield
                        act(E3[:, :], PO[:, 128:256], AF.Exp, scale=-1.0 / 16)
                        yield
                        cp("pool", ebl[:, 8 + j:9 + j], E1[j][:, 127:128])
                        tt("pool", qtl[j][:, :], lqT[:, tk], E1[j][:, :], ALU.mult)
                        yield
                        tt("pool", ktlT[j][:, :], lkT[:, tk], E2[:, :], ALU.mult)
                        yield
                        tt("dve", ktl[j][:, :], kl[:, j, :], E3[:, :], ALU.mult)
                        yield
                        mm(PO[:, 384:512], ktlT[j][:, :], qtl[j][:, :])
                        yield
                        tt("dve", ATl[j][:, :], PO[:, 384:512], UM, ALU.mult)
                        yield

                S.section(5)
                interleave([dec_mlstm(), dec_gdn(), dec_gla()])

                S.section(8)
                def lvmask(i):
                    tt("pool", NUl[:, :, :], Abar[:, :, :], lmu[:, i:i + 1, :].to_broadcast([128, 4, 128]), ALU.mult)
                    tt("pool", NLl[:, :, :], AbarT[:, :, :], lml[:, i:i + 1, :].to_broadcast([128, 4, 128]), ALU.mult)
                lvmask(0)
                idb4 = idb[:, :].unsqueeze(1).to_broadcast([128, 4, 128])
                tt("dve", Rm[:, :, :], idb4, NUl[:, :, :], ALU.subtract)
                tt("dve", RTm[:, :, :], idb4, NLl[:, :, :], ALU.subtract)
                for i in range(1, 7):
                    lvmask(i)
                    last = (i == 6)
                    for j in range(4):
                        mm(PM[:, j * 128:(j + 1) * 128], NLl[:, j, :], Rm[:, j, :])
                    cp("act", Ysb[:, :, :], PM[:, :].rearrange("p (a b) -> p a b", a=4))
                    if not last:
                        for j in range(4):
                            mm(PN[:, j * 128:(j + 1) * 128], NUl[:, j, :], RTm[:, j, :])
                        cp("dve", Ypsb[:, :, :], PN[:, :].rearrange("p (a b) -> p a b", a=4))
                    for j in range(4):
                        mm(PO[:, j * 128:(j + 1) * 128], RTm[:, j, :], Ysb[:, j, :])
                    if not last:
                        for j in range(4):
                            mm(PT1[:, j * 128:(j + 1) * 128], Rm[:, j, :], Ypsb[:, j, :])
                    tt("dve", Rm[:, :, :], Rm[:, :, :], PO[:, :].rearrange("p (a b) -> p a b", a=4), ALU.subtract)
                    if not last:
                        tt("dve", RTm[:, :, :], RTm[:, :, :], PT1[:, :].rearrange("p (a b) -> p a b", a=4), ALU.subtract)
                for j in range(4):
                    mm(PN[:, j * 128:(j + 1) * 128], khat[j][:, :], Rm[:, j, :])
                    tsc("dve", nW0T[j][:, :], PN[:, j * 128:(j + 1) * 128], -1.0, ALU.mult)

                S.section(9)
                def rec_mlstm():
                    for j in range(4):
                        mm(PO[:, 0:129], PTm[j][:, :], vaug[:, j, 0:129], start=True, stop=False)
                        mm(PO[:, 0:129], qtm[j][:, :], Cnb[:, 0:129], start=False, stop=True)
                        mm(PM[:, 0:129], kwm[j][:, :], vaug[:, j, 0:129])
                        yield
                        stt(Cn32[:, :], Cn32[:, :], ebl[:, j:j + 1], PM[:, 0:129], ALU.mult, ALU.add)
                        yield
                        cp("pool", Cnb[:, 0:129], Cn32[:, :])
                        yield
                        cp("act", NDm[:, j, 0:129], PO[:, 0:129])
                        yield

                def rec_gla():
                    for j in range(4):
                        mm(PF[0][:, 0:128], ATl[j][:, :], vl[:, j, :], start=True, stop=False)
                        mm(PF[0][:, 0:128], qtl[j][:, :], Slb[:, :], start=False, stop=True)
                        mm(PF[1][:, 0:128], ktl[j][:, :], vl[:, j, :])
                        yield
                        act(stmp[:, :], PF[1][:, 0:128], AF.Identity, scale=ebl[:, 8 + j:9 + j])
                        yield
                        stt(Sl32[:, :], Sl32[:, :], ebl[:, 8 + j:9 + j], stmp[:, :], ALU.mult, ALU.add)
                        yield
                        cp("pool", Slb[:, :], Sl32[:, :])
                        yield
                        cp("act", Ol[:, j, :], PF[0][:, 0:128])
                        yield

                def rec_gdn():
                    for j in range(4):
                        mm(PN[:, 0:128], Rm[:, j, :], vd[j][:, :], start=True, stop=False)
                        mm(PN[:, 0:128], nW0T[j][:, :], Sdb[:, :], start=False, stop=True)
                        yield
                        tsc("dve", vnew[:, :], PN[:, 0:128], gt[:, 3, j:j + 1], ALU.mult)
                        yield
                        mm(PT1[:, 0:128], QKm[j][:, :], vnew[:, :], start=True, stop=False)
                        mm(PT1[:, 0:128], qtd[j][:, :], Sdb[:, :], start=False, stop=True)
                        mm(PN[:, 128:256], kwd[j][:, :], vnew[:, :])
                        yield
                        stt(Sd32[:, :], Sd32[:, :], ebl[:, 4 + j:5 + j], PN[:, 128:256], ALU.mult, ALU.add)
                        yield
                        cp("pool", Sdb[:, :], Sd32[:, :])
                        yield
                        cp("act", Od[:, j, :], PT1[:, 0:128])
                        yield

                interleave([rec_gdn(), rec_mlstm(), rec_gla()])


                S.section(10)
                bt = brT[B % 2]
                act(post[:, 0:4], NDm[:, :, 128], AF.Abs)
                tsc("dve", post[:, 0:4], post[:, 0:4], 1.0, ALU.max)
                recip(post[:, 4:8], post[:, 0:4])
                for j in range(4):
                    stt(hg[:, j, :], NDm[:, j, 0:128], post[:, 4 + j:5 + j], sigo[:, j, :], ALU.mult, ALU.mult)
                srcs = [(hg, None, G_HM), (Ol, silr, G_HL), (Od, silz, G_HD)]
                for n, (src, gate, gi) in enumerate(srcs):
                    for j in range(4):
                        S.op("act", lambda e, src=src, j=j: e.activation(
                            out=junk[:, :], in_=src[:, j, :], func=AF.Square, accum_out=post[:, 8 + j:9 + j]),
                            reads=[src], writes=[junk, post])
                    act(post[:, 12:16], post[:, 8:12], AF.Ln, bias=EPS, scale=1.0 / HD)
                    act(post[:, 12:16], post[:, 12:16], AF.Exp, scale=-0.5)
                    for j in range(4):
                        if gate is None:
                            tsc("dve", pre_o[:, j, :], src[:, j, :], post[:, 12 + j:13 + j], ALU.mult)
                        else:
                            stt(pre_o[:, j, :], src[:, j, :], post[:, 12 + j:13 + j], gate[:, j, :], ALU.mult, ALU.mult)
                        tr(PB[:, 512 + j * 128:512 + (j + 1) * 128], pre_o[:, j, :], idb[:, :])
                    act(bt[:, n, :], PB[:, 512:1024], AF.Identity, scale=pcol(gi))
                kk = B // 2
                dma("sp", br_loc[kk].rearrange("(n p) t -> p n t", p=128)[:, :, (B % 2) * 512:(B % 2 + 1) * 512],
                    bt[:, :, :])
                if B % 2 == 1:
                    S.cc(lambda e, kk=kk: e.collective_compute("AllGather", ALU.bypass, replica_groups=groups,
                                                               ins=[br_loc[kk]], outs=[br_all[kk]]),
                         reads=[br_loc[kk]], writes=[br_all[kk]])
                    if dbg and l == 0:
                        dma("sp", dbg_br[:, kk * 1024:(kk + 1) * 1024], br_loc[kk])
            S.section(0)
            S.barrier()
        if stop == "p2":
            break

        with ExitStack() as p3:
            def sb3(name, shape, dt=F32):
                return p3.enter_context(nc.sbuf_tensor("%s_l%d" % (name, l), list(shape), dt))
            HT = TOK // 2
            uT = sb3("uT3", [128, 8, HT], BF16)
            brs = sb3("brs", [128, 12, HT], BF16)
            mixT = sb3("mixT", [128, 8, HT], BF16)
            brq = [sb3("brq0", [128, 12, 256], BF16), sb3("brq1", [128, 12, 256], BF16)]
            wgu = [sb3("wgu0", [128, 36 * 128], BF16), sb3("wgu1", [128, 36 * 128], BF16)]
            wo = [sb3("wo0", [128, 1024], BF16), sb3("wo1", [128, 1024], BF16)]
            sgt = [sb3("sgt0", [128, 512]), sb3("sgt1", [128, 512])]
            macc = sb3("macc", [128, 512])
            it = 0
            for hf in range(2):
                t0 = hf * HT
                for k2 in range(2):
                    dma("sp", uT[:, :, k2 * 512:(k2 + 1) * 512],
                        u_loc[hf * 2 + k2].rearrange("(kt p) t -> p kt t", p=128))
                for tb in range(HT // 256):
                    for q in range(4):
                        bq = brq[it % 2]
                        it += 1
                        dma("sp", bq[:, :, :],
                            br_all[2 * q + hf].rearrange("(hn p) t -> p hn t", p=128)[:, :, tb * 256:(tb + 1) * 256])
                        dst = brs[:, :, tb * 256:(tb + 1) * 256]
                        if q == 0:
                            tsc("dve", dst, bq[:, :, :], msel[:, 0:1], ALU.mult)
                        else:
                            stt(dst, bq[:, :, :], msel[:, q:q + 1], dst, ALU.mult, ALU.add)
                ldw(wgu[0][:, :], wgu_d[l, 0])
                for d in range(8):
                    if d + 1 < 8:
                        ldw(wgu[(d + 1) % 2][:, :], wgu_d[l, d + 1])
                    W = wgu[d % 2]
                    for tb in range(HT // 512):
                        tsl = slice(tb * 512, (tb + 1) * 512)
                        for n in range(3):
                            pg = PF[n % 2]
                            pu = PM if n % 2 == 0 else PN
                            for kt in range(8):
                                c0 = (n * 12 + kt) * 128
                                mm(pg[:, :], W[:, c0:c0 + 128], uT[:, kt, tsl], start=(kt == 0), stop=(kt == 7))
                            for h in range(4):
                                c0 = (n * 12 + 8 + h) * 128
                                mm(pu[:, :], W[:, c0:c0 + 128], brs[:, h * 3 + n, tsl], start=(h == 0), stop=(h == 3))
                            sg = sgt[n % 2]
                            act(sg[:, :], pg[:, :], AF.Sigmoid)
                            if n == 0:
                                tt("dve", macc[:, :], sg[:, :], pu[:, :], ALU.mult)
                            elif n == 1:
                                tt("dve", sg[:, :], sg[:, :], pu[:, :], ALU.mult)
                                tt("pool", macc[:, :], macc[:, :], sg[:, :], ALU.add)
                            else:
                                tt("dve", sg[:, :], sg[:, :], pu[:, :], ALU.mult)
                                tt("dve", mixT[:, d, tsl], macc[:, :], sg[:, :], ALU.add)
                ldw(wo[0][:, :], wo_d[l, 0])
                for d in range(8):
                    if d + 1 < 8:
                        ldw(wo[(d + 1) % 2][:, :], wo_d[l, d + 1])
                    W = wo[d % 2]
                    for tb in range(HT // 512):
                        tsl = slice(tb * 512, (tb + 1) * 512)
                        xsl = slice(t0 + tb * 512, t0 + (tb + 1) * 512)
                        pq = PT1 if tb % 2 == 0 else PT2
                        for kt in range(8):
                            mm(pq[:, :], W[:, kt * 128:(kt + 1) * 128], mixT[:, kt, tsl], start=(kt == 0), stop=(kt == 7))
                        tt("dve", xT[:, d, xsl], xT[:, d, xsl], pq[:, :], ALU.add)
            S.barrier()

        with ExitStack() as p4:
            def sb4(name, shape, dt=F32):
                return p4.enter_context(nc.sbuf_tensor("%s_l%d" % (name, l), list(shape), dt))
            u2 = sb4("u2T", [128, 8, TOK], BF16)
            sq = sb4("p4sq", [128, 8, 512], BF16)
            rs1 = sb4("p4rs1", [128, 512])
            rs2 = sb4("p4rs2", [128, 512])
            hT = sb4("hT", [128, 8, TOK], BF16)
            rl = [sb4("rl0", [128, 512], BF16), sb4("rl1", [128, 512], BF16)]
            w1 = [sb4("w1_%d" % i, [128, 1024], BF16) for i in range(3)]
            w2 = [sb4("w2_%d" % i, [128, 1024], BF16) for i in range(3)]
            for blk in range(4):
                rmsnorm_block(blk, G_MLP, prm, u2, sq, rs1, rs2, out_sl=slice(blk * 512, (blk + 1) * 512))
            cnt = 0
            for c in range(4):
                ldw(w1[0][:, :], w1_d[l, c * 8])
                for f in range(8):
                    if f + 1 < 8:
                        ldw(w1[(f + 1) % 3][:, :], w1_d[l, c * 8 + f + 1])
                    W = w1[f % 3]
                    for tb in range(4):
                        tsl = slice(tb * 512, (tb + 1) * 512)
                        pq = PF[cnt % 2]
                        r_ = rl[cnt % 2]
                        cnt += 1
                        for kt in range(8):
                            mm(pq[:, :], W[:, kt * 128:(kt + 1) * 128], u2[:, kt, tsl], start=(kt == 0), stop=(kt == 7))
                        act(r_[:, :], pq[:, :], AF.Relu)
                        tt("pool", hT[:, f, tsl], r_[:, :], r_[:, :], ALU.mult)
                ldw(w2[0][:, :], w2_d[l, c * 8])
                for d in range(8):
                    if d + 1 < 8:
                        ldw(w2[(d + 1) % 3][:, :], w2_d[l, c * 8 + d + 1])
                    W = w2[d % 3]
                    for tb in range(4):
                        tsl = slice(tb * 512, (tb + 1) * 512)
                        pq = PM if (tb % 2 == 0) else PN
                        for ft in range(8):
                            mm(pq[:, :], W[:, ft * 128:(ft + 1) * 128], hT[:, ft, tsl], start=(ft == 0), stop=(ft == 7))
                        tt("dve", xT[:, d, tsl], xT[:, d, tsl], pq[:, :], ALU.add)
            S.barrier()

    with ExitStack() as p5:
        def sb5(name, shape, dt=F32):
            return p5.enter_context(nc.sbuf_tensor(name, list(shape), dt))
        outv = out_d.rearrange("(kt p) t -> p kt t", p=128)
        if do_final:
            sq = sb5("p5sq", [128, 8, 512], BF16)
            rs1 = sb5("p5rs1", [128, 512])
            rs2 = sb5("p5rs2", [128, 512])
            ob = [sb5("p5o0", [128, 8, 512]), sb5("p5o1", [128, 8, 512])]
            for blk in range(4):
                rmsnorm_block(blk, 0, gfin, ob[blk % 2], sq, rs1, rs2)
                dma("sp", outv[:, :, blk * 512:(blk + 1) * 512], ob[blk % 2][:, :, :])
        else:
            dma("sp", outv, xT[:, :, :])
        fin = [out_d] + ([dbg_br] if dbg else [])
        S.final_wait("sp", fin)
        S.emit()
    es.close()
    return nc


def _tile_kxm(w, mt):
    K, M = w.shape
    a = w.reshape(K // 128, 128, M // mt, mt)
    return np.ascontiguousarray(a.transpose(2, 1, 0, 3))


def prep_shared(inp, layers):
    wgu, wo, w1, w2 = [], [], [], []
    for l in layers:
        w_in = inp["w_in"][l]
        g = _tile_kxm(np.ascontiguousarray(w_in[:, O_G:O_G + 3072]), 128)
        g = g.reshape(3, 8, 128, 8, 128)
        up = np.stack([_tile_kxm(inp["w_up"][l][n], 128) for n in range(3)])
        blk = np.concatenate([g, up], axis=3)
        blk = np.ascontiguousarray(blk.transpose(1, 2, 0, 3, 4)).reshape(8, 128, 36 * 128)
        wgu.append(blk)
        wo.append(_tile_kxm(inp["w_out"][l], 128).reshape(8, 128, 1024))
        w1.append(_tile_kxm(inp["w_mlp_in"][l], 128).reshape(32, 128, 1024))
        m2 = inp["w_mlp_out"][l].reshape(4, 8, 128, 8, 128)
        w2.append(np.ascontiguousarray(m2.transpose(0, 3, 2, 1, 4)).reshape(32, 128, 1024))
    return {"wgu": np.stack(wgu), "wo": np.stack(wo), "w1": np.stack(w1), "w2": np.stack(w2)}


def prep_core(inp, layers, c):
    h = c % 4
    hs = slice(h * 128, (h + 1) * 128)
    wh, wlr, prm = [], [], []
    for l in layers:
        w = inp["w_in"][l]
        def col(o):
            return w[:, o + h * 128:o + (h + 1) * 128]
        def one(o):
            return w[:, o + h:o + h + 1]
        cat = np.concatenate([col(O_MQ), col(O_MK), col(O_LQ), col(O_LK), col(O_DQ), col(O_DK), col(O_DV),
                              col(O_MK), col(O_MV), col(O_MO), one(O_MI), one(O_MF), one(O_DB), one(O_DA),
                              col(O_LK), col(O_LV), col(O_LR), col(O_DZ),
                              w[:, O_LLR:O_LLR + 16]], axis=1)
        a = cat.reshape(8, 128, WH).transpose(1, 0, 2)
        wh.append(np.ascontiguousarray(a).reshape(128, 8 * WH))
        wlr.append(np.concatenate([inp["w_gla_lr"][l][:, hs], inp["b_gla"][l][None, hs]], axis=0))
        p = np.zeros((128, NPRM), np.float32)
        p[:, 0] = inp["b_if"][l][h]
        p[:, 1] = inp["b_if"][l][4 + h]
        p[:, 2] = inp["a_log"][l][h]
        p[:, 3] = inp["dt_bias"][l][h]
        p[:, 4:12] = inp["g_norm_mix"][l].reshape(8, 128).T
        p[:, 12:20] = inp["g_norm_mlp"][l].reshape(8, 128).T
        p[:, 20] = inp["g_head_mlstm"][l][hs]
        p[:, 21] = inp["g_head_gla"][l][hs]
        p[:, 22] = inp["g_head_gdn"][l][hs]
        for m in range(3):
            for j in range(4):
                p[:, 23 + m * 4 + j] = inp["conv_gdn"][l][j, m * 512 + h * 128:m * 512 + (h + 1) * 128]
        prm.append(p)
    ms = np.zeros((128, 4), np.float32)
    ms[:, h] = 1.0
    return {"wh": np.stack(wh), "wlr": np.stack(wlr).astype(np.float32), "prm": np.stack(prm), "msel": ms,
            "gfin": np.ascontiguousarray(inp["g_final"].reshape(8, 128).T)}


_PROG = {}


STOP = None
SEC_LIMIT = 1000
BLK_LIMIT = 1000


def _get_prog(nl, do_final, dbg=False):
    k = (nl, do_final, dbg, STOP)
    if k not in _PROG:
        _PROG[k] = build_program(nl, do_final, dbg, STOP)
    return _PROG[k]


def _run(inp, x_cores, layers, do_final, dbg=False):
    nc = _get_prog(len(layers), do_final, dbg)
    shared = prep_shared(inp, layers)
    cst = make_consts()
    in_maps = []
    for c in range(NCORES):
        m = {"xT": x_cores[c], "cst": cst}
        m.update(shared)
        m.update(prep_core(inp, layers, c))
        in_maps.append(m)
    res = run_bass_kernel_spmd(nc, in_maps, core_ids=list(range(NCORES)))
    return res


def kernel(**inputs):
    inp = {k: np.asarray(v) for k, v in inputs.items()}
    x = inp["x"]
    x_cores = []
    for c in range(NCORES):
        b, r = c // 4, c % 4
        x_cores.append(np.ascontiguousarray(x[b, r * TOK:(r + 1) * TOK, :].T))
    if FUSED:
        res = _run(inp, x_cores, [0, 1, 2, 3], True)
        outs = [res.results[c]["outT"] for c in range(NCORES)]
    else:
        for l in range(4):
            res = _run(inp, x_cores, [l], l == 3)
            x_cores = [np.ascontiguousarray(res.results[c]["outT"]) for c in range(NCORES)]
        outs = x_cores
    out = np.empty_like(x)
    for c in range(NCORES):
        b, r = c // 4, c % 4
        out[b, r * TOK:(r + 1) * TOK, :] = outs[c].T
    return out
```

```python
import numpy as np
from contextlib import ExitStack
import concourse.bass as bass
import concourse.mybir as mybir
from concourse.bass_utils import run_bass_kernel_spmd

F32 = mybir.dt.float32
BF16 = mybir.dt.bfloat16
AF = mybir.ActivationFunctionType
ALU = mybir.AluOpType

NCORES = 8
D = 1024
SEQ = 8192
TOK = 2048
NBLK = SEQ // 512
EPS = 1e-6
HD = 128
QS = HD ** -0.5
NPRM = 36
NEGV = -30000.0
FUSED = True

O_MQ, O_MK, O_MV, O_MO, O_MI, O_MF = 0, 512, 1024, 1536, 2048, 2052
O_LQ, O_LK, O_LV, O_LR, O_LLR = 2056, 2568, 3080, 3592, 4104
O_DQ, O_DK, O_DV, O_DZ, O_DB, O_DA, O_G = 4120, 4632, 5144, 5656, 6168, 6172, 6176
WH_F, WH_T1, WH_T2, WH_L = 896, 388, 512, 16
WH = WH_F + WH_T1 + WH_T2 + WH_L

CST = {}
_off = 0
for _n, _w in [("NU", 128), ("UM", 128), ("NEGI", 128), ("NEGS", 128), ("ID", 128), ("ONE", 128),
               ("LMU", 7 * 128), ("LML", 7 * 128)]:
    CST[_n] = (_off, _w)
    _off += _w
NCST = _off


def make_consts():
    c = np.zeros((128, NCST), np.float32)
    s = np.arange(128)[:, None]
    t = np.arange(128)[None, :]
    def put(n, a):
        o, w = CST[n]
        c[:, o:o + w] = a.reshape(128, w)
    put("NU", -(s <= t).astype(np.float32))
    put("UM", (s <= t).astype(np.float32))
    put("NEGI", np.where(s <= t, 0.0, NEGV).astype(np.float32))
    put("NEGS", np.where(s < t, 0.0, NEGV).astype(np.float32))
    put("ID", np.eye(128, dtype=np.float32))
    put("ONE", np.ones((128, 128), np.float32))
    lmu = np.zeros((128, 7, 128), np.float32)
    for i in range(7):
        b = 1 << i
        m = ((s // (2 * b)) == (t // (2 * b))) & ((s % (2 * b)) < b) & ((t % (2 * b)) >= b)
        lmu[:, i, :] = m
    put("LMU", lmu)
    put("LML", np.ascontiguousarray(lmu.transpose(2, 1, 0)))
    return c


class Sched:
    CE = ("pe", "act", "dve", "pool")

    def __init__(self, nc, es, ndma=8):
        self.nc = nc
        self.prog = {e: [] for e in ("pe", "act", "dve", "pool", "sp")}
        self.esem = {e: es.enter_context(nc.semaphore("s_" + e)) for e in self.CE}
        self.ecnt = {e: 0 for e in self.CE}
        self.dsem = {q: [es.enter_context(nc.semaphore("d_%s%d" % (q, i))) for i in range(ndma)]
                     for q in ("sp", "pool")}
        self.dcnt = {q: [0] * ndma for q in ("sp", "pool")}
        self.drr = {"sp": 0, "pool": 0}
        self.ccsem = es.enter_context(nc.semaphore("s_cc"))
        self.cccnt = 0
        self.seen = {e: {} for e in self.prog}
        self.lastw = {}
        self.readers = {}
        self.nops = 0
        self.enabled = True

    @staticmethod
    def key(x):
        if isinstance(x, (str, tuple)):
            return x
        t = getattr(x, "tensor", None)
        return t.name if t is not None else x.name

    PSUM_NAMES = ("PF0", "PF1", "PT1", "PT2", "PM", "PN", "PO", "PB")

    def _deps(self, reads, writes, me=None):
        deps = []
        for r in reads:
            t = self.lastw.get(r)
            if t is not None:
                deps.append(t)
            if r in self.PSUM_NAMES:
                for k, tk in self.readers.get(r, {}).items():
                    if k != me:
                        deps.append(tk)
        for w in writes:
            t = self.lastw.get(w)
            if t is not None:
                deps.append(t)
            deps.extend(self.readers.get(w, {}).values())
        return deps

    def _commit(self, tok, reads, writes):
        for r in reads:
            d = self.readers.setdefault(r, {})
            k = tok[0]
            if k not in d or d[k][2] < tok[2]:
                d[k] = tok
        for w in writes:
            self.lastw[w] = tok
            self.readers[w] = {}

    def _add(self, eng, deps, fn, tok, inc):
        waits = {}
        for (k, sem, val, peng) in deps:
            if eng == "pe" and peng == "pe":
                continue
            if self.seen[eng].get(k, 0) >= val:
                continue
            if k not in waits or waits[k][1] < val:
                waits[k] = (sem, val)
        for k, (sem, val) in waits.items():
            self.seen[eng][k] = val
        self.prog[eng].append((list(waits.values()), fn, tok, inc))
        self.nops += 1

    def section(self, k):
        self.enabled = (k <= SEC_LIMIT)

    def op(self, eng, fn, reads=(), writes=()):
        if not self.enabled:
            return
        reads = [self.key(r) for r in reads]
        writes = [self.key(w) for w in writes]
        deps = self._deps(reads, writes, "e_" + eng)
        self.ecnt[eng] += 1
        tok = ("e_" + eng, self.esem[eng], self.ecnt[eng], eng)
        self._add(eng, deps, fn, tok, 1)
        self._commit(tok, reads, writes)

    def dma(self, q, fn, reads=(), writes=()):
        if not self.enabled:
            return
        reads = [self.key(r) for r in reads]
        writes = [self.key(w) for w in writes]
        deps = self._deps(reads, writes)
        i = self.drr[q]
        self.drr[q] = (i + 1) % len(self.dsem[q])
        k = "d_%s%d" % (q, i)
        sem = self.dsem[q][i]
        if self.dcnt[q][i] > 0:
            deps.append((k, sem, self.dcnt[q][i], "dma"))
        self.dcnt[q][i] += 16
        tok = (k, sem, self.dcnt[q][i], "dma")
        self._add(q, deps, fn, tok, 16)
        self._commit(tok, reads, writes)

    def cc(self, fn, reads=(), writes=()):
        if not self.enabled:
            return
        reads = [self.key(r) for r in reads]
        writes = [self.key(w) for w in writes]
        deps = self._deps(reads, writes)
        if self.cccnt > 0:
            deps.append(("cc", self.ccsem, self.cccnt, "cc"))
        self.cccnt += 1
        tok = ("cc", self.ccsem, self.cccnt, "cc")
        self._add("pool", deps, fn, tok, 1)
        self._commit(tok, reads, writes)

    def barrier(self):
        deps = []
        for e in self.CE:
            if self.ecnt[e] > 0:
                deps.append(("e_" + e, self.esem[e], self.ecnt[e], e + "_b"))
        for q in ("sp", "pool"):
            for i, sem in enumerate(self.dsem[q]):
                if self.dcnt[q][i] > 0:
                    deps.append(("d_%s%d" % (q, i), sem, self.dcnt[q][i], "dma"))
        if self.cccnt > 0:
            deps.append(("cc", self.ccsem, self.cccnt, "cc"))
        for e in self.prog:
            self._add(e, list(deps), None, None, 0)

    def final_wait(self, eng, keys):
        deps = []
        for k in keys:
            t = self.lastw.get(self.key(k))
            if t is not None:
                deps.append(t)
        self._add(eng, deps, None, None, 0)

    def emit(self):
        nc = self.nc
        with nc.Block() as block:
            def run(name, e):
                for waits, fn, tok, inc in self.prog[name]:
                    for sem, val in waits:
                        e.wait_ge(sem, val)
                    if fn is None:
                        continue
                    ins = fn(e)
                    ins.then_inc(tok[1], inc)

            @block.tensor
            def _(e):
                run("pe", e)

            @block.scalar
            def _(e):
                run("act", e)

            @block.vector
            def _(e):
                run("dve", e)

            @block.gpsimd
            def _(e):
                run("pool", e)

            @block.sync
            def _(e):
                run("sp", e)


def build_program(nl, do_final, dbg=False, stop=None):
    nc = bass.Bass("TRN2", target_bir_lowering=False)
    es = ExitStack()

    def din(name, shape, dt=F32):
        return nc.dram_tensor(name, list(shape), dt, kind="ExternalInput").ap()

    xT_d = din("xT", [D, TOK])
    wh_d = din("wh", [nl, 128, 8 * WH])
    wlr_d = din("wlr", [nl, 17, 128])
    prm_d = din("prm", [nl, 128, NPRM])
    gfin_d = din("gfin", [128, 8])
    msel_d = din("msel", [128, 4])
    cst_d = din("cst", [128, NCST])
    wgu_d = din("wgu", [nl, 8, 128, 36 * 128])
    wo_d = din("wo", [nl, 8, 128, 1024])
    w1_d = din("w1", [nl, 32, 128, 1024])
    w2_d = din("w2", [nl, 32, 128, 1024])
    out_d = nc.dram_tensor("outT", [D, TOK], F32, kind="ExternalOutput").ap()
    u_loc = [nc.dram_tensor("u_loc%d" % k, [D, 512], BF16, kind="Internal").ap() for k in range(4)]
    u_all = [nc.dram_tensor("u_all%d" % k, [4 * D, 512], BF16, kind="Internal").ap() for k in range(4)]
    br_loc = [nc.dram_tensor("br_loc%d" % k, [384, 1024], BF16, kind="Internal").ap() for k in range(8)]
    br_all = [nc.dram_tensor("br_all%d" % k, [4 * 384, 1024], BF16, kind="Internal").ap() for k in range(8)]
    if dbg:
        dbg_br = nc.dram_tensor("dbg_br", [384, SEQ], BF16, kind="ExternalOutput").ap()
    groups = [[0, 1, 2, 3], [4, 5, 6, 7]]

    S = Sched(nc, es)

    def sb(name, shape, dt=F32):
        return es.enter_context(nc.sbuf_tensor(name, list(shape), dt))

    def ps(name, shape, dt=F32):
        return es.enter_context(nc.psum_tensor(name, list(shape), dt))

    def rw(reads, writes):
        return [r for r in reads if r is not None and not isinstance(r, (int, float))], writes

    def mm(out, lhsT, rhs, start=True, stop=True):
        S.op("pe", lambda e: e.matmul(out, lhsT=lhsT, rhs=rhs, start=start, stop=stop),
             reads=[lhsT, rhs], writes=[out])

    def tr(out, in_, ident):
        S.op("pe", lambda e: e.transpose(out, in_, ident), reads=[in_, ident], writes=[out])

    def act(out, in_, func, bias=None, scale=None, eng="act"):
        kw = {}
        r = [in_]
        if bias is not None:
            kw["bias"] = bias
            if not isinstance(bias, (int, float)):
                r.append(bias)
        if scale is not None:
            kw["scale"] = scale
            if not isinstance(scale, (int, float)):
                r.append(scale)
        S.op("act", lambda e: e.activation(out=out, in_=in_, func=func, **kw), reads=r, writes=[out])

    def tt(eng, out, in0, in1, op):
        S.op(eng, lambda e: e.tensor_tensor(out=out, in0=in0, in1=in1, op=op), reads=[in0, in1], writes=[out])

    def tsc(eng, out, in0, s1, op0, s2=None, op1=None):
        r = [in0] + [s for s in (s1, s2) if s is not None and not isinstance(s, (int, float))]
        if op1 is None:
            S.op(eng, lambda e: e.tensor_scalar(out=out, in0=in0, scalar1=s1, scalar2=None, op0=op0),
                 reads=r, writes=[out])
        else:
            S.op(eng, lambda e: e.tensor_scalar(out=out, in0=in0, scalar1=s1, scalar2=s2, op0=op0, op1=op1),
                 reads=r, writes=[out])

    def stt(out, in0, scalar, in1, op0, op1):
        r = [in0, in1] + ([scalar] if not isinstance(scalar, (int, float)) else [])
        S.op("dve", lambda e: e.scalar_tensor_tensor(out=out, in0=in0, scalar=scalar, in1=in1, op0=op0, op1=op1),
             reads=r, writes=[out])

    def cp(eng, out, in_):
        if eng == "act":
            S.op("act", lambda e: e.activation(out=out, in_=in_, func=AF.Copy), reads=[in_], writes=[out])
        else:
            S.op(eng, lambda e: e.tensor_copy(out=out, in_=in_), reads=[in_], writes=[out])

    def recip(out, in_):
        S.op("dve", lambda e: e.reciprocal(out=out, in_=in_), reads=[in_], writes=[out])

    def memset(eng, ap, v):
        S.op(eng, lambda e: e.memset(ap, v), reads=[], writes=[ap])

    def dma(q, out, in_, **kw):
        S.dma(q, lambda e: e.dma_start(out=out, in_=in_, **kw), reads=[in_], writes=[out])

    def ldw(out, in_):
        S.dma("pool", lambda e: e.dma_start(out=out, in_=in_, max_dma_last_dim=8192), reads=[in_], writes=[out])

    xT = sb("xT_s", [128, 8, TOK])
    cst = sb("cst_s", [128, 6 * 128])
    idb = sb("idb", [128, 128], BF16)
    oneb = sb("oneb", [128, 128], BF16)
    lmu = sb("lmu", [128, 7, 128], BF16)
    lml = sb("lml", [128, 7, 128], BF16)
    prm = sb("prm_s", [128, NPRM])
    drv = sb("drv_s", [128, 4])
    gfin = sb("gfin_s", [128, 8])
    msel = sb("msel_s", [128, 4])

    def C(n):
        o, w = CST[n]
        return cst[:, o:o + w]

    NU, UM, NEGI, NEGS, ID32, ONE32 = C("NU"), C("UM"), C("NEGI"), C("NEGS"), C("ID"), C("ONE")

    PF = [ps("PF0", [128, 512]), ps("PF1", [128, 512])]
    PT1 = ps("PT1", [128, 512])
    PT2 = ps("PT2", [128, 512])
    PM = ps("PM", [128, 512])
    PN = ps("PN", [128, 512])
    PO = ps("PO", [128, 512])
    PB = ps("PB", [128, 1024], BF16)

    dma("sp", cst[:, :], cst_d[:, 0:6 * 128])
    dma("sp", gfin[:, :], gfin_d)
    dma("sp", msel[:, :], msel_d)
    dma("sp", xT[:, :, :], xT_d.rearrange("(kt p) t -> p kt t", p=128))
    cp("dve", idb[:, :], ID32)
    cp("dve", oneb[:, :], ONE32)
    o_, w_ = CST["LMU"]
    ldw(lmu[:, :, :], cst_d[:, o_:o_ + w_].rearrange("p (a b) -> p a b", a=7))
    o_, w_ = CST["LML"]
    ldw(lml[:, :, :], cst_d[:, o_:o_ + w_].rearrange("p (a b) -> p a b", a=7))

    G_MIX, G_MLP, G_HM, G_HL, G_HD, CW = 4, 12, 20, 21, 22, 23

    def pcol(i):
        return prm[:, i:i + 1]

    def rmsnorm_block(blk, gbase, gt, out_tile, sq, rs1, rs2, out_sl=slice(0, 512)):
        tsl = slice(blk * 512, (blk + 1) * 512)
        act(sq[:, :, :], xT[:, :, tsl], AF.Square)
        for kt in range(8):
            mm(PF[0][:, :], oneb[:, :], sq[:, kt, :], start=(kt == 0), stop=(kt == 7))
        act(rs1[:, :], PF[0][:, :], AF.Ln, bias=EPS, scale=1.0 / D)
        act(rs2[:, :], rs1[:, :], AF.Exp, scale=-0.5)
        for kt in range(8):
            stt(out_tile[:, kt, out_sl], xT[:, kt, tsl], gt[:, gbase + kt:gbase + kt + 1], rs2[:, :], ALU.mult, ALU.mult)

    for l in range(nl):
        if stop == "p0":
            break
        dma("sp", prm[:, :], prm_d[l])
        tsc("dve", drv[:, 0:1], prm[:, 1:2], -1.0, ALU.mult)
        act(drv[:, 1:2], prm[:, 2:3], AF.Exp)

        with ExitStack() as p1:
            def sb1(name, shape, dt=F32):
                return p1.enter_context(nc.sbuf_tensor("%s_l%d" % (name, l), list(shape), dt))
            sq = sb1("p1sq", [128, 8, 512], BF16)
            rs1 = sb1("p1rs1", [128, 512])
            rs2 = sb1("p1rs2", [128, 512])
            ub = [sb1("p1u0", [128, 8, 512], BF16), sb1("p1u1", [128, 8, 512], BF16)]
            for blk in range(4):
                rmsnorm_block(blk, G_MIX, prm, ub[blk % 2], sq, rs1, rs2)
                dma("sp", u_loc[blk].rearrange("(kt p) t -> p kt t", p=128), ub[blk % 2][:, :, :])
                S.cc(lambda e, blk=blk: e.collective_compute("AllGather", ALU.bypass, replica_groups=groups,
                                                             ins=[u_loc[blk]], outs=[u_all[blk]]),
                     reads=[u_loc[blk]], writes=[u_all[blk]])
            S.barrier()
        if stop == "p1":
            break

        with ExitStack() as p2:
            def sb2(name, shape, dt=F32):
                return p2.enter_context(nc.sbuf_tensor("%s_l%d" % (name, l), list(shape), dt))

            whs2 = sb2("whs", [128, 8 * WH], BF16)
            whs = whs2[:, :].rearrange("p (kt c) -> p kt c", kt=8)
            wlr = sb2("wlr_s", [17, 128])
            ldw(whs2[:, :], wh_d[l])
            dma("sp", wlr[:, :], wlr_d[l])
            WF = lambda kt, g: whs[:, kt, g * 128:(g + 1) * 128]
            WT1 = lambda kt: whs[:, kt, WH_F:WH_F + WH_T1]
            WT2 = lambda kt: whs[:, kt, WH_F + WH_T1:WH_F + WH_T1 + WH_T2]
            WLL = lambda kt: whs[:, kt, WH_F + WH_T1 + WH_T2:WH]

            ut = [sb2("ut0", [128, 8, 512], BF16), sb2("ut1", [128, 8, 512], BF16)]
            qTm = sb2("qTm", [128, 512], BF16)
            kTm = sb2("kTm", [128, 512], BF16)
            lqT = sb2("lqT", [128, 512])
            lkT = sb2("lkT", [128, 512])
            XC = [sb2("XC%d" % m, [128, 515]) for m in range(3)]
            cacc3 = [sb2("cacc%d" % m, [128, 512]) for m in range(3)]
            csil = [sb2("csil%d" % m, [128, 512]) for m in range(2)]
            csq = sb2("csq", [128, 512], BF16)
            crn1 = sb2("crn1", [128, 512])
            crn2 = sb2("crn2", [128, 512])
            dT = [sb2("dT%d" % m, [128, 512], BF16) for m in range(3)]
            llrT = sb2("llrT", [17, 512])
            vaug = sb2("vaug", [128, 4, 192], BF16)
            sigo = sb2("sigo", [128, 4, 128], BF16)
            sm = sb2("sm", [128, 4, 4])
            km = sb2("km", [128, 4, 128])
            kl = sb2("kl", [128, 4, 128])
            vl = sb2("vl", [128, 4, 128], BF16)
            silr = sb2("silr", [128, 4, 128], BF16)
            silz = sb2("silz", [128, 4, 128], BF16)
            gt = sb2("gt", [128, 8, 4])
            LFB = sb2("LFB", [128, 128])
            LFBd = sb2("LFBd", [128, 128])
            coltm = sb2("coltm", [128, 4])
            coltd = sb2("coltd", [128, 4])
            DmT1 = sb2("DmT", [128, 128])
            EbM1 = sb2("EbM", [128, 128])
            DmT = [DmT1] * 4
            EbM = [EbM1] * 4
            ebl = sb2("ebl", [128, 12])
            PTm = [sb2("PTm%d" % j, [128, 128], BF16) for j in range(4)]
            qtm = [sb2("qtm%d" % j, [128, 128], BF16) for j in range(4)]
            kwm = [sb2("kwm%d" % j, [128, 128], BF16) for j in range(4)]
            GaT1 = sb2("GaT", [128, 128])
            GaT = [GaT1] * 4
            GaS = sb2("GaS", [128, 128])
            EbD1 = sb2("EbD", [128, 128])
            EbD = [EbD1] * 4
            Abar = sb2("Abar", [128, 4, 128], BF16)
            AbarT = sb2("AbarT", [128, 4, 128], BF16)
            QKm = [sb2("QKm%d" % j, [128, 128], BF16) for j in range(4)]
            qtd = [sb2("qtd%d" % j, [128, 128], BF16) for j in range(4)]
            khat = [sb2("khat%d" % j, [128, 128], BF16) for j in range(4)]
            kwd = [sb2("kwd%d" % j, [128, 128], BF16) for j in range(4)]
            vd = [sb2("vd%d" % j, [128, 128], BF16) for j in range(4)]
            NUl = sb2("NUl", [128, 4, 128], BF16)
            NLl = sb2("NLl", [128, 4, 128], BF16)
            Rm = sb2("Rm", [128, 4, 128], BF16)
            RTm = sb2("RTm", [128, 4, 128], BF16)
            Ysb = sb2("Ysb", [128, 4, 128], BF16)
            Ypsb = sb2("Ypsb", [128, 4, 128], BF16)
            nW0T = [sb2("nW0T%d" % j, [128, 128], BF16) for j in range(4)]
            vnew = sb2("vnew", [128, 128], BF16)
            e4 = sb2("e4", [128, 128])
            spl = sb2("spl", [128, 128])
            E1a = sb2("E1", [128, 128])
            E1 = [E1a] * 4
            E2 = sb2("E2", [128, 128])
            E3 = sb2("E3", [128, 128])
            qtl = [sb2("qtl%d" % j, [128, 128], BF16) for j in range(4)]
            ktlT = [sb2("ktlT%d" % j, [128, 128], BF16) for j in range(4)]
            ktl = [sb2("ktl%d" % j, [128, 128], BF16) for j in range(4)]
            ATl = [sb2("ATl%d" % j, [128, 128], BF16) for j in range(4)]
            NDm = sb2("NDm", [128, 4, 160])
            Ol = sb2("Ol", [128, 4, 128])
            Od = sb2("Od", [128, 4, 128])
            hg = sb2("hg", [128, 4, 128])
            junk = e4
            pre_o = sb2("pre_o", [128, 4, 128], BF16)
            brT1 = sb2("brT0", [128, 3, 512], BF16)
            brT = [brT1, brT1]
            Cn32 = sb2("Cn32", [128, 129])
            Cnb = sb2("Cnb", [128, 192], BF16)
            Sl32 = sb2("Sl32", [128, 128])
            Slb = sb2("Slb", [128, 128], BF16)
            Sd32 = sb2("Sd32", [128, 128])
            Sdb = sb2("Sdb", [128, 128], BF16)
            stmp = sb2("stmp", [128, 128])
            post = sb2("post", [128, 16])

            memset("dve", Cn32[:, :], 0.0)
            memset("dve", Cnb[:, :], 0.0)
            memset("dve", Sl32[:, :], 0.0)
            memset("dve", Slb[:, :], 0.0)
            memset("dve", Sd32[:, :], 0.0)
            memset("dve", Sdb[:, :], 0.0)
            memset("dve", vaug[:, :, :], 1.0)
            memset("dve", llrT[:, :], 1.0)
            for m in range(3):
                memset("dve", XC[m][:, 0:3], 0.0)

            def load_ut(B):
                rr, lb = B // 4, B % 4
                dma("sp", ut[B % 2][:, :, :],
                    u_all[lb].rearrange("(r kt p) t -> p r kt t", r=4, kt=8, p=128)[:, rr, :, :])

            load_ut(0)
            for B in range(min(NBLK, BLK_LIMIT)):
                if B + 1 < min(NBLK, BLK_LIMIT):
                    load_ut(B + 1)
                U = ut[B % 2]
                S.section(1)
                for g in range(7):
                    pf = PF[g % 2]
                    for kt in range(8):
                        mm(pf[:, :], WF(kt, g), U[:, kt, :], start=(kt == 0), stop=(kt == 7))
                    if g == 0:
                        act(qTm[:, :], pf[:, :], AF.Copy, scale=QS)
                    elif g == 1:
                        cp("dve", kTm[:, :], pf[:, :])
                    elif g == 2:
                        act(lqT[:, :], pf[:, :], AF.Copy, scale=QS)
                    elif g == 3:
                        cp("dve", lkT[:, :], pf[:, :])
                    else:
                        m = g - 4
                        if m % 2 == 0:
                            cp("act", XC[m][:, 3:515], pf[:, :])
                        else:
                            cp("dve", XC[m][:, 3:515], pf[:, :])
                for kt in range(8):
                    mm(PF[1][0:16, :], WLL(kt), U[:, kt, :], start=(kt == 0), stop=(kt == 7))
                cp("dve", llrT[0:16, :], PF[1][0:16, :])

                S.section(2)
                for m in range(3):
                    cw = lambda j, m=m: pcol(CW + m * 4 + j)
                    ca = cacc3[m]
                    tsc("dve", ca[:, :], XC[m][:, 3:515], cw(3), ALU.mult)
                    for j in (2, 1, 0):
                        stt(ca[:, :], XC[m][:, j:j + 512], cw(j), ca[:, :], ALU.mult, ALU.add)
                    cp("pool", XC[m][:, 0:3], XC[m][:, 512:515])
                act(csil[0][:, :], cacc3[0][:, :], AF.Silu)
                act(csil[1][:, :], cacc3[1][:, :], AF.Silu)
                act(dT[2][:, :], cacc3[2][:, :], AF.Silu)
                for m in range(2):
                    act(csq[:, :], csil[m][:, :], AF.Square)
                    mm(PF[m][:, :], oneb[:, :], csq[:, :])
                    act(crn1[:, :], PF[m][:, :], AF.Ln, bias=EPS)
                    act(crn2[:, :], crn1[:, :], AF.Exp, scale=-0.5)
                    if m == 0:
                        stt(dT[0][:, :], csil[0][:, :], QS, crn2[:, :], ALU.mult, ALU.mult)
                    else:
                        tt("dve", dT[1][:, :], csil[1][:, :], crn2[:, :], ALU.mult)

                S.section(3)
                for j in range(4):
                    tk = slice(j * 128, (j + 1) * 128)
                    for kt in range(8):
                        mm(PT1[:, 0:WH_T1], U[:, kt, tk], WT1(kt), start=(kt == 0), stop=(kt == 7))
                    for kt in range(8):
                        mm(PT2[:, :], U[:, kt, tk], WT2(kt), start=(kt == 0), stop=(kt == 7))
                    S.section(3.1)
                    cp("dve", km[:, j, :], PT1[:, 0:128])
                    S.section(3.2)
                    cp("dve", vaug[:, j, 0:128], PT1[:, 128:256])
                    S.section(3.3)
                    cp("dve", sigo[:, j, :], PT1[:, 256:384])
                    S.section(3.4)
                    cp("dve", sm[:, j, :], PT1[:, 384:388])
                    S.section(3.5)
                    cp("act", kl[:, j, :], PT2[:, 0:128])
                    cp("act", vl[:, j, :], PT2[:, 128:256])
                    S.section(3.6)
                    cp("act", silr[:, j, :], PT2[:, 256:384])
                    cp("act", silz[:, j, :], PT2[:, 384:512])
                    S.section(3)
                act(silr[:, :, :], silr[:, :, :], AF.Silu)
                act(silz[:, :, :], silz[:, :, :], AF.Silu)
                act(sigo[:, :, :], sigo[:, :, :], AF.Sigmoid)

                S.section(4)
                act(gt[:, 4, :], sm[:, :, 1], AF.Exp, bias=drv[:, 0:1], scale=-1.0)
                act(gt[:, 0, :], gt[:, 4, :], AF.Ln, bias=1.0)
                tsc("dve", gt[:, 1, :], sm[:, :, 0], pcol(0), ALU.add)
                act(gt[:, 5, :], sm[:, :, 3], AF.Exp, bias=pcol(3))
                act(gt[:, 6, :], gt[:, 5, :], AF.Ln, bias=1.0)
                tsc("dve", gt[:, 2, :], gt[:, 6, :], drv[:, 1:2], ALU.mult)
                act(gt[:, 7, :], sm[:, :, 2], AF.Exp, scale=-1.0)
                tsc("dve", gt[:, 7, :], gt[:, 7, :], 1.0, ALU.add)
                recip(gt[:, 3, :], gt[:, 7, :])

                def interleave(gens):
                    gens = list(gens)
                    while gens:
                        for g_ in list(gens):
                            try:
                                next(g_)
                            except StopIteration:
                                gens.remove(g_)

                def dec_mlstm():
                    for j in range(4):
                        tk = slice(j * 128, (j + 1) * 128)
                        nlc = gt[:, 0, j:j + 1]
                        mm(PM[:, 0:1], NU, nlc)
                        tsc("dve", LFB[:, :], ONE32, nlc, ALU.mult)
                        yield
                        mm(PM[:, 128:256], LFB[:, :], NU)
                        mm(PM[:, 256:384], LFB[:, :], NU, start=True, stop=False)
                        mm(PM[:, 256:384], ID32, NEGI, start=False, stop=True)
                        mm(PM[:, 384:512], kTm[:, tk], qTm[:, tk])
                        yield
                        tt("dve", coltm[:, 0:1], gt[:, 1, j:j + 1], PM[:, 0:1], ALU.subtract)
                        yield
                        act(DmT[j][:, :], PM[:, 256:384], AF.Exp, bias=coltm[:, 0:1])
                        yield
                        act(EbM[j][:, :], PM[:, 128:256], AF.Exp)
                        yield
                        tt("dve", PTm[j][:, :], PM[:, 384:512], DmT[j][:, :], ALU.mult)
                        yield
                        cp("pool", ebl[:, j:j + 1], EbM[j][:, 127:128])
                        tt("pool", qtm[j][:, :], qTm[:, tk], EbM[j][:, :], ALU.mult)
                        yield
                        tsc("pool", kwm[j][:, :], km[:, j, :], DmT[j][:, 127:128], ALU.mult, 0.0, ALU.add)
                        yield

                def dec_gdn():
                    for j in range(4):
                        tk = slice(j * 128, (j + 1) * 128)
                        gsc = gt[:, 2, j:j + 1]
                        mm(PN[:, 0:1], NU, gsc)
                        tsc("dve", LFBd[:, :], ONE32, gsc, ALU.mult)
                        yield
                        mm(PN[:, 128:256], LFBd[:, :], NU)
                        mm(PN[:, 256:384], LFBd[:, :], NU, start=True, stop=False)
                        mm(PN[:, 256:384], ID32, NEGI, start=False, stop=True)
                        mm(PN[:, 384:512], LFBd[:, :], NU, start=True, stop=False)
                        mm(PN[:, 384:512], ID32, NEGS, start=False, stop=True)
                        mm(PT1[:, 0:128], dT[1][:, tk], dT[1][:, tk])
                        mm(PT1[:, 128:256], dT[1][:, tk], dT[0][:, tk])
                        tr(PB[:, 0:128], dT[1][:, tk], idb[:, :])
                        tr(PB[:, 128:256], dT[2][:, tk], idb[:, :])
                        yield
                        tsc("dve", coltd[:, 1:2], PN[:, 0:1], -1.0, ALU.mult)
                        yield
                        act(coltd[:, 2:3], PN[:, 0:1], AF.Exp)
                        yield
                        act(GaT[j][:, :], PN[:, 256:384], AF.Exp, bias=coltd[:, 1:2])
                        yield
                        act(GaS[:, :], PN[:, 384:512], AF.Exp, bias=coltd[:, 1:2])
                        yield
                        act(EbD[j][:, :], PN[:, 128:256], AF.Exp)
                        yield
                        cp("act", vd[j][:, :], PB[:, 128:256])
                        yield
                        stt(Abar[:, j, :], PT1[:, 0:128], gt[:, 3, j:j + 1], GaS[:, :], ALU.mult, ALU.mult)
                        yield
                        tr(PB[:, 256:384], Abar[:, j, :], idb[:, :])
                        tt("dve", QKm[j][:, :], PT1[:, 128:256], GaT[j][:, :], ALU.mult)
                        yield
                        cp("pool", ebl[:, 4 + j:5 + j], EbD[j][:, 127:128])
                        tt("pool", qtd[j][:, :], dT[0][:, tk], EbD[j][:, :], ALU.mult)
                        yield
                        tsc("dve", khat[j][:, :], PB[:, 0:128], coltd[:, 2:3], ALU.mult)
                        yield
                        tsc("dve", kwd[j][:, :], PB[:, 0:128], GaT[j][:, 127:128], ALU.mult)
                        yield
                        cp("act", AbarT[:, j, :], PB[:, 256:384])
                        yield

                def dec_gla():
                    for j in range(4):
                        tk = slice(j * 128, (j + 1) * 128)
                        mm(PO[:, 0:128], llrT[0:17, tk], wlr[0:17, :])
                        yield
                        act(e4[:, :], PO[:, 0:128], AF.Exp, scale=-1.0)
                        yield
                        act(spl[:, :], e4[:, :], AF.Ln, bias=1.0)
                        yield
                        mm(PO[:, 128:256], NU, spl[:, :])
                        mm(PO[:, 256:384], spl[:, :], NU)
                        yield
                        act(E1[j][:, :], PO[:, 256:384], AF.Exp, scale=1.0 / 16)
                        yield
                        act(E2[:, :], PO[:, 256:384], AF.Exp, scale=-1.0 / 16)
                        yield
                        act(E3[:, :], PO[:, 128:256], AF.Exp, scale=-1.0 / 16)
                        yield
                        cp("pool", ebl[:, 8 + j:9 + j], E1[j][:, 127:128])
                        tt("pool", qtl[j][:, :], lqT[:, tk], E1[j][:, :], ALU.mult)
                        yield
                        tt("pool", ktlT[j][:, :], lkT[:, tk], E2[:, :], ALU.mult)
                        yield
                        tt("dve", ktl[j][:, :], kl[:, j, :], E3[:, :], ALU.mult)
                        yield
                        mm(PO[:, 384:512], ktlT[j][:, :], qtl[j][:, :])
                        yield
                        tt("dve", ATl[j][:, :], PO[:, 384:512], UM, ALU.mult)
                        yield

                S.section(5)
                interleave([dec_mlstm(), dec_gdn(), dec_gla()])

                S.section(8)
                def lvmask(i):
                    tt("pool", NUl[:, :, :], Abar[:, :, :], lmu[:, i:i + 1, :].to_broadcast([128, 4, 128]), ALU.mult)
                    tt("pool", NLl[:, :, :], AbarT[:, :, :], lml[:, i:i + 1, :].to_broadcast([128, 4, 128]), ALU.mult)
                lvmask(0)
                idb4 = idb[:, :].unsqueeze(1).to_broadcast([128, 4, 128])
                tt("dve", Rm[:, :, :], idb4, NUl[:, :, :], ALU.subtract)
                tt("dve", RTm[:, :, :], idb4, NLl[:, :, :], ALU.subtract)
                for i in range(1, 7):
                    lvmask(i)
                    last = (i == 6)
                    for j in range(4):
                        mm(PM[:, j * 128:(j + 1) * 128], NLl[:, j, :], Rm[:, j, :])
                    cp("act", Ysb[:, :, :], PM[:, :].rearrange("p (a b) -> p a b", a=4))
                    if not last:
                        for j in range(4):
                            mm(PN[:, j * 128:(j + 1) * 128], NUl[:, j, :], RTm[:, j, :])
                        cp("dve", Ypsb[:, :, :], PN[:, :].rearrange("p (a b) -> p a b", a=4))
                    for j in range(4):
                        mm(PO[:, j * 128:(j + 1) * 128], RTm[:, j, :], Ysb[:, j, :])
                    if not last:
                        for j in range(4):
                            mm(PT1[:, j * 128:(j + 1) * 128], Rm[:, j, :], Ypsb[:, j, :])
                    tt("dve", Rm[:, :, :], Rm[:, :, :], PO[:, :].rearrange("p (a b) -> p a b", a=4), ALU.subtract)
                    if not last:
                        tt("dve", RTm[:, :, :], RTm[:, :, :], PT1[:, :].rearrange("p (a b) -> p a b", a=4), ALU.subtract)
                for j in range(4):
                    mm(PN[:, j * 128:(j + 1) * 128], khat[j][:, :], Rm[:, j, :])
                    tsc("dve", nW0T[j][:, :], PN[:, j * 128:(j + 1) * 128], -1.0, ALU.mult)

                S.section(9)
                def rec_mlstm():
                    for j in range(4):
                        mm(PO[:, 0:129], PTm[j][:, :], vaug[:, j, 0:129], start=True, stop=False)
                        mm(PO[:, 0:129], qtm[j][:, :], Cnb[:, 0:129], start=False, stop=True)
                        mm(PM[:, 0:129], kwm[j][:, :], vaug[:, j, 0:129])
                        yield
                        stt(Cn32[:, :], Cn32[:, :], ebl[:, j:j + 1], PM[:, 0:129], ALU.mult, ALU.add)
                        yield
                        cp("pool", Cnb[:, 0:129], Cn32[:, :])
                        yield
                        cp("act", NDm[:, j, 0:129], PO[:, 0:129])
                        yield

                def rec_gla():
                    for j in range(4):
                        mm(PF[0][:, 0:128], ATl[j][:, :], vl[:, j, :], start=True, stop=False)
                        mm(PF[0][:, 0:128], qtl[j][:, :], Slb[:, :], start=False, stop=True)
                        mm(PF[1][:, 0:128], ktl[j][:, :], vl[:, j, :])
                        yield
                        act(stmp[:, :], PF[1][:, 0:128], AF.Identity, scale=ebl[:, 8 + j:9 + j])
                        yield
                        stt(Sl32[:, :], Sl32[:, :], ebl[:, 8 + j:9 + j], stmp[:, :], ALU.mult, ALU.add)
                        yield
                        cp("pool", Slb[:, :], Sl32[:, :])
                        yield
                        cp("act", Ol[:, j, :], PF[0][:, 0:128])
                        yield

                def rec_gdn():
                    for j in range(4):
                        mm(PN[:, 0:128], Rm[:, j, :], vd[j][:, :], start=True, stop=False)
                        mm(PN[:, 0:128], nW0T[j][:, :], Sdb[:, :], start=False, stop=True)
                        yield
                        tsc("dve", vnew[:, :], PN[:, 0:128], gt[:, 3, j:j + 1], ALU.mult)
                        yield
                        mm(PT1[:, 0:128], QKm[j][:, :], vnew[:, :], start=True, stop=False)
                        mm(PT1[:, 0:128], qtd[j][:, :], Sdb[:, :], start=False, stop=True)
                        mm(PN[:, 128:256], kwd[j][:, :], vnew[:, :])
                        yield
                        stt(Sd32[:, :], Sd32[:, :], ebl[:, 4 + j:5 + j], PN[:, 128:256], ALU.mult, ALU.add)
                        yield
                        cp("pool", Sdb[:, :], Sd32[:, :])
                        yield
                        cp("act", Od[:, j, :], PT1[:, 0:128])
                        yield

                interleave([rec_gdn(), rec_mlstm(), rec_gla()])


                S.section(10)
                bt = brT[B % 2]
                act(post[:, 0:4], NDm[:, :, 128], AF.Abs)
                tsc("dve", post[:, 0:4], post[:, 0:4], 1.0, ALU.max)
                recip(post[:, 4:8], post[:, 0:4])
                for j in range(4):
                    stt(hg[:, j, :], NDm[:, j, 0:128], post[:, 4 + j:5 + j], sigo[:, j, :], ALU.mult, ALU.mult)
                srcs = [(hg, None, G_HM), (Ol, silr, G_HL), (Od, silz, G_HD)]
                for n, (src, gate, gi) in enumerate(srcs):
                    for j in range(4):
                        S.op("act", lambda e, src=src, j=j: e.activation(
                            out=junk[:, :], in_=src[:, j, :], func=AF.Square, accum_out=post[:, 8 + j:9 + j]),
                            reads=[src], writes=[junk, post])
                    act(post[:, 12:16], post[:, 8:12], AF.Ln, bias=EPS, scale=1.0 / HD)
                    act(post[:, 12:16], post[:, 12:16], AF.Exp, scale=-0.5)
                    for j in range(4):
                        if gate is None:
                            tsc("dve", pre_o[:, j, :], src[:, j, :], post[:, 12 + j:13 + j], ALU.mult)
                        else:
                            stt(pre_o[:, j, :], src[:, j, :], post[:, 12 + j:13 + j], gate[:, j, :], ALU.mult, ALU.mult)
                        tr(PB[:, 512 + j * 128:512 + (j + 1) * 128], pre_o[:, j, :], idb[:, :])
                    act(bt[:, n, :], PB[:, 512:1024], AF.Identity, scale=pcol(gi))
                kk = B // 2
                dma("sp", br_loc[kk].rearrange("(n p) t -> p n t", p=128)[:, :, (B % 2) * 512:(B % 2 + 1) * 512],
                    bt[:, :, :])
                if B % 2 == 1:
                    S.cc(lambda e, kk=kk: e.collective_compute("AllGather", ALU.bypass, replica_groups=groups,
                                                               ins=[br_loc[kk]], outs=[br_all[kk]]),
                         reads=[br_loc[kk]], writes=[br_all[kk]])
                    if dbg and l == 0:
                        dma("sp", dbg_br[:, kk * 1024:(kk + 1) * 1024], br_loc[kk])
            S.section(0)
            S.barrier()
        if stop == "p2":
            break

        with ExitStack() as p3:
            def sb3(name, shape, dt=F32):
                return p3.enter_context(nc.sbuf_tensor("%s_l%d" % (name, l), list(shape), dt))
            HT = TOK // 2
            uT = sb3("uT3", [128, 8, HT], BF16)
            brs = sb3("brs", [128, 12, HT], BF16)
            mixT = sb3("mixT", [128, 8, HT], BF16)
            brq = [sb3("brq0", [128, 12, 256], BF16), sb3("brq1", [128, 12, 256], BF16)]
            wgu = [sb3("wgu0", [128, 36 * 128], BF16), sb3("wgu1", [128, 36 * 128], BF16)]
            wo = [sb3("wo0", [128, 1024], BF16), sb3("wo1", [128, 1024], BF16)]
            sgt = [sb3("sgt0", [128, 512]), sb3("sgt1", [128, 512])]
            macc = sb3("macc", [128, 512])
            it = 0
            for hf in range(2):
                t0 = hf * HT
                for k2 in range(2):
                    dma("sp", uT[:, :, k2 * 512:(k2 + 1) * 512],
                        u_loc[hf * 2 + k2].rearrange("(kt p) t -> p kt t", p=128))
                for tb in range(HT // 256):
                    for q in range(4):
                        bq = brq[it % 2]
                        it += 1
                        dma("sp", bq[:, :, :],
                            br_all[2 * q + hf].rearrange("(hn p) t -> p hn t", p=128)[:, :, tb * 256:(tb + 1) * 256])
                        dst = brs[:, :, tb * 256:(tb + 1) * 256]
                        if q == 0:
                            tsc("dve", dst, bq[:, :, :], msel[:, 0:1], ALU.mult)
                        else:
                            stt(dst, bq[:, :, :], msel[:, q:q + 1], dst, ALU.mult, ALU.add)
                ldw(wgu[0][:, :], wgu_d[l, 0])
                for d in range(8):
                    if d + 1 < 8:
                        ldw(wgu[(d + 1) % 2][:, :], wgu_d[l, d + 1])
                    W = wgu[d % 2]
                    for tb in range(HT // 512):
                        tsl = slice(tb * 512, (tb + 1) * 512)
                        for n in range(3):
                            pg = PF[n % 2]
                            pu = PM if n % 2 == 0 else PN
                            for kt in range(8):
                                c0 = (n * 12 + kt) * 128
                                mm(pg[:, :], W[:, c0:c0 + 128], uT[:, kt, tsl], start=(kt == 0), stop=(kt == 7))
                            for h in range(4):
                                c0 = (n * 12 + 8 + h) * 128
                                mm(pu[:, :], W[:, c0:c0 + 128], brs[:, h * 3 + n, tsl], start=(h == 0), stop=(h == 3))
                            sg = sgt[n % 2]
                            act(sg[:, :], pg[:, :], AF.Sigmoid)
                            if n == 0:
                                tt("dve", macc[:, :], sg[:, :], pu[:, :], ALU.mult)
                            elif n == 1:
                                tt("dve", sg[:, :], sg[:, :], pu[:, :], ALU.mult)
                                tt("pool", macc[:, :], macc[:, :], sg[:, :], ALU.add)
                            else:
                                tt("dve", sg[:, :], sg[:, :], pu[:, :], ALU.mult)
                                tt("dve", mixT[:, d, tsl], macc[:, :], sg[:, :], ALU.add)
                ldw(wo[0][:, :], wo_d[l, 0])
                for d in range(8):
                    if d + 1 < 8:
                        ldw(wo[(d + 1) % 2][:, :], wo_d[l, d + 1])
                    W = wo[d % 2]
                    for tb in range(HT // 512):
                        tsl = slice(tb * 512, (tb + 1) * 512)
                        xsl = slice(t0 + tb * 512, t0 + (tb + 1) * 512)
                        pq = PT1 if tb % 2 == 0 else PT2
                        for kt in range(8):
                            mm(pq[:, :], W[:, kt * 128:(kt + 1) * 128], mixT[:, kt, tsl], start=(kt == 0), stop=(kt == 7))
                        tt("dve", xT[:, d, xsl], xT[:, d, xsl], pq[:, :], ALU.add)
            S.barrier()

        with ExitStack() as p4:
            def sb4(name, shape, dt=F32):
                return p4.enter_context(nc.sbuf_tensor("%s_l%d" % (name, l), list(shape), dt))
            u2 = sb4("u2T", [128, 8, TOK], BF16)
            sq = sb4("p4sq", [128, 8, 512], BF16)
            rs1 = sb4("p4rs1", [128, 512])
            rs2 = sb4("p4rs2", [128, 512])
            hT = sb4("hT", [128, 8, TOK], BF16)
            rl = [sb4("rl0", [128, 512], BF16), sb4("rl1", [128, 512], BF16)]
            w1 = [sb4("w1_%d" % i, [128, 1024], BF16) for i in range(3)]
            w2 = [sb4("w2_%d" % i, [128, 1024], BF16) for i in range(3)]
            for blk in range(4):
                rmsnorm_block(blk, G_MLP, prm, u2, sq, rs1, rs2, out_sl=slice(blk * 512, (blk + 1) * 512))
            cnt = 0
            for c in range(4):
                ldw(w1[0][:, :], w1_d[l, c * 8])
                for f in range(8):
                    if f + 1 < 8:
                        ldw(w1[(f + 1) % 3][:, :], w1_d[l, c * 8 + f + 1])
                    W = w1[f % 3]
                    for tb in range(4):
                        tsl = slice(tb * 512, (tb + 1) * 512)
                        pq = PF[cnt % 2]
                        r_ = rl[cnt % 2]
                        cnt += 1
                        for kt in range(8):
                            mm(pq[:, :], W[:, kt * 128:(kt + 1) * 128], u2[:, kt, tsl], start=(kt == 0), stop=(kt == 7))
                        act(r_[:, :], pq[:, :], AF.Relu)
                        tt("pool", hT[:, f, tsl], r_[:, :], r_[:, :], ALU.mult)
                ldw(w2[0][:, :], w2_d[l, c * 8])
                for d in range(8):
                    if d + 1 < 8:
                        ldw(w2[(d + 1) % 3][:, :], w2_d[l, c * 8 + d + 1])
                    W = w2[d % 3]
                    for tb in range(4):
                        tsl = slice(tb * 512, (tb + 1) * 512)
                        pq = PM if (tb % 2 == 0) else PN
                        for ft in range(8):
                            mm(pq[:, :], W[:, ft * 128:(ft + 1) * 128], hT[:, ft, tsl], start=(ft == 0), stop=(ft == 7))
                        tt("dve", xT[:, d, tsl], xT[:, d, tsl], pq[:, :], ALU.add)
            S.barrier()

    with ExitStack() as p5:
        def sb5(name, shape, dt=F32):
            return p5.enter_context(nc.sbuf_tensor(name, list(shape), dt))
        outv = out_d.rearrange("(kt p) t -> p kt t", p=128)
        if do_final:
            sq = sb5("p5sq", [128, 8, 512], BF16)
            rs1 = sb5("p5rs1", [128, 512])
            rs2 = sb5("p5rs2", [128, 512])
            ob = [sb5("p5o0", [128, 8, 512]), sb5("p5o1", [128, 8, 512])]
            for blk in range(4):
                rmsnorm_block(blk, 0, gfin, ob[blk % 2], sq, rs1, rs2)
                dma("sp", outv[:, :, blk * 512:(blk + 1) * 512], ob[blk % 2][:, :, :])
        else:
            dma("sp", outv, xT[:, :, :])
        fin = [out_d] + ([dbg_br] if dbg else [])
        S.final_wait("sp", fin)
        S.emit()
    es.close()
    return nc


def _tile_kxm(w, mt):
    K, M = w.shape
    a = w.reshape(K // 128, 128, M // mt, mt)
    return np.ascontiguousarray(a.transpose(2, 1, 0, 3))


def prep_shared(inp, layers):
    wgu, wo, w1, w2 = [], [], [], []
    for l in layers:
        w_in = inp["w_in"][l]
        g = _tile_kxm(np.ascontiguousarray(w_in[:, O_G:O_G + 3072]), 128)
        g = g.reshape(3, 8, 128, 8, 128)
        up = np.stack([_tile_kxm(inp["w_up"][l][n], 128) for n in range(3)])
        blk = np.concatenate([g, up], axis=3)
        blk = np.ascontiguousarray(blk.transpose(1, 2, 0, 3, 4)).reshape(8, 128, 36 * 128)
        wgu.append(blk)
        wo.append(_tile_kxm(inp["w_out"][l], 128).reshape(8, 128, 1024))
        w1.append(_tile_kxm(inp["w_mlp_in"][l], 128).reshape(32, 128, 1024))
        m2 = inp["w_mlp_out"][l].reshape(4, 8, 128, 8, 128)
        w2.append(np.ascontiguousarray(m2.transpose(0, 3, 2, 1, 4)).reshape(32, 128, 1024))
    return {"wgu": np.stack(wgu), "wo": np.stack(wo), "w1": np.stack(w1), "w2": np.stack(w2)}


def prep_core(inp, layers, c):
    h = c % 4
    hs = slice(h * 128, (h + 1) * 128)
    wh, wlr, prm = [], [], []
    for l in layers:
        w = inp["w_in"][l]
        def col(o):
            return w[:, o + h * 128:o + (h + 1) * 128]
        def one(o):
            return w[:, o + h:o + h + 1]
        cat = np.concatenate([col(O_MQ), col(O_MK), col(O_LQ), col(O_LK), col(O_DQ), col(O_DK), col(O_DV),
                              col(O_MK), col(O_MV), col(O_MO), one(O_MI), one(O_MF), one(O_DB), one(O_DA),
                              col(O_LK), col(O_LV), col(O_LR), col(O_DZ),
                              w[:, O_LLR:O_LLR + 16]], axis=1)
        a = cat.reshape(8, 128, WH).transpose(1, 0, 2)
        wh.append(np.ascontiguousarray(a).reshape(128, 8 * WH))
        wlr.append(np.concatenate([inp["w_gla_lr"][l][:, hs], inp["b_gla"][l][None, hs]], axis=0))
        p = np.zeros((128, NPRM), np.float32)
        p[:, 0] = inp["b_if"][l][h]
        p[:, 1] = inp["b_if"][l][4 + h]
        p[:, 2] = inp["a_log"][l][h]
        p[:, 3] = inp["dt_bias"][l][h]
        p[:, 4:12] = inp["g_norm_mix"][l].reshape(8, 128).T
        p[:, 12:20] = inp["g_norm_mlp"][l].reshape(8, 128).T
        p[:, 20] = inp["g_head_mlstm"][l][hs]
        p[:, 21] = inp["g_head_gla"][l][hs]
        p[:, 22] = inp["g_head_gdn"][l][hs]
        for m in range(3):
            for j in range(4):
                p[:, 23 + m * 4 + j] = inp["conv_gdn"][l][j, m * 512 + h * 128:m * 512 + (h + 1) * 128]
        prm.append(p)
    ms = np.zeros((128, 4), np.float32)
    ms[:, h] = 1.0
    return {"wh": np.stack(wh), "wlr": np.stack(wlr).astype(np.float32), "prm": np.stack(prm), "msel": ms,
            "gfin": np.ascontiguousarray(inp["g_final"].reshape(8, 128).T)}


_PROG = {}


STOP = None
SEC_LIMIT = 1000
BLK_LIMIT = 1000


def _get_prog(nl, do_final, dbg=False):
    k = (nl, do_final, dbg, STOP)
    if k not in _PROG:
        _PROG[k] = build_program(nl, do_final, dbg, STOP)
    return _PROG[k]


def _run(inp, x_cores, layers, do_final, dbg=False):
    nc = _get_prog(len(layers), do_final, dbg)
    shared = prep_shared(inp, layers)
    cst = make_consts()
    in_maps = []
    for c in range(NCORES):
        m = {"xT": x_cores[c], "cst": cst}
        m.update(shared)
        m.update(prep_core(inp, layers, c))
        in_maps.append(m)
    res = run_bass_kernel_spmd(nc, in_maps, core_ids=list(range(NCORES)))
    return res


def kernel(**inputs):
    inp = {k: np.asarray(v) for k, v in inputs.items()}
    x = inp["x"]
    x_cores = []
    for c in range(NCORES):
        b, r = c // 4, c % 4
        x_cores.append(np.ascontiguousarray(x[b, r * TOK:(r + 1) * TOK, :].T))
    if FUSED:
        res = _run(inp, x_cores, [0, 1, 2, 3], True)
        outs = [res.results[c]["outT"] for c in range(NCORES)]
    else:
        for l in range(4):
            res = _run(inp, x_cores, [l], l == 3)
            x_cores = [np.ascontiguousarray(res.results[c]["outT"]) for c in range(NCORES)]
        outs = x_cores
    out = np.empty_like(x)
    for c in range(NCORES):
        b, r = c // 4, c % 4
        out[b, r * TOK:(r + 1) * TOK, :] = outs[c].T
    return out
```
